# Optimizing a Trainium2 kernel written in Bass

```python
import jax, jax.numpy as jnp
from jax import lax
import numpy as np

D_MODEL = 2048
BATCH = 8
SEQ = 2048
DEPTH = 4

N_BRANCH = 4
BRANCH_W = 512

ML_HEADS = 4
ML_DQK = 64
ML_DV = 128
ML_CHUNK = 128
ML_CONV = 3

ATT_WINDOW = (128, 512, 2048)
ATT_DIL = (1, 4, 16)
ATT_NGROUP = 3
ATT_HEADS = 8
ATT_DH = 64
ATT_W = ATT_NGROUP * ATT_HEADS * ATT_DH

SG_CHUNK = 128
SG_GROUPS = 4
SG_DG = BRANCH_W // SG_GROUPS

POOL_WINDOWS = (2, 4, 8, 16)
POOL_DG = BRANCH_W // len(POOL_WINDOWS)

N_EGROUPS = 4
EXP_PER_GROUP = 8
N_EXPERTS = N_EGROUPS * EXP_PER_GROUP
TOP_K = 2
D_EXPERT = 512
MOE_BLOCK = 128

ALPHA = (2.0 * DEPTH) ** 0.25
BETA = (8.0 * DEPTH) ** -0.25
LN_EPS = 1e-5
NEG = -1e30

IN_SIZES = (ML_HEADS * ML_DQK, ML_HEADS * ML_DQK, ML_HEADS * ML_DV, ML_HEADS * ML_DV, 2 * 2 * ML_HEADS,
            ATT_W, ATT_W, ATT_W, BRANCH_W, BRANCH_W, BRANCH_W)
D_IN = sum(IN_SIZES)
IN_SPLITS = tuple(int(c) for c in np.cumsum(IN_SIZES)[:-1])

kernel_name = "hybrid_bidir_mlstm_dilattn_sgmlp_pool_hmoe"


def _layer_norm(x, g, b):
    xf = x.astype(jnp.float32)
    mu = xf.mean(-1, keepdims=True)
    var = jnp.mean(jnp.square(xf - mu), -1, keepdims=True)
    return ((xf - mu) * lax.rsqrt(var + LN_EPS) * g + b).astype(x.dtype)


def _conv_centred(x, w):
    k = w.shape[0]
    p = k // 2
    s = x.shape[1]
    xp = jnp.pad(x, ((0, 0), (p, p), (0, 0)))
    return sum(xp[:, j:j + s] * w[j] for j in range(k))


def _mlstm_scan(q, k, v, ig, lf):
    B, H, S, _ = q.shape
    nc = S // ML_CHUNK

    def chunks(a):
        a = a.reshape(a.shape[:2] + (nc, ML_CHUNK) + a.shape[3:])
        return jnp.moveaxis(a, 2, 0)

    tril = jnp.tril(jnp.ones((ML_CHUNK, ML_CHUNK), dtype=bool))

    def step(carry, inp):
        C, n, m = carry
        qc, kc, vc, ic, fc = inp
        b = jnp.cumsum(fc, axis=-1)
        dmat = jnp.where(tril, b[..., :, None] - b[..., None, :] + ic[..., None, :], -jnp.inf)
        m_inter = m[..., None] + b
        m_t = jnp.maximum(m_inter, dmat.max(-1))
        w_inter = jnp.exp(m_inter - m_t)
        s = jnp.einsum('bhtd,bhsd->bhts', qc, kc) * jnp.exp(dmat - m_t[..., None])
        num = w_inter[..., None] * jnp.einsum('bhvd,bhtd->bhtv', C, qc) + jnp.einsum('bhts,bhsv->bhtv', s, vc)
        den = w_inter * jnp.einsum('bhd,bhtd->bht', n, qc) + s.sum(-1)
        h = num / jnp.maximum(jnp.abs(den), jnp.exp(-m_t))[..., None]
        g = b[..., -1:] - b + ic
        m_new = jnp.maximum(m + b[..., -1], g.max(-1))
        decay = jnp.exp(m + b[..., -1] - m_new)
        wk = jnp.exp(g - m_new[..., None])
        C = decay[..., None, None] * C + jnp.einsum('bhs,bhsv,bhsd->bhvd', wk, vc, kc)
        n = decay[..., None] * n + jnp.einsum('bhs,bhsd->bhd', wk, kc)
        return (C, n, m_new), h

    init = (jnp.zeros((B, H, ML_DV, ML_DQK), jnp.float32),
            jnp.zeros((B, H, ML_DQK), jnp.float32),
            jnp.zeros((B, H), jnp.float32))
    _, h = lax.scan(step, init, (chunks(q), chunks(k), chunks(v), chunks(ig), chunks(lf)))
    return jnp.moveaxis(h, 0, 2).reshape(B, H, S, ML_DV)


def _mlstm_branch(q, k, v, o, gates, conv_w, gate_b, norm_w):
    B, S, _ = q.shape
    qk = jax.nn.silu(_conv_centred(jnp.concatenate([q, k], axis=-1), conv_w))
    q, k = jnp.split(qk, 2, axis=-1)

    def heads(a, d):
        return a.reshape(B, S, ML_HEADS, d).transpose(0, 2, 1, 3).astype(jnp.float32)

    qh = heads(q, ML_DQK) * (ML_DQK ** -0.5)
    kh, vh = heads(k, ML_DQK), heads(v, ML_DV)
    g = (gates.astype(jnp.float32).reshape(B, S, 2, 2, ML_HEADS) + gate_b).transpose(2, 3, 0, 4, 1)
    h_fwd = _mlstm_scan(qh, kh, vh, g[0, 0], jax.nn.log_sigmoid(g[0, 1]))
    fl = lambda a: jnp.flip(a, axis=2)
    h_bwd = fl(_mlstm_scan(fl(qh), fl(kh), fl(vh), fl(g[1, 0]), fl(jax.nn.log_sigmoid(g[1, 1]))))
    h = h_fwd + h_bwd
    mu = h.mean(-1, keepdims=True)
    var = jnp.mean(jnp.square(h - mu), -1, keepdims=True)
    hn = ((h - mu) * lax.rsqrt(var + LN_EPS)).transpose(0, 2, 1, 3).reshape(B, S, ML_HEADS * ML_DV)
    return hn * norm_w * jax.nn.sigmoid(o.astype(jnp.float32))


def _alibi_slopes():
    n = ATT_NGROUP * ATT_HEADS
    s = 2.0 ** (-8.0 * np.arange(1, n + 1) / n)
    return jnp.asarray(s.reshape(ATT_NGROUP, ATT_HEADS), jnp.float32)


def _banded_attention(q, k, v, slope, w):
    N, L, H, Dh = q.shape
    nb = -(-L // w)
    Lp = nb * w
    qb = jnp.pad(q, ((0, 0), (0, Lp - L), (0, 0), (0, 0))).reshape(N, nb, w, H, Dh)
    padk = ((0, 0), (w, Lp - L + w), (0, 0), (0, 0))
    kp, vp = jnp.pad(k, padk), jnp.pad(v, padk)

    def band(a):
        return jnp.concatenate([a[:, j * w:j * w + Lp].reshape(N, nb, w, H, Dh) for j in range(3)], axis=2)

    kb, vb = band(kp), band(vp)
    qi = jnp.arange(nb)[:, None, None] * w + jnp.arange(w)[None, :, None]
    ki = jnp.arange(nb)[:, None, None] * w - w + jnp.arange(3 * w)[None, None, :]
    rel = ki - qi
    valid = (jnp.abs(rel) <= w) & (ki >= 0) & (ki < L)
    s = jnp.einsum('nbqhd,nbkhd->nbhqk', qb, kb).astype(jnp.float32) * (Dh ** -0.5)
    s = s - slope[None, :, None, None] * jnp.abs(rel).astype(jnp.float32)[:, None]
    s = jnp.where(valid[:, None], s, NEG)
    lse = jax.nn.logsumexp(s, axis=-1)
    p = jnp.exp(s - lse[..., None])
    o = jnp.einsum('nbhqk,nbkhd->nbqhd', p, vb).reshape(N, Lp, H, Dh)[:, :L]
    lse = lse.transpose(0, 1, 3, 2).reshape(N, Lp, H)[:, :L]
    return o, lse


def _dilated_attention(q, k, v):
    B, S = q.shape[:2]
    slopes = _alibi_slopes()
    outs, lses = [], []
    for g in range(ATT_NGROUP):
        dil = ATT_DIL[g]
        neigh = ATT_WINDOW[g] // (2 * dil)
        Ls = S // dil

        def to_sub(a):
            return a.reshape(B, Ls, dil, ATT_HEADS, ATT_DH).transpose(0, 2, 1, 3, 4).reshape(B * dil, Ls, ATT_HEADS, ATT_DH)

        o, lse = _banded_attention(to_sub(q[:, :, g]), to_sub(k[:, :, g]), to_sub(v[:, :, g]), slopes[g] * dil, neigh)
        outs.append(o.reshape(B, dil, Ls, ATT_HEADS, ATT_DH).transpose(0, 2, 1, 3, 4).reshape(B, S, ATT_HEADS, ATT_DH))
        lses.append(lse.reshape(B, dil, Ls, ATT_HEADS).transpose(0, 2, 1, 3).reshape(B, S, ATT_HEADS))
    wgt = jax.nn.softmax(jnp.stack(lses, 0), axis=0)
    out = jnp.einsum('gbsh,gbshd->bshd', wgt, jnp.stack(outs, 0))
    return out.reshape(B, S, ATT_HEADS * ATT_DH)


def _spatial_gating(u, v, ln_g, ln_b, w_s, b_s):
    B, S, _ = u.shape
    u, v = jax.nn.gelu(u), jax.nn.gelu(v)
    v = _layer_norm(v, ln_g, ln_b)
    vc = v.reshape(B, S // SG_CHUNK, SG_CHUNK, SG_GROUPS, SG_DG)
    mixed = jnp.einsum('gts,bcsgd->bctgd', w_s, vc) + b_s.T[None, None, :, :, None]
    return u * mixed.reshape(B, S, BRANCH_W)


def _multiscale_pool(p, w_pool, scale):
    B, S, _ = p.shape
    cs = jnp.pad(jnp.cumsum(p.astype(jnp.float32), axis=1), ((0, 0), (1, 0), (0, 0)))
    t = jnp.arange(S)
    outs = []
    for g, win in enumerate(POOL_WINDOWS):
        sl = slice(g * POOL_DG, (g + 1) * POOL_DG)
        lo = jnp.clip(t - win // 2, 0, S)
        hi = jnp.clip(t + win // 2, 0, S)
        mean = (cs[:, hi, sl] - cs[:, lo, sl]) / (hi - lo).astype(jnp.float32)[None, :, None]
        outs.append(mean - p[:, :, sl])
    d = jnp.stack(outs, axis=2)
    y = jnp.einsum('bsgc,gcd->bsgd', d, w_pool).reshape(B, S, BRANCH_W)
    return y * scale


def _mixer(x, w_in, ml_conv_w, ml_gate_b, ml_norm_w, sg_ln_g, sg_ln_b, sg_w, sg_b,
           pool_w, pool_scale, w_gate, b_gate, w_branch, w_out):
    B, S, D = x.shape
    z = jnp.einsum('bsd,dc->bsc', x, w_in)
    mq, mk, mv, mo, mg, aq, ak, av, su, sv, pp = jnp.split(z, IN_SPLITS, axis=-1)
    y_ml = _mlstm_branch(mq, mk, mv, mo, mg, ml_conv_w, ml_gate_b, ml_norm_w)
    heads = lambda a: a.reshape(B, S, ATT_NGROUP, ATT_HEADS, ATT_DH)
    y_at = _dilated_attention(heads(aq), heads(ak), heads(av))
    y_sg = _spatial_gating(su, sv, sg_ln_g, sg_ln_b, sg_w, sg_b)
    y_pl = _multiscale_pool(pp, pool_w, pool_scale)
    ys = jnp.stack([y_ml.astype(x.dtype), y_at.astype(x.dtype), y_sg.astype(x.dtype), y_pl.astype(x.dtype)], axis=2)
    proj = jnp.einsum('bsnc,ncd->bsnd', ys, w_branch)
    gates = jax.nn.sigmoid(jnp.einsum('bsd,de->bse', x, w_gate) + b_gate).reshape(B, S, N_BRANCH, D)
    merged = jnp.einsum('bsnd,bsnd->bsd', gates, proj)
    return jnp.einsum('bsd,de->bse', merged, w_out)


def _expert_blocks(xf, w1, w3, w2, e_id, wts):
    T, D = xf.shape
    A = T * TOP_K
    e_flat = e_id.reshape(A).astype(jnp.int32)
    tok = jnp.arange(A, dtype=jnp.int32) // TOP_K
    counts = jnp.bincount(e_flat, length=N_EXPERTS)
    padded = (counts + MOE_BLOCK - 1) // MOE_BLOCK * MOE_BLOCK
    pad_end = jnp.cumsum(padded)
    pad_start = pad_end - padded
    raw_start = jnp.cumsum(counts) - counts
    order = jnp.argsort(e_flat)
    e_sorted = e_flat[order]
    dest = pad_start[e_sorted] + jnp.arange(A) - raw_start[e_sorted]
    nb = -(-A // MOE_BLOCK) + N_EXPERTS
    slot_tok = jnp.zeros(nb * MOE_BLOCK, jnp.int32).at[dest].set(tok[order])
    slot_w = jnp.zeros(nb * MOE_BLOCK, wts.dtype).at[dest].set(wts.reshape(A)[order])
    blk_e = jnp.minimum(jnp.searchsorted(pad_end, jnp.arange(nb) * MOE_BLOCK, side='right'), N_EXPERTS - 1)

    def run(args):
        e, idx, w = args
        xb = xf[idx]
        h = jax.nn.silu(xb @ w1[e]) * (xb @ w3[e])
        return (h @ w2[e]) * w[:, None].astype(xf.dtype)

    yb = lax.map(run, (blk_e, slot_tok.reshape(nb, MOE_BLOCK), slot_w.reshape(nb, MOE_BLOCK)))
    return jax.ops.segment_sum(yb.reshape(nb * MOE_BLOCK, D), slot_tok, num_segments=T)


def _hmoe(x, w_rg, b_rg, w_re, b_re, w1, w3, w2):
    B, S, D = x.shape
    T = B * S
    xf = x.reshape(T, D)
    lg = (xf @ w_rg).astype(jnp.float32) + b_rg
    pg = jax.nn.softmax(lg, axis=-1)
    g_sel = jnp.argmax(lg, axis=-1)
    p_sel = jnp.take_along_axis(pg, g_sel[:, None], axis=-1)
    le = ((xf @ w_re).astype(jnp.float32) + b_re).reshape(T, N_EGROUPS, EXP_PER_GROUP)
    le = jnp.take_along_axis(le, g_sel[:, None, None], axis=1)[:, 0]
    top_l, top_i = lax.top_k(le, TOP_K)
    wts = jax.nn.softmax(top_l, axis=-1) * p_sel
    e_id = g_sel[:, None] * EXP_PER_GROUP + top_i
    return _expert_blocks(xf, w1, w3, w2, e_id, wts).reshape(B, S, D)


def setup_inputs(seed: int = 0) -> dict:
    key = jax.random.key(seed)
    ks = jax.random.split(key, 32)
    L, D = DEPTH, D_MODEL
    nrm = lambda i, shape, scale: jax.random.normal(ks[i], shape, jnp.float32) * scale
    x = nrm(0, (BATCH, SEQ, D), 1.0)
    w_in = nrm(1, (L, D, D_IN), D ** -0.5)
    ml_conv_w = 1.0 / ML_CONV + nrm(2, (L, ML_CONV, 2 * ML_HEADS * ML_DQK), 0.2)
    ig_b = nrm(3, (L, 2, 1, ML_HEADS), 0.1)
    fg_b = 3.0 + 3.0 * jax.random.uniform(ks[4], (L, 2, 1, ML_HEADS), jnp.float32)
    ml_gate_b = jnp.concatenate([ig_b, fg_b], axis=2)
    ml_norm_w = 1.0 + nrm(5, (L, ML_HEADS * ML_DV), 0.02)
    sg_ln_g = 1.0 + nrm(6, (L, BRANCH_W), 0.02)
    sg_ln_b = nrm(7, (L, BRANCH_W), 0.02)
    sg_w = nrm(8, (L, SG_GROUPS, SG_CHUNK, SG_CHUNK), SG_CHUNK ** -0.5)
    sg_b = 1.0 + nrm(9, (L, SG_GROUPS, SG_CHUNK), 0.02)
    pool_w = nrm(10, (L, len(POOL_WINDOWS), POOL_DG, POOL_DG), POOL_DG ** -0.5)
    pool_scale = 1.0 + nrm(11, (L, BRANCH_W), 0.02)
    w_gate = nrm(12, (L, D, N_BRANCH * D), D ** -0.5)
    b_gate = nrm(13, (L, N_BRANCH * D), 0.02)
    w_branch = nrm(14, (L, N_BRANCH, BRANCH_W, D), BRANCH_W ** -0.5 * BETA)
    w_out = nrm(15, (L, D, D), D ** -0.5 * BETA)
    ln1_g = 1.0 + nrm(16, (L, D), 0.02)
    ln1_b = nrm(17, (L, D), 0.02)
    w_router_group = nrm(18, (L, D, N_EGROUPS), D ** -0.5)
    b_router_group = nrm(19, (L, N_EGROUPS), 0.01)
    w_router_expert = nrm(20, (L, D, N_EXPERTS), D ** -0.5)
    b_router_expert = nrm(21, (L, N_EXPERTS), 0.01)
    w_exp_gate = nrm(22, (L, N_EXPERTS, D, D_EXPERT), D ** -0.5)
    w_exp_up = nrm(23, (L, N_EXPERTS, D, D_EXPERT), D ** -0.5)
    w_exp_down = nrm(24, (L, N_EXPERTS, D_EXPERT, D), D_EXPERT ** -0.5 * BETA)
    ln2_g = 1.0 + nrm(25, (L, D), 0.02)
    ln2_b = nrm(26, (L, D), 0.02)
    return {"x": x, "w_in": w_in, "ml_conv_w": ml_conv_w, "ml_gate_b": ml_gate_b, "ml_norm_w": ml_norm_w,
            "sg_ln_g": sg_ln_g, "sg_ln_b": sg_ln_b, "sg_w": sg_w, "sg_b": sg_b,
            "pool_w": pool_w, "pool_scale": pool_scale, "w_gate": w_gate, "b_gate": b_gate,
            "w_branch": w_branch, "w_out": w_out, "ln1_g": ln1_g, "ln1_b": ln1_b,
            "w_router_group": w_router_group, "b_router_group": b_router_group,
            "w_router_expert": w_router_expert, "b_router_expert": b_router_expert,
            "w_exp_gate": w_exp_gate, "w_exp_up": w_exp_up, "w_exp_down": w_exp_down,
            "ln2_g": ln2_g, "ln2_b": ln2_b}


def reference(x, w_in, ml_conv_w, ml_gate_b, ml_norm_w, sg_ln_g, sg_ln_b, sg_w, sg_b,
              pool_w, pool_scale, w_gate, b_gate, w_branch, w_out, ln1_g, ln1_b,
              w_router_group, b_router_group, w_router_expert, b_router_expert,
              w_exp_gate, w_exp_up, w_exp_down, ln2_g, ln2_b):
    for l in range(DEPTH):
        y = _mixer(x, w_in[l], ml_conv_w[l], ml_gate_b[l], ml_norm_w[l], sg_ln_g[l], sg_ln_b[l],
                   sg_w[l], sg_b[l], pool_w[l], pool_scale[l], w_gate[l], b_gate[l], w_branch[l], w_out[l])
        x = _layer_norm(ALPHA * x + y, ln1_g[l], ln1_b[l])
        y = _hmoe(x, w_router_group[l], b_router_group[l], w_router_expert[l], b_router_expert[l],
                  w_exp_gate[l], w_exp_up[l], w_exp_down[l])
        x = _layer_norm(ALPHA * x + y, ln2_g[l], ln2_b[l])
    return x
```

```python
import contextlib
import math
import os
import numpy as np
import concourse.bass as bass
import concourse.mybir as mybir
from concourse.bass_utils import run_bass_kernel_spmd

F32 = mybir.dt.float32
BF16 = mybir.dt.bfloat16
I32 = mybir.dt.int32
AF = mybir.ActivationFunctionType
ALU = mybir.AluOpType
AX = mybir.AxisListType

S = 2048
D = 2048
NT = 16
D_IN = 7696
OFF_MQ, OFF_MK, OFF_MV, OFF_MO, OFF_MG = 0, 256, 512, 1024, 1536
OFF_AQ, OFF_AK, OFF_AV = 1552, 3088, 4624
OFF_SU, OFF_SV, OFF_PP = 6160, 6672, 7184
ALPHA = 8.0 ** 0.25
LN_EPS = 1e-5
ATT_R = (1, 2, 8)
ATT_DIL = (1, 4, 16)
NBLK = 48
BLKS = 256
NSLOT = NBLK * BLKS
ARN = 67000

ENGS = ("sync", "act", "dve", "pool", "pe")
N_DMA_SEMS = 16


class Prog:
    def __init__(self, nc):
        self.nc = nc
        self.ops = {e: [] for e in ENGS}
        self.cnt = {e: 0 for e in ENGS}
        self.dcnt = {e: 0 for e in ENGS}
        self.res = {}
        self.last = {}
        self.bar = {e: set() for e in ENGS}

    def _deps(self, eng, reads, writes):
        deps = set(self.bar[eng])
        self.bar[eng] = set()
        for k in reads:
            r = self.res.get(k)
            if r and r[0] is not None:
                deps.add(r[0])
        for k in writes:
            r = self.res.get(k)
            if r:
                if r[0] is not None:
                    deps.add(r[0])
                deps.update(r[1])
        return deps

    def _commit(self, tok, reads, writes):
        self.last[tok[0]] = tok
        for k in reads:
            r = self.res.setdefault(k, [None, []])
            r[1].append(tok)
        for k in writes:
            self.res[k] = [tok, []]

    def op(self, eng, fn, reads=(), writes=()):
        deps = self._deps(eng, reads, writes)
        self.cnt[eng] += 1
        tok = (("c", eng), self.cnt[eng], eng)
        self.ops[eng].append((fn, deps, tok, 1))
        self._commit(tok, reads, writes)
        return tok

    def dma(self, eng, fn, reads=(), writes=()):
        deps = self._deps(eng, reads, writes)
        i = self.dcnt[eng]
        self.dcnt[eng] += 1
        tok = (("d", eng, i % N_DMA_SEMS), 16 * (i // N_DMA_SEMS + 1), "dma_" + eng)
        prev = self.last.get(tok[0])
        if prev is not None:
            deps.add(prev)
        self.ops[eng].append((fn, deps, tok, 16))
        self._commit(tok, reads, writes)
        return tok

    def barrier(self):
        toks = set(self.last.values())
        for e in ENGS:
            self.bar[e] |= toks
        self.res = {}

    def emit(self):
        nc = self.nc
        finals = set(self.last.values())
        with contextlib.ExitStack() as st:
            sems = {}
            for e in ("act", "dve", "pool", "pe"):
                sems[("c", e)] = st.enter_context(nc.semaphore("c_" + e))
            for e in ("sync", "act", "pool"):
                for j in range(N_DMA_SEMS):
                    sems[("d", e, j)] = st.enter_context(nc.semaphore("d_%s_%d" % (e, j)))
            block = st.enter_context(nc.Block())
            ops = self.ops

            def run(engname, engobj, fin=()):
                waited = {}

                def waits(deps):
                    for (sk, val, deng) in sorted(deps, key=str):
                        if deng == "pe" and engname == "pe":
                            continue
                        if waited.get(sk, 0) >= val:
                            continue
                        engobj.wait_ge(sems[sk], val)
                        waited[sk] = val

                for fn, deps, tok, inc in ops[engname]:
                    waits(deps)
                    ins = fn(engobj)
                    ins.then_inc(sems[tok[0]], inc)
                waits([f for f in fin if not (f[2] == "pe" and engname == "pe")])

            @block.sync
            def _(sync):
                run("sync", sync, finals)

            @block.scalar
            def _(scalar):
                run("act", scalar)

            @block.vector
            def _(vector):
                run("dve", vector)

            @block.gpsimd
            def _(gpsimd):
                run("pool", gpsimd)

            @block.tensor
            def _(tensor):
                run("pe", tensor)


STOP = [None]
SKIP_UNUSED = bool(int(os.environ.get("SKIP_UNUSED", "1")))


def alibi_slope(g, h):
    n = 24
    return 2.0 ** (-8.0 * (g * 8 + h + 1) / n)


def host_consts():
    c = {}
    c["ident"] = np.eye(128, dtype=np.float32)
    s = np.arange(128)[:, None]
    t = np.arange(128)[None, :]
    c["tri_f"] = (s <= t).astype(np.float32)
    c["tri_b"] = (s >= t).astype(np.float32)
    c["mneg_f"] = np.where(s <= t, 0.0, -30000.0).astype(np.float32)
    c["mneg_b"] = np.where(s >= t, 0.0, -30000.0).astype(np.float32)
    c["tri_s"] = (s < t).astype(np.float32)
    c["ones"] = np.ones((128, 128), np.float32)
    tiles = []
    for g in range(3):
        dil = ATT_DIL[g]
        for o in range(-ATT_R[g], ATT_R[g] + 1):
            delta = (t - s) - 128 * o
            ok = (np.abs(delta) <= 64 * dil) & (delta % dil == 0)
            tiles.append(np.where(ok, np.abs(delta).astype(np.float32), 1e5))
    c["dist"] = np.ascontiguousarray(np.stack(tiles, axis=1).astype(np.float32))
    pe = np.zeros((128, 4, 2, 8), np.float32)
    for g, w in enumerate((2, 4, 8, 16)):
        h = w // 2
        for j in range(h):
            tt = j
            pe[:, g, 0, j] = 1.0 / (min(tt + h, S) - max(tt - h, 0))
            tt = S - h + j
            pe[:, g, 1, j] = 1.0 / (min(tt + h, S) - max(tt - h, 0))
    c["pool_edge"] = pe.reshape(128, 64)
    thr = np.zeros((128, NBLK, 32), np.float32)
    thr[:] = (float(BLKS) * np.arange(NBLK))[None, :, None]
    c["thr"] = thr.reshape(128, NBLK * 32)
    io = np.zeros((128, 4), np.float32)
    io[:] = 4.0 * np.arange(128)[:, None] + np.arange(4)[None, :]
    c["iow"] = io
    return c


CONST_SHAPES = {"ident": [128, 128], "tri_f": [128, 128], "tri_b": [128, 128], "mneg_f": [128, 128],
                "mneg_b": [128, 128], "tri_s": [128, 128], "ones": [128, 128], "dist": [128, 25, 128],
                "pool_edge": [128, 64], "thr": [128, NBLK * 32], "iow": [128, 4]}

WEIGHT_SHAPES = {
    "w_in": [D, D_IN], "ml_conv_w": [3, 512], "ml_gate_b": [16], "ml_norm_w": [512],
    "sg_ln_g": [512], "sg_ln_b": [512], "sg_w": [4, 128, 128], "sg_b": [4, 128],
    "pool_w": [4, 128, 128], "pool_scale": [512], "w_gate": [D, 4 * D], "b_gate": [4 * D],
    "w_branch": [4, 512, D], "w_out": [D, D], "ln1_g": [D], "ln1_b": [D],
    "w_router_group": [D, 4], "b_router_group": [4], "w_router_expert": [D, 32], "b_router_expert": [32],
    "w_exp_gate": [32, D, 512], "w_exp_up": [32, D, 512], "w_exp_down": [32, 512, D],
    "ln2_g": [D], "ln2_b": [D],
    "w_router": [128, 16 * 36], "b_router": [36],
}


def build(depth, stop_after=None, dbg=False):
    nc = bass.Bass("TRN2", target_bir_lowering=False)
    x_in = nc.dram_tensor("x", [S, D], F32, kind="ExternalInput").ap()
    W = {k: nc.dram_tensor(k, [depth] + v, F32, kind="ExternalInput").ap() for k, v in WEIGHT_SHAPES.items()}
    C = {k: nc.dram_tensor("c_" + k, v, F32, kind="ExternalInput").ap() for k, v in CONST_SHAPES.items()}
    y_out = nc.dram_tensor("y", [S, D], F32, kind="ExternalOutput").ap()
    okind = "ExternalOutput" if dbg else "Internal"
    XA = nc.dram_tensor("XA", [S, D], F32, kind=okind).ap()
    XB = nc.dram_tensor("XB", [S, D], F32, kind="Internal").ap()
    YST = nc.dram_tensor("YST", [4, 4, 128, S], BF16, kind=okind).ap()
    MT = nc.dram_tensor("MT", [16, 128, S], BF16, kind="Internal").ap()
    XS = nc.dram_tensor("XS", [NSLOT, D], BF16, kind="Internal").ap()
    YB = nc.dram_tensor("YB", [NSLOT, D], F32, kind="Internal").ap()

    st = contextlib.ExitStack()
    with st:
        def sb(name, shape, dt):
            return st.enter_context(nc.sbuf_tensor(name, shape, dt))

        xT = sb("xT", [128, 16, S], BF16)
        identb = sb("identb", [128, 128], BF16)
        identf = sb("identf", [128, 128], F32)
        onesf = sb("onesf", [128, 128], F32)
        AR = sb("AR", [128, ARN], BF16)
        logits = sb("logits", [128, NT * 36], F32)
        ps = [st.enter_context(nc.psum_tensor("ps%d" % i, [128, 512], F32)) for i in range(8)]
        p = Prog(nc)

        class Arena:
            def __init__(self):
                self.off = 0

            def reset(self):
                self.off = 0

            def f32(self, n):
                o = (self.off + 15) // 16 * 16
                self.off = o + 2 * n
                assert self.off <= ARN, self.off
                return AR[:, o:o + 2 * n].bitcast(F32)

            def bf(self, n):
                o = (self.off + 15) // 16 * 16
                self.off = o + n
                assert self.off <= ARN, self.off
                return AR[:, o:o + n]

            def i32(self, n):
                o = (self.off + 15) // 16 * 16
                self.off = o + 2 * n
                assert self.off <= ARN, self.off
                return AR[:, o:o + 2 * n].bitcast(I32)

        ar = Arena()

        def psb(i, n=512):
            return ps[i][:, 0:n // 2].bitcast(BF16)

        p.dma("pool", lambda e: e.dma_start(out=identb[:], in_=C["ident"]), writes=["identb"])
        p.dma("sync", lambda e: e.dma_start(out=identf[:], in_=C["ident"]), writes=["identf"])
        p.dma("sync", lambda e: e.dma_start(out=onesf[:], in_=C["ones"]), writes=["onesf"])

        def tile_to_xT(src, src_key, tt, pbank, hb, hb_key, lo=None):
            cp(p, "act", hb, src, reads=[src_key], writes=[hb_key])
            for half in range(2):
                bank = pbank + half
                pk = "ps%d" % bank
                for jj in range(8):
                    dc = half * 8 + jj
                    trp(p, psb(bank, 1024)[:, jj * 128:(jj + 1) * 128], hb[:, dc * 128:(dc + 1) * 128], identb[:],
                        reads=[hb_key, "identb"], writes=[pk])
                cp(p, "act" if half == 0 else "dve", xT[:, half * 8:(half + 1) * 8, tt * 128:(tt + 1) * 128],
                   psb(bank, 1024).rearrange("p (a b) -> p a b", b=128), reads=[pk], writes=[("xT", tt)])
            if lo is not None:
                lob, lob_key, loT, loT_key = lo
                tt_(p, "dve", lob, src, hb, ALU.subtract, reads=[src_key, hb_key], writes=[lob_key])
                for half in range(2):
                    bank = pbank + half
                    pk = "ps%d" % bank
                    for jj in range(8):
                        dc = half * 8 + jj
                        trp(p, psb(bank, 1024)[:, jj * 128:(jj + 1) * 128], lob[:, dc * 128:(dc + 1) * 128], identb[:],
                            reads=[lob_key, "identb"], writes=[pk])
                    cp(p, "act" if half == 0 else "dve", loT[:, half * 8:(half + 1) * 8, :],
                       psb(bank, 1024).rearrange("p (a b) -> p a b", b=128), reads=[pk], writes=[loT_key])

        XT_ALL = [("xT", tt) for tt in range(NT)]

        def load_w_fm(dst, key, wap, lo, ncols):
            p.dma("pool", lambda e: e.dma_start(
                out=dst, in_=wap[:, lo:lo + ncols].rearrange("(dc q) c -> q dc c", q=128)), writes=[key])

        def bcast_load(dst, key, vec_ap, n):
            for c0 in range(0, n, 512):
                c1 = min(n, c0 + 512)
                p.dma("sync", lambda e, c0=c0, c1=c1: e.dma_start(out=dst[:, c0:c1], in_=vec_ap[c0:c1].partition_broadcast(128)), writes=[key])

        def layer_norm_rows(src, src_key, dst, dst_key, n, gt, bt, gb_keys, stats, mv, tmpk):
            nch = max(1, n // 512)
            w = n // nch
            for c in range(nch):
                p.op("dve", lambda e, c=c: e.bn_stats(out=stats[:, c * 6:(c + 1) * 6], in_=src[:, c * w:(c + 1) * w]),
                     reads=[src_key], writes=[tmpk + "st"])
            p.op("dve", lambda e: e.bn_aggr(out=mv[:, 0:2], in_=stats[:, 0:nch * 6]), reads=[tmpk + "st"], writes=[tmpk + "mv"])
            p.op("act", lambda e: e.activation(out=mv[:, 2:3], in_=mv[:, 1:2], func=AF.Sqrt, bias=LN_EPS),
                 reads=[tmpk + "mv"], writes=[tmpk + "sd"])
            p.op("dve", lambda e: e.reciprocal(out=mv[:, 3:4], in_=mv[:, 2:3]), reads=[tmpk + "sd"], writes=[tmpk + "rs"])
            p.op("dve", lambda e: e.tensor_scalar(out=dst, in0=src, scalar1=mv[:, 0:1], scalar2=mv[:, 3:4],
                                                  op0=ALU.subtract, op1=ALU.mult),
                 reads=[src_key, tmpk + "mv", tmpk + "rs"], writes=[dst_key])
            if gt is not None:
                p.op("dve", lambda e: e.tensor_tensor(out=dst, in0=dst, in1=gt, op=ALU.mult),
                     reads=[dst_key, gb_keys[0]], writes=[dst_key])
                p.op("dve", lambda e: e.tensor_tensor(out=dst, in0=dst, in1=bt, op=ALU.add),
                     reads=[dst_key, gb_keys[1]], writes=[dst_key])

        cur_x = x_in
        for l in range(depth):
            last = (l == depth - 1)
            wl = {k: v[l] for k, v in W.items()}
            wl["_l"] = l
            wl["_W"] = W
            if l == 0:
                p.barrier()
                ar.reset()
                xt_tiles = [ar.f32(2048) for _ in range(2)]
                hbA = [ar.bf(2048) for _ in range(2)]
                for tt in range(NT):
                    b = tt % 2
                    p.dma("sync", lambda e, tt=tt, b=b, cx=cur_x: e.dma_start(out=xt_tiles[b], in_=cx[tt * 128:(tt + 1) * 128, :]),
                          writes=[("xld", b)])
                    tile_to_xT(xt_tiles[b], ("xld", b), tt, 0, hbA[b], ("hbA", b))

            if stop_after == "A":
                break
            STOP[0] = stop_after
            stage_mlstm(nc, p, ar, ps, psb, xT, XT_ALL, identb, identf, onesf, wl, C, YST, load_w_fm, bcast_load,
                        layer_norm_rows)
            if stop_after in ("mlstm", "ml1", "ml2"):
                break
            stage_attn(nc, p, ar, ps, psb, xT, XT_ALL, identb, wl, C, YST, load_w_fm)
            if stop_after == "attn":
                break
            stage_sg(nc, p, ar, ps, psb, xT, XT_ALL, identb, identf, wl, C, YST, load_w_fm, bcast_load, layer_norm_rows)
            stage_pool(nc, p, ar, ps, psb, xT, XT_ALL, wl, C, YST, load_w_fm)
            if stop_after == "branches":
                break
            stage_merge(nc, p, ar, ps, psb, xT, XT_ALL, identf, wl, C, YST, MT, cur_x, XA, load_w_fm, bcast_load,
                        layer_norm_rows, tile_to_xT, logits=(None if os.environ.get("NOROUTER") else logits))
            if stop_after in ("ln1", "mg1"):
                break
            dst = y_out if last else XB
            stage_moe(nc, p, ar, ps, psb, xT, XT_ALL, identb, identf, onesf, wl, C, XA, XS, YB, dst, bcast_load,
                      layer_norm_rows, tile_to_xT, logits, first=(l == 0), last=last)
            cur_x = XB
        p.barrier()
        p.emit()
    return nc


def mm(p, out, lhsT, rhs, start=True, stop=True, reads=(), writes=()):
    return p.op("pe", lambda e: e.matmul(out, lhsT=lhsT, rhs=rhs, start=start, stop=stop), reads, writes)


def trp(p, out, in_, ident, reads=(), writes=()):
    return p.op("pe", lambda e: e.transpose(out=out, in_=in_, identity=ident), reads, writes)


def act(p, out, in_, func, reads=(), writes=(), bias=None, scale=None):
    kw = {}
    if bias is not None:
        kw["bias"] = bias
    if scale is not None:
        kw["scale"] = scale
    return p.op("act", lambda e: e.activation(out=out, in_=in_, func=func, **kw), reads, writes)


def ts(p, eng, out, in0, s1, s2, op0, op1=None, reads=(), writes=()):
    if op1 is None:
        return p.op(eng, lambda e: e.tensor_scalar(out=out, in0=in0, scalar1=s1, scalar2=None, op0=op0), reads, writes)
    return p.op(eng, lambda e: e.tensor_scalar(out=out, in0=in0, scalar1=s1, scalar2=s2, op0=op0, op1=op1), reads, writes)


def stt(p, out, in0, scalar, in1, op0, op1, reads=(), writes=()):
    return p.op("dve", lambda e: e.scalar_tensor_tensor(out=out, in0=in0, scalar=scalar, in1=in1, op0=op0, op1=op1),
                reads, writes)


def tt_(p, eng, out, in0, in1, op, reads=(), writes=()):
    return p.op(eng, lambda e: e.tensor_tensor(out=out, in0=in0, in1=in1, op=op), reads, writes)


def cp(p, eng, out, in_, reads=(), writes=()):
    if eng == "act":
        return p.op("act", lambda e: e.activation(out=out, in_=in_, func=AF.Copy), reads, writes)
    return p.op(eng, lambda e: e.tensor_copy(out=out, in_=in_), reads, writes)


def ms(p, eng, ap, val, writes=()):
    return p.op(eng, lambda e: e.memset(ap, val), (), writes)


def dm(p, eng, out, in_, reads=(), writes=(), **kw):
    return p.dma(eng, lambda e: e.dma_start(out=out, in_=in_, **kw), reads, writes)


def xt_keys(t0, n):
    return [("xT", t) for t in range(t0, t0 + n)]


def proj_fm(p, ps, xT, wt, wkey, ncols, evac):
    for q in range(4):
        b = q % 2
        pk = "ps%d" % b
        for dc in range(16):
            mm(p, ps[b][0:ncols, :], wt[:, dc, :], xT[:, dc, q * 512:(q + 1) * 512], dc == 0, dc == 15,
               reads=[wkey] + xt_keys(q * 4, 4), writes=[pk])
        evac(q, ps[b][0:ncols, :], pk)


def proj_tm(p, ps, xT, wt, wkey, ncols, tile, bank):
    pk = "ps%d" % bank
    for dc in range(16):
        mm(p, ps[bank][:, 0:ncols], xT[:, dc, tile * 128:(tile + 1) * 128], wt[:, dc, :], dc == 0, dc == 15,
           reads=[wkey, ("xT", tile)], writes=[pk])
    return ps[bank][:, 0:ncols], pk


def stage_mlstm(nc, p, ar, ps, psb, xT, XT_ALL, identb, identf, onesf, wl, C, YST, load_w_fm, bcast_load,
                layer_norm_rows):
    p.barrier()
    ar.reset()
    w_in = wl["w_in"]
    qkT = ar.bf(4 * S).rearrange("p (c t) -> p c t", t=S)
    ktok = ar.bf(NT * 256).rearrange("p (a b) -> p a b", b=256)
    vaug = ar.bf(NT * 4 * 129).rearrange("p (a h v) -> p a h v", h=4, v=129)
    convw = ar.f32(12)
    gb = ar.f32(16)
    normw = ar.f32(512)
    tri = [ar.f32(128), ar.f32(128)]
    mneg = [ar.f32(128), ar.f32(128)]
    gtok = ar.f32(NT * 16).rearrange("p (a b) -> p a b", b=16)
    G = {k: ar.f32(128) for k in ("lf", "ig", "b", "tot", "imb", "wk", "dec", "ebt", "tmp")}
    mark = ar.off
    for j in range(3):
        dm(p, "sync", convw[:, j * 4:(j + 1) * 4], wl["ml_conv_w"][j].rearrange("(c q) -> q c", q=128),
           writes=["convw"], allow_slow_non_contiguous=True)
    bcast_load(gb, "gb", wl["ml_gate_b"], 16)
    bcast_load(normw, "normw", wl["ml_norm_w"], 512)
    dm(p, "sync", tri[0], C["tri_f"], writes=["tri0"])
    dm(p, "sync", tri[1], C["tri_b"], writes=["tri1"])
    dm(p, "sync", mneg[0], C["mneg_f"], writes=["mneg0"])
    dm(p, "sync", mneg[1], C["mneg_b"], writes=["mneg1"])
    wqk = ar.bf(16 * 128).rearrange("p (a b) -> p a b", b=128)
    zqk = ar.f32(2050)
    ctmp = ar.f32(2048)
    wv = ar.bf(16 * 512).rearrange("p (a b) -> p a b", b=512)
    wg = ar.bf(16 * 16).rearrange("p (a b) -> p a b", b=16)
    ms(p, "pool", zqk[:, 0:1], 0.0, writes=["zqk"])
    ms(p, "pool", zqk[:, 2049:2050], 0.0, writes=["zqk"])
    for ch in range(4):
        load_w_fm(wqk, "wqk", w_in, OFF_MQ + ch * 128, 128)

        def evac(q, pap, pk):
            cp(p, "act", zqk[:, 1 + q * 512:1 + (q + 1) * 512], pap, reads=[pk], writes=["zqk"])
        proj_fm(p, ps, xT, wqk, "wqk", 128, evac)
        ts(p, "dve", ctmp, zqk[:, 0:2048], convw[:, ch:ch + 1], None, ALU.mult, reads=["zqk", "convw"], writes=["ctmp"])
        stt(p, ctmp, zqk[:, 1:2049], convw[:, 4 + ch:5 + ch], ctmp, ALU.mult, ALU.add, reads=["zqk", "convw", "ctmp"], writes=["ctmp"])
        stt(p, ctmp, zqk[:, 2:2050], convw[:, 8 + ch:9 + ch], ctmp, ALU.mult, ALU.add, reads=["zqk", "convw", "ctmp"], writes=["ctmp"])
        act(p, qkT[:, ch, :], ctmp, AF.Silu, reads=["ctmp"], writes=[("qkT", ch)])
        if ch < 2:
            ts(p, "pool", qkT[:, ch, :], qkT[:, ch, :], 0.125, None, ALU.mult, reads=[("qkT", ch)], writes=[("qkT", ch)])
    for tile in range(NT):
        b = 2 + tile % 2
        pk = "ps%d" % b
        for kc in range(2):
            trp(p, psb(b)[:, kc * 128:(kc + 1) * 128], qkT[:, 2 + kc, tile * 128:(tile + 1) * 128], identb[:],
                reads=[("qkT", 2 + kc), "identb"], writes=[pk])
        cp(p, "dve", ktok[:, tile, :], psb(b)[:, 0:256], reads=[pk], writes=["ktok"])
    load_w_fm(wv, "wv", w_in, OFF_MV, 512)
    load_w_fm(wg, "wg", w_in, OFF_MG, 16)
    ms(p, "pool", vaug[:, :, :, 128:129], 1.0, writes=["vaug"])
    for tile in range(NT):
        b = 4 + tile % 2
        pap, pk = proj_tm(p, ps, xT, wv, "wv", 512, tile, b)
        cp(p, "act", vaug[:, tile, :, 0:128], pap.rearrange("p (h v) -> p h v", v=128), reads=[pk], writes=["vaug"])
        b2 = 6 + tile % 2
        pap2, pk2 = proj_tm(p, ps, xT, wg, "wg", 16, tile, b2)
        tt_(p, "dve", gtok[:, tile, :], pap2, gb, ALU.add, reads=[pk2, "gb"], writes=["gtok"])
    gv = gtok.rearrange("p a (d y h) -> p d a y h", d=2, y=2, h=4)

    def g4(t):
        return t.rearrange("p (d a h) -> p d a h", d=2, h=4)
    cp(p, "dve", g4(G["ig"]), gv[:, :, :, 0, :], reads=["gtok"], writes=["ig"])
    act(p, g4(G["tmp"]), gv[:, :, :, 1, :], AF.Exp, reads=["gtok"], writes=["gtmp"], scale=-1.0)
    act(p, G["tmp"], G["tmp"], AF.Ln, reads=["gtmp"], writes=["gtmp"], bias=1.0)
    ts(p, "dve", G["lf"], G["tmp"], -1.0, None, ALU.mult, reads=["gtmp"], writes=["lf"])
    for d in range(2):
        mm(p, ps[0][:, d * 64:(d + 1) * 64], tri[d], G["lf"][:, d * 64:(d + 1) * 64], True, True,
           reads=["tri%d" % d, "lf"], writes=["ps0"])
    cp(p, "dve", G["b"], ps[0][:, 0:128], reads=["ps0"], writes=["gb_"])
    mm(p, ps[1][:, 0:128], onesf[:], G["lf"], True, True, reads=["onesf", "lf"], writes=["ps1"])
    cp(p, "dve", G["tot"], ps[1][:, 0:128], reads=["ps1"], writes=["tot"])
    tt_(p, "dve", G["imb"], G["ig"], G["b"], ALU.subtract, reads=["ig", "gb_"], writes=["imb"])
    tt_(p, "dve", G["tmp"], G["tot"], G["imb"], ALU.add, reads=["tot", "imb", "gtmp"], writes=["gtmp"])
    act(p, G["wk"], G["tmp"], AF.Exp, reads=["gtmp"], writes=["wk"])
    act(p, G["dec"], G["tot"], AF.Exp, reads=["tot"], writes=["dec"])
    act(p, G["ebt"], G["b"], AF.Exp, reads=["gb_"], writes=["ebt"])

    if STOP[0] == "ml1":
        return
    p.barrier()
    ar.off = mark
    hsum = ar.f32(NT * 512).rearrange("p (a h v) -> p a h v", h=4, v=128)
    mark2 = ar.off
    CT = ar.f32(8 * 129).rearrange("p (u v) -> p u v", v=129)
    CTb = ar.bf(8 * 130).rearrange("p (u v) -> p u v", v=130)
    NS = 2
    Rt = [ar.f32(128) for _ in range(NS)]
    AT = [ar.f32(128) for _ in range(NS)]
    ST = [ar.bf(128) for _ in range(NS)]
    nsb = [ar.f32(132) for _ in range(NS)]
    ddt = [ar.f32(4) for _ in range(NS)]
    kw = [[ar.bf(128) for _ in range(2)] for _ in range(NS)]
    for sidx in range(NS):
        for hh in range(2):
            ms(p, "pool", kw[sidx][hh], 0.0, writes=[("kw", sidx, hh)])
    unit = 0
    import os
    for d in range(2):
        order = list(range(NT)) if d == 0 else list(range(NT - 1, -1, -1))
        for ci, c in enumerate(order):
            for h in range(4):
                if unit >= int(os.environ.get("MLU", "100000")):
                    continue
                sl = unit % NS
                unit += 1
                b0, b1, b2 = 4 * sl, 4 * sl + 1, 4 * sl + 2
                k0, k1, k2 = "ps%d" % b0, "ps%d" % b1, "ps%d" % b2
                col = d * 64 + c * 4 + h
                hh, pc = h % 2, h // 2
                rows = slice(hh * 64, (hh + 1) * 64)
                u = d * 4 + h
                tsl = slice(c * 128, (c + 1) * 128)
                ts(p, "pool", Rt[sl], tri[d], G["lf"][:, col:col + 1], None, ALU.mult, reads=["tri%d" % d, "lf"], writes=[("Rt", sl)])
                mm(p, ps[b0][:, 0:128], onesf[:], Rt[sl], True, False, reads=["onesf", ("Rt", sl)], writes=[k0])
                mm(p, ps[b0][:, 0:128], identf[:], mneg[d], False, True, reads=["identf", "mneg%d" % d], writes=[k0])
                act(p, AT[sl], ps[b0][:, 0:128], AF.Exp, reads=[k0, "imb"], writes=[("AT", sl)], bias=G["imb"][:, col:col + 1])
                mm(p, ps[b1][:, 0:128], qkT[rows, 2 + pc, tsl], qkT[rows, pc, tsl], True, True,
                   reads=[("qkT", 2 + pc), ("qkT", pc)], writes=[k1])
                tt_(p, "dve", ST[sl], ps[b1][:, 0:128], AT[sl], ALU.mult, reads=[k1, ("AT", sl)], writes=[("ST", sl)])
                mm(p, ps[b2][:, 0:129], ST[sl], vaug[:, c, h, :], True, True, reads=[("ST", sl), "vaug"], writes=[k2])
                cp(p, "act", nsb[sl][:, 0:129], ps[b2][:, 0:129], reads=[k2], writes=[("nsb", sl)])
                if ci > 0:
                    mm(p, ps[b0][:, 256:385], qkT[rows, pc, tsl], CTb[rows, u, 0:129], True, True,
                       reads=[("qkT", pc), ("CTb", u)], writes=[k0])
                    stt(p, nsb[sl][:, 0:129], ps[b0][:, 256:385], G["ebt"][:, col:col + 1], nsb[sl][:, 0:129], ALU.mult, ALU.add,
                        reads=[k0, "ebt", ("nsb", sl)], writes=[("nsb", sl)])
                dd = ddt[sl]
                stt(p, dd[:, 0:1], nsb[sl][:, 128:129], -1.0, nsb[sl][:, 128:129], ALU.mult, ALU.max, reads=[("nsb", sl)], writes=[("dd", sl)])
                ts(p, "dve", dd[:, 1:2], dd[:, 0:1], 1.0, None, ALU.max, reads=[("dd", sl)], writes=[("dd", sl)])
                p.op("dve", lambda e, dd=dd: e.reciprocal(out=dd[:, 2:3], in_=dd[:, 1:2]), reads=[("dd", sl)], writes=[("dd", sl)])
                if d == 0:
                    ts(p, "dve", hsum[:, c, h, :], nsb[sl][:, 0:128], dd[:, 2:3], None, ALU.mult,
                       reads=[("nsb", sl), ("dd", sl)], writes=[("hsum", c, h)])
                else:
                    stt(p, hsum[:, c, h, :], nsb[sl][:, 0:128], dd[:, 2:3], hsum[:, c, h, :], ALU.mult, ALU.add,
                        reads=[("nsb", sl), ("dd", sl), ("hsum", c, h)], writes=[("hsum", c, h)])
                if ci < NT - 1:
                    ts(p, "pool", kw[sl][hh][:, rows], ktok[:, c, h * 64:(h + 1) * 64], G["wk"][:, col:col + 1], None, ALU.mult,
                       reads=["ktok", "wk"], writes=[("kw", sl, hh)])
                    mm(p, ps[b1][:, 256:385], kw[sl][hh], vaug[:, c, h, :], True, True, reads=[("kw", sl, hh), "vaug"], writes=[k1])
                    if ci == 0:
                        cp(p, "dve", CT[rows, u, :], ps[b1][rows, 256:385], reads=[k1], writes=[("CT", u)])
                    else:
                        stt(p, CT[rows, u, :], CT[rows, u, :], G["dec"][rows, col:col + 1], ps[b1][rows, 256:385], ALU.mult, ALU.add,
                            reads=[k1, "dec", ("CT", u)], writes=[("CT", u)])
                    cp(p, "act", CTb[rows, u, 0:129], CT[rows, u, :], reads=[("CT", u)], writes=[("CTb", u)])
    if STOP[0] == "ml2":
        return
    p.barrier()
    ar.off = mark2
    wo = ar.bf(16 * 512).rearrange("p (a b) -> p a b", b=512)
    yT = ar.bf(4 * S).rearrange("p (c t) -> p c t", t=S)
    osig = [ar.f32(512) for _ in range(2)]
    hn = [ar.f32(512) for _ in range(2)]
    ybf = [ar.bf(512) for _ in range(2)]
    stats = ar.f32(24)
    mv = ar.f32(16)
    load_w_fm(wo, "wo", w_in, OFF_MO, 512)
    for tile in range(NT):
        s2 = tile % 2
        pap, pk = proj_tm(p, ps, xT, wo, "wo", 512, tile, s2)
        act(p, osig[s2], pap, AF.Sigmoid, reads=[pk], writes=[("osig", s2)])
        for h in range(4):
            layer_norm_rows(hsum[:, tile, h, :], ("hsum", tile, h), hn[s2][:, h * 128:(h + 1) * 128], ("hn", s2), 128,
                            None, None, None, stats[:, h * 6:(h + 1) * 6], mv[:, h * 4:(h + 1) * 4], "mlln%d" % h)
        tt_(p, "pool", hn[s2], hn[s2], normw, ALU.mult, reads=[("hn", s2), "normw"], writes=[("hn", s2)])
        tt_(p, "pool", ybf[s2], hn[s2], osig[s2], ALU.mult, reads=[("hn", s2), ("osig", s2)], writes=[("ybf", s2)])
        b = 2 + s2
        pk2 = "ps%d" % b
        for c4 in range(4):
            trp(p, psb(b)[:, c4 * 128:(c4 + 1) * 128], ybf[s2][:, c4 * 128:(c4 + 1) * 128], identb[:],
                reads=[("ybf", s2), "identb"], writes=[pk2])
        cp(p, "act", yT[:, :, tile * 128:(tile + 1) * 128], psb(b)[:, 0:512].rearrange("p (c t) -> p c t", t=128),
           reads=[pk2], writes=["yT"])
    for c4 in range(4):
        dm(p, "sync", YST[0, c4], yT[:, c4, :], reads=["yT"], writes=["YST0"])


def stage_attn(nc, p, ar, ps, psb, xT, XT_ALL, identb, wl, C, YST, load_w_fm):
    p.barrier()
    ar.reset()
    w_in = wl["w_in"]
    dist = ar.f32(25 * 128).rearrange("p (a b) -> p a b", b=128)
    dm(p, "sync", dist, C["dist"], writes=["dist"])
    qT = ar.bf(3 * S).rearrange("p (g t) -> p g t", t=S)
    kT = ar.bf(3 * S).rearrange("p (g t) -> p g t", t=S)
    vat = ar.bf(NT * 3 * 2 * 65).rearrange("p (a g h v) -> p a g h v", g=3, h=2, v=65)
    wch = [ar.bf(16 * 128).rearrange("p (a b) -> p a b", b=128) for _ in range(2)]
    wv3 = ar.bf(16 * 384).rearrange("p (a b) -> p a b", b=384)
    yT = ar.bf(S)
    LAG = 3
    NL = LAG + 2
    Lt = [ar.f32(512) for _ in range(NL)]
    Pt = [ar.bf(512) for _ in range(NL)]
    ytile = [ar.bf(128) for _ in range(2)]
    rd = [ar.f32(2) for _ in range(2)]
    base = (0, 3, 8)
    ms(p, "pool", vat[:, :, :, :, 64:65], 1.0, writes=["vat"])
    wi = 0
    for hp in range(4):
        for g in range(3):
            for which, dstT, off in (("q", qT, OFF_AQ), ("k", kT, OFF_AK)):
                wb = wch[wi % 2]
                wk_ = ("wch", wi % 2)
                wi += 1
                load_w_fm(wb, wk_, w_in, off + (g * 4 + hp) * 128, 128)

                def evac(q, pap, pk, dstT=dstT, g=g, which=which):
                    cp(p, "act", dstT[:, g, q * 512:(q + 1) * 512], pap, reads=[pk], writes=[(which + "T", g)])
                proj_fm(p, ps, xT, wb, wk_, 128, evac)
            p.dma("pool", lambda e, g=g, hp=hp: e.dma_start(
                out=wv3[:, :, g * 128:(g + 1) * 128],
                in_=w_in[:, OFF_AV + (g * 8 + 2 * hp) * 64:OFF_AV + (g * 8 + 2 * hp) * 64 + 128].rearrange("(dc q) c -> q dc c", q=128)),
                writes=["wv3"])
        for tile in range(NT):
            b = 2 + tile % 2
            pap, pk = proj_tm(p, ps, xT, wv3, "wv3", 384, tile, b)
            cp(p, "dve", vat[:, tile, :, :, 0:64], pap.rearrange("p (g h v) -> p g h v", g=3, h=2), reads=[pk], writes=["vat"])
        batches = []
        for qt in range(NT):
            for h2 in range(2):
                units = []
                for g in range(3):
                    kbs = list(range(max(0, qt - ATT_R[g]), min(NT - 1, qt + ATT_R[g]) + 1))
                    for i0 in range(0, len(kbs), 4):
                        units.append((g, kbs[i0:i0 + 4]))
                for ui, (g, kbs) in enumerate(units):
                    batches.append((qt, h2, g, kbs, ui == 0, ui == len(units) - 1))

        def front(i):
            qt, h2, g, kbs, first, last = batches[i]
            qs = slice(qt * 128, (qt + 1) * 128)
            head = 2 * hp + h2
            rows = slice(h2 * 64, (h2 + 1) * 64)
            n = len(kbs)
            sb_ = i % 4
            sk = "ps%d" % sb_
            sl = i % NL
            for j, kb in enumerate(kbs):
                mm(p, ps[sb_][:, j * 128:(j + 1) * 128], kT[rows, g, kb * 128:(kb + 1) * 128], qT[rows, g, qs], True, True,
                   reads=[("kT", g), ("qT", g)], writes=[sk])
            idx0 = base[g] + (kbs[0] - qt) + ATT_R[g]
            stt(p, Lt[sl][:, 0:n * 128], dist[:, idx0:idx0 + n, :].rearrange("p a b -> p (a b)"),
                -8.0 * alibi_slope(g, head), ps[sb_][:, 0:n * 128], ALU.mult, ALU.add,
                reads=["dist", sk], writes=[("Lt", sl)])
            act(p, Pt[sl][:, 0:n * 128], Lt[sl][:, 0:n * 128], AF.Exp, reads=[("Lt", sl)], writes=[("Pt", sl)], scale=0.125)

        def back(i):
            qt, h2, g, kbs, first, last = batches[i]
            qs = slice(qt * 128, (qt + 1) * 128)
            sl = i % NL
            ab = 6 + h2
            ak = "ps%d" % ab
            n = len(kbs)
            for j, kb in enumerate(kbs):
                mm(p, ps[ab][:, 0:65], Pt[sl][:, j * 128:(j + 1) * 128], vat[:, kb, g, h2, :], first and j == 0, last and j == n - 1,
                   reads=[("Pt", sl), "vat"], writes=[ak])
            if last:
                p.op("dve", lambda e, ab=ab, h2=h2: e.reciprocal(out=rd[h2][:, 0:1], in_=ps[ab][:, 64:65]), reads=[ak], writes=[("rd", h2)])
                ts(p, "dve", ytile[qt % 2][:, h2 * 64:(h2 + 1) * 64], ps[ab][:, 0:64], rd[h2][:, 0:1], None, ALU.mult,
                   reads=[ak, ("rd", h2)], writes=[("ytile", qt % 2)])
                if h2 == 1:
                    tb = 4 + qt % 2
                    trp(p, psb(tb)[:, 0:128], ytile[qt % 2], identb[:], reads=[("ytile", qt % 2), "identb"], writes=["ps%d" % tb])
                    cp(p, "act", yT[:, qs], psb(tb)[:, 0:128], reads=["ps%d" % tb], writes=["yTa"])

        nb_ = len(batches)
        for i in range(nb_ + LAG):
            if i < nb_:
                front(i)
            if i >= LAG:
                back(i - LAG)
        dm(p, "sync", YST[1, hp], yT, reads=["yTa"], writes=["YST1"])


def stage_sg(nc, p, ar, ps, psb, xT, XT_ALL, identb, identf, wl, C, YST, load_w_fm, bcast_load, layer_norm_rows):
    p.barrier()
    ar.reset()
    w_in = wl["w_in"]
    wu = ar.bf(16 * 512).rearrange("p (a b) -> p a b", b=512)
    wv = ar.bf(16 * 512).rearrange("p (a b) -> p a b", b=512)
    lng = ar.f32(512)
    lnb = ar.f32(512)
    wsf = ar.f32(512).rearrange("p (g s) -> p g s", s=128)
    wsT = ar.bf(512).rearrange("p (g t) -> p g t", t=128)
    bs = ar.f32(4)
    yT = ar.bf(4 * S).rearrange("p (c t) -> p c t", t=S)
    u = [ar.f32(512) for _ in range(2)]
    v = [ar.f32(512) for _ in range(2)]
    vn = [ar.bf(512) for _ in range(2)]
    ysg = [ar.bf(512) for _ in range(2)]
    stats = ar.f32(8)
    mv = ar.f32(4)
    load_w_fm(wu, "wu", w_in, OFF_SU, 512)
    load_w_fm(wv, "wv", w_in, OFF_SV, 512)
    bcast_load(lng, "lng", wl["sg_ln_g"], 512)
    bcast_load(lnb, "lnb", wl["sg_ln_b"], 512)
    dm(p, "sync", wsf, wl["sg_w"].rearrange("g t s -> t g s"), writes=["wsf"])
    dm(p, "sync", bs, wl["sg_b"].rearrange("g t -> t g"), writes=["bs"], allow_slow_non_contiguous=True)
    for g in range(4):
        trp(p, ps[7][:, g * 128:(g + 1) * 128], wsf[:, g, :], identf[:], reads=["wsf", "identf"], writes=["ps7"])
    cp(p, "dve", wsT.rearrange("p g t -> p (g t)"), ps[7][:, 0:512], reads=["ps7"], writes=["wsT"])
    for tile in range(NT):
        s2 = tile % 2
        pap, pk = proj_tm(p, ps, xT, wu, "wu", 512, tile, 0 + s2)
        act(p, u[s2], pap, AF.Gelu, reads=[pk], writes=[("u", s2)])
        pap, pk = proj_tm(p, ps, xT, wv, "wv", 512, tile, 2 + s2)
        act(p, v[s2], pap, AF.Gelu, reads=[pk], writes=[("v", s2)])
        layer_norm_rows(v[s2], ("v", s2), v[s2], ("v", s2), 512, lng, lnb, ("lng", "lnb"), stats, mv, "sgln")
        cp(p, "pool", vn[s2], v[s2], reads=[("v", s2)], writes=[("vn", s2)])
        b = 4 + s2
        pk = "ps%d" % b
        for g in range(4):
            mm(p, ps[b][:, g * 128:(g + 1) * 128], wsT[:, g, :], vn[s2][:, g * 128:(g + 1) * 128], True, True,
               reads=["wsT", ("vn", s2)], writes=[pk])
        for g in range(4):
            stt(p, ysg[s2][:, g * 128:(g + 1) * 128], ps[b][:, g * 128:(g + 1) * 128], bs[:, g:g + 1], u[s2][:, g * 128:(g + 1) * 128],
                ALU.add, ALU.mult, reads=[pk, "bs", ("u", s2)], writes=[("ysg", s2)])
        b2 = 6
        for c4 in range(4):
            trp(p, psb(b2)[:, c4 * 128:(c4 + 1) * 128], ysg[s2][:, c4 * 128:(c4 + 1) * 128], identb[:], reads=[("ysg", s2), "identb"], writes=["ps6"])
        cp(p, "act", yT[:, :, tile * 128:(tile + 1) * 128], psb(b2)[:, 0:512].rearrange("p (c t) -> p c t", t=128), reads=["ps6"], writes=["yT"])
    for c4 in range(4):
        dm(p, "sync", YST[2, c4], yT[:, c4, :], reads=["yT"], writes=["YST2"])


def stage_pool(nc, p, ar, ps, psb, xT, XT_ALL, wl, C, YST, load_w_fm):
    p.barrier()
    ar.reset()
    w_in = wl["w_in"]
    PADW = S + 32
    wch = [ar.bf(16 * 128).rearrange("p (a b) -> p a b", b=128) for _ in range(2)]
    wp = ar.bf(512).rearrange("p (g d) -> p g d", d=128)
    psc = ar.f32(4)
    edge = ar.f32(64).rearrange("p (g s j) -> p g s j", s=2, j=8)
    pp = ar.f32(PADW)
    A = [ar.f32(PADW) for _ in range(2)]
    dT = ar.bf(S)
    yT = ar.bf(S)
    p.dma("pool", lambda e: e.dma_start(out=wp, in_=wl["pool_w"].rearrange("g c d -> c g d")), writes=["wp"])
    dm(p, "sync", psc, wl["pool_scale"].rearrange("(g q) -> q g", q=128), writes=["psc"], allow_slow_non_contiguous=True)
    dm(p, "sync", edge.rearrange("p g s j -> p (g s j)"), C["pool_edge"], writes=["edge"])
    ms(p, "pool", pp, 0.0, writes=["pp"])
    ms(p, "pool", A[0], 0.0, writes=["A0"])
    ms(p, "pool", A[1], 0.0, writes=["A1"])
    O = 16
    E0, EN = O - 8, S + 16
    for g in range(4):
        wb = wch[g % 2]
        wk_ = ("wch", g % 2)
        load_w_fm(wb, wk_, w_in, OFF_PP + g * 128, 128)

        def evac(q, pap, pk):
            cp(p, "act", pp[:, O + q * 512:O + (q + 1) * 512], pap, reads=[pk], writes=["pp"])
        proj_fm(p, ps, xT, wb, wk_, 128, evac)
        tt_(p, "pool", A[0][:, E0:E0 + EN], pp[:, E0 - 1:E0 - 1 + EN], pp[:, E0:E0 + EN], ALU.add, reads=["pp", "A0"], writes=["A0"])
        cur = 0
        sh = 1
        for step in range(g):
            nxt = 1 - cur
            tt_(p, "pool", A[nxt][:, E0:E0 + EN], A[cur][:, E0 - sh:E0 - sh + EN], A[cur][:, E0 + sh:E0 + sh + EN], ALU.add,
                reads=["A%d" % cur, "A%d" % nxt], writes=["A%d" % nxt])
            cur = nxt
            sh *= 2
        w = (2, 4, 8, 16)[g]
        hw = w // 2
        stt(p, dT[:, :], A[cur][:, O:O + S], 1.0 / w, pp[:, O:O + S], ALU.mult, ALU.subtract, reads=["A%d" % cur, "pp"], writes=["dT"])
        for side, c0 in ((0, 0), (1, S - hw)):
            tt_(p, "dve", A[cur][:, O + c0:O + c0 + hw], A[cur][:, O + c0:O + c0 + hw], edge[:, g, side, 0:hw], ALU.mult,
                reads=["A%d" % cur, "edge", "dT"], writes=["A%d" % cur])
            tt_(p, "dve", dT[:, c0:c0 + hw], A[cur][:, O + c0:O + c0 + hw], pp[:, O + c0:O + c0 + hw], ALU.subtract,
                reads=["A%d" % cur, "pp"], writes=["dT"])
        for q in range(4):
            b = 2 + q % 2
            pk = "ps%d" % b
            mm(p, ps[b][:, :], wp[:, g, :], dT[:, q * 512:(q + 1) * 512], True, True, reads=["wp", "dT"], writes=[pk])
            act(p, yT[:, q * 512:(q + 1) * 512], ps[b][:, :], AF.Identity, reads=[pk, "psc"], writes=["yT"], scale=psc[:, g:g + 1])
        dm(p, "sync", YST[3, g], yT, reads=["yT"], writes=["YST3"])


def stage_merge(nc, p, ar, ps, psb, xT, XT_ALL, identf, wl, C, YST, MT, cur_x, XA, load_w_fm, bcast_load,
                layer_norm_rows, tile_to_xT, logits=None):
    p.barrier()
    ar.reset()
    yst = ar.bf(16 * S).rearrange("p (n k t) -> p n k t", n=4, k=4)
    wg = [ar.bf(16 * 4 * 128).rearrange("p (k n c) -> p k n c", n=4, c=128) for _ in range(2)]
    wb = [ar.bf(4 * 4 * 128).rearrange("p (n k c) -> p n k c", k=4, c=128) for _ in range(2)]
    bg = ar.f32(64)
    gsb = [ar.f32(512) for _ in range(2)]
    macc = [ar.f32(512) for _ in range(2)]
    tmpm = [ar.f32(512) for _ in range(2)]
    mTb = [ar.bf(512) for _ in range(2)]
    for n in range(4):
        for k in range(4):
            dm(p, "sync", yst[:, n, k, :], YST[n, k], writes=["yst"])
    bgr = ar.f32(128)
    dm(p, "sync", bgr[0:64, :], wl["b_gate"].rearrange("(c q) -> c q", q=128), writes=["bgr"])
    trp(p, ps[7][:, 0:64], bgr[0:64, :], identf[0:64, 0:64], reads=["bgr", "identf"], writes=["ps7"])
    cp(p, "dve", bg, ps[7][:, 0:64], reads=["ps7"], writes=["bg"])
    w_gate, w_branch = wl["w_gate"], wl["w_branch"]
    it = 0
    for dc in range(16):
        b = dc % 2
        for n in range(4):
            p.dma("pool", lambda e, n=n, dc=dc, b=b: e.dma_start(
                out=wg[b][:, :, n, :], in_=w_gate[:, n * D + dc * 128:n * D + (dc + 1) * 128].rearrange("(k q) c -> q k c", q=128)),
                writes=[("wg", b)])
            p.dma("pool", lambda e, n=n, dc=dc, b=b: e.dma_start(
                out=wb[b][:, n, :, :], in_=w_branch[n, :, dc * 128:(dc + 1) * 128].rearrange("(k q) c -> q k c", q=128)),
                writes=[("wb", b)])
        for q in range(4):
            tq = slice(q * 512, (q + 1) * 512)
            i2 = it % 2
            it += 1
            for n in range(4):
                ga = n % 2
                gk = "ps%d" % ga
                for k in range(16):
                    mm(p, ps[ga][:, :], wg[b][:, k, n, :], xT[:, k, tq], k == 0, k == 15, reads=[("wg", b)] + xt_keys(q * 4, 4), writes=[gk])
                act(p, gsb[n % 2], ps[ga][:, :], AF.Sigmoid, reads=[gk, "bg"], writes=[("gsb", n % 2)], bias=bg[:, n * 16 + dc:n * 16 + dc + 1])
                pa = 2 + n % 2
                pk = "ps%d" % pa
                for k in range(4):
                    mm(p, ps[pa][:, :], wb[b][:, n, k, :], yst[:, n, k, tq], k == 0, k == 3, reads=[("wb", b), "yst"], writes=[pk])
                if n == 0:
                    tt_(p, "dve", macc[i2], ps[pa][:, :], gsb[n % 2], ALU.mult, reads=[pk, ("gsb", n % 2)], writes=[("macc", i2)])
                else:
                    tt_(p, "dve", tmpm[n % 2], ps[pa][:, :], gsb[n % 2], ALU.mult, reads=[pk, ("gsb", n % 2)], writes=[("tmpm", n % 2)])
                    if n < 3:
                        tt_(p, "pool", macc[i2], macc[i2], tmpm[n % 2], ALU.add, reads=[("macc", i2), ("tmpm", n % 2)], writes=[("macc", i2)])
                    else:
                        tt_(p, "pool", mTb[i2], macc[i2], tmpm[n % 2], ALU.add, reads=[("macc", i2), ("tmpm", n % 2)], writes=[("mTb", i2)])
            dm(p, "sync", MT[dc, :, tq], mTb[i2], reads=[("mTb", i2)], writes=["MT"])

    if STOP[0] == "mg1":
        return
    p.barrier()
    ar.reset()
    wout = ar.bf(16 * D).rearrange("p (k c) -> p k c", c=D)
    lng = ar.f32(D)
    lnb = ar.f32(D)
    mt = [ar.bf(16 * 128).rearrange("p (k t) -> p k t", t=128) for _ in range(2)]
    xres = [ar.f32(D) for _ in range(2)]
    sres = xres
    stats = ar.f32(24)
    mv = ar.f32(4)
    wr = ar.f32(16 * 36).rearrange("p (k c) -> p k c", c=36)
    whb = ar.bf(16 * 36).rearrange("p (k c) -> p k c", c=36)
    wlb = ar.bf(16 * 36).rearrange("p (k c) -> p k c", c=36)
    wtmp = ar.f32(16 * 36).rearrange("p (k c) -> p k c", c=36)
    brb = ar.f32(36)
    hb = [ar.bf(D) for _ in range(2)]
    lob = ar.bf(D)
    loT = ar.bf(16 * 128).rearrange("p (k t) -> p k t", t=128)
    for oc in range(4):
        p.dma("pool", lambda e, oc=oc: e.dma_start(out=wout[:, :, oc * 512:(oc + 1) * 512],
                                                   in_=wl["w_out"][:, oc * 512:(oc + 1) * 512].rearrange("(k q) c -> q k c", q=128)),
              writes=["wout"])
    bcast_load(lng, "lng", wl["ln1_g"], D)
    bcast_load(lnb, "lnb", wl["ln1_b"], D)
    dm(p, "sync", wr.rearrange("p k c -> p (k c)"), wl["w_router"], writes=["wr"])
    bcast_load(brb, "brb", wl["b_router"], 36)
    cp(p, "dve", whb, wr, reads=["wr"], writes=["whb"])
    tt_(p, "dve", wtmp, wr, whb, ALU.subtract, reads=["wr", "whb"], writes=["wtmp"])
    cp(p, "dve", wlb, wtmp, reads=["wtmp"], writes=["wlb"])
    P2 = int(os.environ.get("P2STOP", "99"))
    for tile in range(NT):
        b = tile % 2
        rs = slice(tile * 128, (tile + 1) * 128)
        for k in range(16):
            dm(p, "sync", mt[b][:, k, :], MT[k, :, rs], reads=["MT"], writes=[("mt", b)])
        dm(p, "sync", xres[b], cur_x[rs, :], writes=[("sres", b)])
        for oc in range(4):
            ok = "ps%d" % oc
            for k in range(16):
                mm(p, ps[oc][:, :], mt[b][:, k, :], wout[:, k, oc * 512:(oc + 1) * 512], k == 0, k == 15, reads=[("mt", b), "wout"], writes=[ok])
            stt(p, sres[b][:, oc * 512:(oc + 1) * 512], xres[b][:, oc * 512:(oc + 1) * 512], ALPHA, ps[oc][:, :], ALU.mult, ALU.add,
                reads=[ok, ("sres", b)], writes=[("sres", b)])
        layer_norm_rows(sres[b], ("sres", b), sres[b], ("sres", b), D, lng, lnb, ("lng", "lnb"), stats, mv, "ln1")
        dm(p, "sync", XA[rs, :], sres[b], reads=[("sres", b)], writes=["XA"])
        if P2 <= 4:
            continue
        tsl = slice(tile * 128, (tile + 1) * 128)
        tile_to_xT(sres[b], ("sres", b), tile, 4, hb[b], ("hb", b), lo=((lob, "lob", loT, "loT") if logits is not None else None))
        if P2 <= 5:
            continue
        if logits is not None:
            n = 0
            for k in range(16):
                for a_, ak_, w_, wk_ in ((xT[:, k, tsl], ("xT", tile), whb, "whb"), (xT[:, k, tsl], ("xT", tile), wlb, "wlb"),
                                         (loT[:, k, :], "loT", whb, "whb")):
                    mm(p, ps[6][:, 0:36], a_, w_[:, k, :], n == 0, n == 47, reads=[ak_, wk_], writes=["ps6"])
                    n += 1
            tt_(p, "dve", logits[:, tile * 36:(tile + 1) * 36], ps[6][:, 0:36], brb, ALU.add, reads=["ps6", "brb"], writes=["logits"])


def make_in_map(x_b, weights, consts):
    m = {"x": np.ascontiguousarray(x_b, dtype=np.float32)}
    weights = dict(weights)
    wr = np.concatenate([np.asarray(weights["w_router_group"]), np.asarray(weights["w_router_expert"])], axis=-1)
    L_ = wr.shape[0]
    weights["w_router"] = wr.reshape(L_, 16, 128, 36).transpose(0, 2, 1, 3)
    weights["b_router"] = np.concatenate([np.asarray(weights["b_router_group"]), np.asarray(weights["b_router_expert"])], axis=-1)
    for k, shp in WEIGHT_SHAPES.items():
        w = np.asarray(weights[k], dtype=np.float32)
        m[k] = np.ascontiguousarray(w.reshape([w.shape[0]] + shp))
    for k, v in consts.items():
        m["c_" + k] = np.ascontiguousarray(v.reshape(CONST_SHAPES[k]))
    return m


def bc(ap, axis, shape):
    return ap.unsqueeze(axis).to_broadcast(shape)


def red(p, eng, out, in_, op, axis, reads=(), writes=()):
    return p.op(eng, lambda e: e.tensor_reduce(out=out, in_=in_, axis=axis, op=op), reads, writes)


def stage_moe(nc, p, ar, ps, psb, xT, XT_ALL, identb, identf, onesf, wl, C, XA, XS, YB, dst, bcast_load,
              layer_norm_rows, tile_to_xT, logits, first, last):
    p.barrier()
    ar.reset()
    IOA = bass.IndirectOffsetOnAxis
    desti = ar.i32(32)
    idxW = ar.i32(NBLK * 4).rearrange("p (b q) -> p b q", q=4)
    idxW2 = ar.i32(NBLK * 4).rearrange("p (b q) -> p b q", q=4)
    wts = ar.f32(32).rearrange("p (a k) -> p a k", k=2)
    mark = ar.off
    L3 = logits.rearrange("p (a c) -> p a c", c=36)
    lg = L3[:, :, 0:4]
    le = L3[:, :, 4:36]
    mx = ar.f32(16)
    ohg = ar.f32(64).rearrange("p (a g) -> p a g", g=4)
    eg = ar.f32(64).rearrange("p (a g) -> p a g", g=4)
    se = ar.f32(16)
    psel = ar.f32(16)
    pen = ar.f32(64).rearrange("p (a g) -> p a g", g=4)
    lem = ar.f32(512).rearrange("p (a g e) -> p a g e", g=4, e=8)
    lem2 = ar.f32(512).rearrange("p (a c) -> p a c", c=32)
    m1 = ar.f32(16)
    m2 = ar.f32(16)
    E = ar.f32(1024).rearrange("p (k a c) -> p k a c", k=2, c=32)
    tot = ar.f32(1024).rearrange("p (j c) -> p j c", c=32)
    off = ar.f32(1024).rearrange("p (j c) -> p j c", c=32)
    Sm = ar.f32(1024).rearrange("p (j c) -> p j c", c=32)
    cnt = ar.f32(32)
    cnti = ar.i32(32)
    padv = ar.f32(32)
    pst = ar.f32(33)
    pend = ar.f32(32)
    destf = ar.f32(32)
    thr = ar.f32(NBLK * 32).rearrange("p (b c) -> p b c", c=32)
    cmpv = ar.f32(NBLK * 32).rearrange("p (b c) -> p b c", c=32)
    bex = ar.f32(NBLK)
    iow = ar.f32(4)
    idxf = ar.f32(NBLK * 4).rearrange("p (b q) -> p b q", q=4)
    tris = ar.f32(128)
    e21 = ar.f32(16)
    rden = ar.f32(16)
    dm(p, "sync", thr.rearrange("p b c -> p (b c)"), C["thr"], writes=["thr"])
    dm(p, "sync", iow, C["iow"], writes=["iow"])
    dm(p, "sync", tris, C["tri_s"], writes=["tris"])
    lemf = lem.rearrange("p a g e -> p a (g e)")
    red(p, "dve", mx, lg, ALU.max, AX.X, reads=["logits"], writes=["mx"])
    tt_(p, "dve", ohg, lg, bc(mx, 2, [128, NT, 4]), ALU.is_equal, reads=["logits", "mx"], writes=["ohg"])
    tt_(p, "dve", eg, lg, bc(mx, 2, [128, NT, 4]), ALU.subtract, reads=["logits", "mx"], writes=["eg"])
    act(p, eg, eg, AF.Exp, reads=["eg"], writes=["eg"])
    red(p, "dve", se, eg, ALU.add, AX.X, reads=["eg"], writes=["se"])
    p.op("dve", lambda e: e.reciprocal(out=psel, in_=se), reads=["se"], writes=["psel"])
    ts(p, "dve", pen, ohg, -1.0, 1e30, ALU.add, ALU.mult, reads=["ohg"], writes=["pen"])
    tt_(p, "dve", lem, le.rearrange("p a (g e) -> p a g e", e=8), bc(pen, 3, [128, NT, 4, 8]), ALU.add, reads=["logits", "pen"], writes=["lem"])
    red(p, "dve", m1, lemf, ALU.max, AX.X, reads=["lem"], writes=["m1"])
    tt_(p, "dve", E[:, 0], lemf, bc(m1, 2, [128, NT, 32]), ALU.is_equal, reads=["lem", "m1"], writes=["E"])
    stt(p, lem2, E[:, 0], -1e30, lemf, ALU.mult, ALU.add, reads=["E", "lem"], writes=["lem2"])
    red(p, "dve", m2, lem2, ALU.max, AX.X, reads=["lem2"], writes=["m2"])
    tt_(p, "dve", E[:, 1], lem2, bc(m2, 2, [128, NT, 32]), ALU.is_equal, reads=["lem2", "m2", "E"], writes=["E"])
    tt_(p, "dve", e21, m2, m1, ALU.subtract, reads=["m1", "m2"], writes=["e21"])
    act(p, e21, e21, AF.Exp, reads=["e21"], writes=["e21"])
    ts(p, "dve", rden, e21, 1.0, None, ALU.add, reads=["e21"], writes=["rden"])
    p.op("dve", lambda e: e.reciprocal(out=rden, in_=rden), reads=["rden"], writes=["rden"])
    tt_(p, "dve", rden, rden, psel, ALU.mult, reads=["rden", "psel"], writes=["rden"])
    cp(p, "dve", wts[:, :, 0], rden, reads=["rden"], writes=["wts"])
    tt_(p, "dve", wts[:, :, 1], rden, e21, ALU.mult, reads=["rden", "e21", "wts"], writes=["wts"])
    Ef = E.rearrange("p k a c -> p (k a c)")
    for hf in range(2):
        mm(p, ps[hf][:, :], tris, Ef[:, hf * 512:(hf + 1) * 512], True, True, reads=["tris", "E"], writes=["ps%d" % hf])
        mm(p, ps[2 + hf][:, :], onesf[:], Ef[:, hf * 512:(hf + 1) * 512], True, True, reads=["onesf", "E"], writes=["ps%d" % (2 + hf)])
        cp(p, "dve", tot.rearrange("p j c -> p (j c)")[:, hf * 512:(hf + 1) * 512], ps[2 + hf][:, :], reads=["ps%d" % (2 + hf)], writes=["tot"])
    ms(p, "dve", off[:, 0, :], 0.0, writes=["off"])
    for j in range(31):
        tt_(p, "dve", off[:, j + 1, :], off[:, j, :], tot[:, j, :], ALU.add, reads=["off", "tot"], writes=["off"])
    tt_(p, "dve", cnt, off[:, 31, :], tot[:, 31, :], ALU.add, reads=["off", "tot"], writes=["cnt"])
    ts(p, "dve", cnt, cnt, float(BLKS - 1), None, ALU.add, reads=["cnt"], writes=["cnt"])
    cp(p, "dve", cnti, cnt, reads=["cnt"], writes=["cnti"])
    p.op("dve", lambda e: e.tensor_single_scalar(out=cnti, in_=cnti, scalar=8, op=ALU.arith_shift_right), reads=["cnti"], writes=["cnti"])
    cp(p, "dve", padv, cnti, reads=["cnti"], writes=["padv"])
    ts(p, "dve", padv, padv, float(BLKS), None, ALU.mult, reads=["padv"], writes=["padv"])
    ms(p, "dve", pst[:, 0:1], 0.0, writes=["pst"])
    for e_ in range(32):
        tt_(p, "dve", pst[:, e_ + 1:e_ + 2], pst[:, e_:e_ + 1], padv[:, e_:e_ + 1], ALU.add, reads=["pst", "padv"], writes=["pst"])
    cp(p, "dve", pend, pst[:, 1:33], reads=["pst"], writes=["pend"])
    tt_(p, "dve", Sm, off, bc(pst[:, 0:32], 1, [128, 32, 32]), ALU.add, reads=["off", "pst"], writes=["Sm"])
    Smf = Sm.rearrange("p j c -> p (j c)")
    for hf in range(2):
        tt_(p, "dve", Smf[:, hf * 512:(hf + 1) * 512], ps[hf][:, :], Smf[:, hf * 512:(hf + 1) * 512], ALU.add, reads=["ps%d" % hf, "Sm"], writes=["Sm"])
    tt_(p, "dve", Smf, Smf, Ef, ALU.mult, reads=["Sm", "E"], writes=["Sm"])
    red(p, "dve", destf, Sm, ALU.add, AX.X, reads=["Sm"], writes=["destf"])
    cp(p, "dve", desti, destf, reads=["destf"], writes=["desti"])
    tt_(p, "dve", cmpv, thr, bc(pend, 1, [128, NBLK, 32]), ALU.is_ge, reads=["thr", "pend"], writes=["cmpv"])
    red(p, "dve", bex, cmpv, ALU.add, AX.X, reads=["cmpv"], writes=["bex"])
    bigt = ar.f32(NBLK)
    ts(p, "dve", bigt, bex, 31.5, (1.0e6 if SKIP_UNUSED else 0.0), ALU.is_ge, ALU.mult, reads=["bex"], writes=["bigt"])
    ts(p, "dve", bex, bex, 31.0, None, ALU.min, reads=["bex", "bigt"], writes=["bex"])
    cp(p, "dve", idxf, bc(iow, 1, [128, NBLK, 4]), reads=["iow"], writes=["idxf"])
    stt(p, idxf, bc(bex, 2, [128, NBLK, 4]), 512.0, idxf, ALU.mult, ALU.add, reads=["bex", "idxf"], writes=["idxf"])
    ts(p, "dve", idxf, idxf, float(wl["_l"] * 16384), None, ALU.add, reads=["idxf"], writes=["idxf"])
    cp(p, "dve", idxW2, idxf, reads=["idxf"], writes=["idxW2"])
    tt_(p, "dve", idxf, idxf, bc(bigt, 2, [128, NBLK, 4]), ALU.add, reads=["bigt", "idxf", "idxW2"], writes=["idxf"])
    cp(p, "dve", idxW, idxf, reads=["idxf"], writes=["idxW"])

    p.barrier()
    ar.off = mark
    xb = [ar.bf(D) for _ in range(2)]
    if first:
        ms(p, "pool", xb[0], 0.0, writes=[("xb", 0)])
        for b in range(NSLOT // 128):
            dm(p, "sync", XS[b * 128:(b + 1) * 128, :], xb[0], reads=[("xb", 0)], writes=["XS"])
        p.barrier()
    for tile in range(NT):
        b = tile % 2
        p.dma("pool", lambda e, tile=tile, b=b: e.dma_start(out=xb[b], in_=XA[tile * 128:(tile + 1) * 128, :]), writes=[("xb", b)])
        for k in range(2):
            j = k * NT + tile
            p.dma("pool", lambda e, j=j, b=b: e.indirect_dma_start(
                out=XS[:, :], out_offset=IOA(ap=desti[:, j:j + 1], axis=0), in_=xb[b], in_offset=None),
                reads=[("xb", b), "desti"], writes=["XSs"])
    p.barrier()
    ar.off = mark
    w1b = [ar.bf(16 * 512).rearrange("p (j f) -> p j f", f=512) for _ in range(2)]
    w3b = [ar.bf(16 * 512).rearrange("p (j f) -> p j f", f=512) for _ in range(2)]
    w2b = ar.bf(4 * D).rearrange("p (j c) -> p j c", c=D)
    xs = [ar.bf(D) for _ in range(2)]
    XsT = [ar.bf(16 * 128).rearrange("p (j s) -> p j s", s=128) for _ in range(2)]
    hs = [ar.f32(512) for _ in range(2)]
    Hb = [ar.bf(512) for _ in range(2)]
    HT = [ar.bf(512).rearrange("p (j s) -> p j s", s=128) for _ in range(2)]
    ybs = [ar.f32(D) for _ in range(2)]
    W1 = wl["_W"]["w_exp_gate"].rearrange("l e (r a) f -> (l e r) (a f)", a=4)
    W3 = wl["_W"]["w_exp_up"].rearrange("l e (r a) f -> (l e r) (a f)", a=4)
    W2 = wl["_W"]["w_exp_down"].rearrange("l e f c -> (l e f) c")
    nrows = int(W1.shape[0])
    _rh = {}

    def bkw_(e):
        if not SKIP_UNUSED:
            return {}
        if "r" not in _rh:
            r = e.alloc_register("moe_bound_%d" % wl["_l"])
            e.reg_mov(r, nrows - 1)
            _rh["r"] = r
        return dict(bounds_check=_rh["r"], oob_is_err=False)
    for b in range(NBLK):
        s2 = b % 2
        for q in range(4):
            p.dma("pool", lambda e, b=b, q=q, s2=s2: e.indirect_dma_start(
                out=w1b[s2][:, 4 * q:4 * q + 4, :].rearrange("p j f -> p (j f)"), out_offset=None, in_=W1,
                in_offset=IOA(ap=idxW[:, b, q:q + 1], axis=0), **bkw_(e)), reads=["idxW"], writes=[("w1b", s2)])
            p.dma("pool", lambda e, b=b, q=q, s2=s2: e.indirect_dma_start(
                out=w3b[s2][:, 4 * q:4 * q + 4, :].rearrange("p j f -> p (j f)"), out_offset=None, in_=W3,
                in_offset=IOA(ap=idxW[:, b, q:q + 1], axis=0), **bkw_(e)), reads=["idxW"], writes=[("w3b", s2)])
        for q in range(4):
            p.dma("pool", lambda e, b=b, q=q: e.indirect_dma_start(
                out=w2b.rearrange("p j c -> p (j c)")[:, q * D:(q + 1) * D], out_offset=None, in_=W2,
                in_offset=IOA(ap=idxW2[:, b, q:q + 1], axis=0)), reads=["idxW2"], writes=["w2b"])
        for sub in range(2):
            r0 = b * BLKS + sub * 128
            dm(p, "sync", xs[sub], XS[r0:r0 + 128, :], reads=["XSs"], writes=[("xs", sub)])
            xv = xs[sub].rearrange("s (q j) -> s j q", j=16)
            for hf in range(2):
                for jj in range(8):
                    j = hf * 8 + jj
                    trp(p, psb(hf, 1024)[:, jj * 128:(jj + 1) * 128], xv[:, j, :], identb[:], reads=[("xs", sub), "identb"], writes=["ps%d" % hf])
                cp(p, "act" if hf == 0 else "dve", XsT[sub][:, hf * 8:(hf + 1) * 8, :].rearrange("p j s -> p (j s)"), psb(hf, 1024),
                   reads=["ps%d" % hf], writes=[("XsT", sub)])
        for sub in range(2):
            for j in range(16):
                mm(p, ps[2][:, :], XsT[sub][:, j, :], w1b[s2][:, j, :], j == 0, j == 15, reads=[("XsT", sub), ("w1b", s2)], writes=["ps2"])
            for j in range(16):
                mm(p, ps[3][:, :], XsT[sub][:, j, :], w3b[s2][:, j, :], j == 0, j == 15, reads=[("XsT", sub), ("w3b", s2)], writes=["ps3"])
            act(p, hs[sub], ps[2][:, :], AF.Silu, reads=["ps2"], writes=[("hs", sub)])
            tt_(p, "dve", Hb[sub], ps[3][:, :], hs[sub], ALU.mult, reads=["ps3", ("hs", sub)], writes=[("Hb", sub)])
        for sub in range(2):
            hv = Hb[sub].rearrange("s (q j) -> s j q", j=4)
            for j in range(4):
                trp(p, psb(sub, 1024)[:, j * 128:(j + 1) * 128], hv[:, j, :], identb[:], reads=[("Hb", sub), "identb"], writes=["ps%d" % sub])
            cp(p, "act", HT[sub].rearrange("p j s -> p (j s)"), psb(sub, 1024)[:, 0:512], reads=["ps%d" % sub], writes=[("HT", sub)])
        for sub in range(2):
            r0 = b * BLKS + sub * 128
            for oc in range(4):
                for j in range(4):
                    mm(p, ps[4 + oc][:, :], HT[sub][:, j, :], w2b[:, j, oc * 512:(oc + 1) * 512], j == 0, j == 3, reads=[("HT", sub), "w2b"], writes=["ps%d" % (4 + oc)])
                cp(p, "act" if oc % 2 == 0 else "dve", ybs[sub][:, oc * 512:(oc + 1) * 512], ps[4 + oc][:, :], reads=["ps%d" % (4 + oc)], writes=[("ybs", sub)])
            dm(p, "sync", YB[r0:r0 + 128, :], ybs[sub], reads=[("ybs", sub)], writes=["YB"])
    p.barrier()
    ar.off = mark
    lng = ar.f32(D)
    lnb = ar.f32(D)
    y0 = [ar.f32(D) for _ in range(2)]
    y1 = [ar.f32(D) for _ in range(2)]
    x1 = [ar.f32(D) for _ in range(2)]
    hb2 = [ar.bf(D) for _ in range(2)]
    stats = ar.f32(24)
    mv = ar.f32(4)
    bcast_load(lng, "lng", wl["ln2_g"], D)
    bcast_load(lnb, "lnb", wl["ln2_b"], D)
    for tile in range(NT):
        b = tile % 2
        rs = slice(tile * 128, (tile + 1) * 128)
        for k, yk in ((0, y0), (1, y1)):
            j = k * NT + tile
            p.dma("pool", lambda e, j=j, yk=yk, b=b: e.indirect_dma_start(
                out=yk[b], out_offset=None, in_=YB[:, :], in_offset=IOA(ap=desti[:, j:j + 1], axis=0)),
                reads=["desti", "YB"], writes=[("y%d" % k, b)])
        dm(p, "sync", x1[b], XA[rs, :], writes=[("x1", b)])
        ts(p, "dve", y0[b], y0[b], wts[:, tile, 0:1], None, ALU.mult, reads=[("y0", b), "wts"], writes=[("y0", b)])
        stt(p, y0[b], y1[b], wts[:, tile, 1:2], y0[b], ALU.mult, ALU.add, reads=[("y0", b), ("y1", b), "wts"], writes=[("y0", b)])
        stt(p, x1[b], x1[b], ALPHA, y0[b], ALU.mult, ALU.add, reads=[("y0", b), ("x1", b)], writes=[("x1", b)])
        layer_norm_rows(x1[b], ("x1", b), x1[b], ("x1", b), D, lng, lnb, ("lng", "lnb"), stats, mv, "ln2")
        dm(p, "sync", dst[rs, :], x1[b], reads=[("x1", b)], writes=["dst"])
        if not last:
            tile_to_xT(x1[b], ("x1", b), tile, 0, hb2[b], ("hb2", b))


DEPTH = 4
_NC_CACHE = {}


def kernel(**inputs):
    x = np.asarray(inputs["x"], dtype=np.float32)
    weights = {k: inputs[k] for k in WEIGHT_SHAPES if k in inputs}
    consts = host_consts()
    if "nc" not in _NC_CACHE:
        _NC_CACHE["nc"] = build(DEPTH)
    nc = _NC_CACHE["nc"]
    base = make_in_map(x[0], weights, consts)
    in_maps = []
    for c in range(8):
        m = dict(base)
        m["x"] = np.ascontiguousarray(x[c])
        in_maps.append(m)
    res = run_bass_kernel_spmd(nc, in_maps, core_ids=list(range(8)))
    return np.stack([np.asarray(r["y"], dtype=np.float32) for r in res.results], axis=0)
```

```python
import contextlib
import math
import os
import numpy as np
import concourse.bass as bass
import concourse.mybir as mybir
from concourse.bass_utils import run_bass_kernel_spmd

F32 = mybir.dt.float32
BF16 = mybir.dt.bfloat16
I32 = mybir.dt.int32
AF = mybir.ActivationFunctionType
ALU = mybir.AluOpType
AX = mybir.AxisListType

S = 2048
D = 2048
NT = 16
D_IN = 7696
OFF_MQ, OFF_MK, OFF_MV, OFF_MO, OFF_MG = 0, 256, 512, 1024, 1536
OFF_AQ, OFF_AK, OFF_AV = 1552, 3088, 4624
OFF_SU, OFF_SV, OFF_PP = 6160, 6672, 7184
ALPHA = 8.0 ** 0.25
LN_EPS = 1e-5
ATT_R = (1, 2, 8)
ATT_DIL = (1, 4, 16)
NBLK = 48
BLKS = 256
NSLOT = NBLK * BLKS
ARN = 71000

ENGS = ("sync", "act", "dve", "pool", "pe")
N_DMA_SEMS = 16


class Prog:
    def __init__(self, nc):
        self.nc = nc
        self.ops = {e: [] for e in ENGS}
        self.cnt = {e: 0 for e in ENGS}
        self.dcnt = {e: 0 for e in ENGS}
        self.res = {}
        self.last = {}
        self.bar = {e: set() for e in ENGS}

    def _deps(self, eng, reads, writes):
        deps = set(self.bar[eng])
        self.bar[eng] = set()
        for k in reads:
            r = self.res.get(k)
            if r and r[0] is not None:
                deps.add(r[0])
        for k in writes:
            r = self.res.get(k)
            if r:
                if r[0] is not None:
                    deps.add(r[0])
                deps.update(r[1])
        return deps

    def _commit(self, tok, reads, writes):
        self.last[tok[0]] = tok
        for k in reads:
            r = self.res.setdefault(k, [None, []])
            r[1].append(tok)
        for k in writes:
            self.res[k] = [tok, []]

    def op(self, eng, fn, reads=(), writes=()):
        deps = self._deps(eng, reads, writes)
        self.cnt[eng] += 1
        tok = (("c", eng), self.cnt[eng], eng)
        self.ops[eng].append((fn, deps, tok, 1))
        self._commit(tok, reads, writes)
        return tok

    def dma(self, eng, fn, reads=(), writes=()):
        deps = self._deps(eng, reads, writes)
        i = self.dcnt[eng]
        self.dcnt[eng] += 1
        tok = (("d", eng, i % N_DMA_SEMS), 16 * (i // N_DMA_SEMS + 1), "dma_" + eng)
        prev = self.last.get(tok[0])
        if prev is not None:
            deps.add(prev)
        self.ops[eng].append((fn, deps, tok, 16))
        self._commit(tok, reads, writes)
        return tok

    def barrier(self):
        toks = set(self.last.values())
        for e in ENGS:
            self.bar[e] |= toks
        self.res = {}

    def emit(self):
        nc = self.nc
        finals = set(self.last.values())
        with contextlib.ExitStack() as st:
            sems = {}
            for e in ("act", "dve", "pool", "pe"):
                sems[("c", e)] = st.enter_context(nc.semaphore("c_" + e))
            for e in ("sync", "act", "pool"):
                for j in range(N_DMA_SEMS):
                    sems[("d", e, j)] = st.enter_context(nc.semaphore("d_%s_%d" % (e, j)))
            block = st.enter_context(nc.Block())
            ops = self.ops

            def run(engname, engobj, fin=()):
                waited = {}

                def waits(deps):
                    for (sk, val, deng) in sorted(deps, key=str):
                        if deng == "pe" and engname == "pe":
                            continue
                        if waited.get(sk, 0) >= val:
                            continue
                        engobj.wait_ge(sems[sk], val)
                        waited[sk] = val

                for fn, deps, tok, inc in ops[engname]:
                    waits(deps)
                    ins = fn(engobj)
                    ins.then_inc(sems[tok[0]], inc)
                waits([f for f in fin if not (f[2] == "pe" and engname == "pe")])

            @block.sync
            def _(sync):
                run("sync", sync, finals)

            @block.scalar
            def _(scalar):
                run("act", scalar)

            @block.vector
            def _(vector):
                run("dve", vector)

            @block.gpsimd
            def _(gpsimd):
                run("pool", gpsimd)

            @block.tensor
            def _(tensor):
                run("pe", tensor)


STOP = [None]
SKIP_UNUSED = bool(int(os.environ.get("SKIP_UNUSED", "1")))


def alibi_slope(g, h):
    n = 24
    return 2.0 ** (-8.0 * (g * 8 + h + 1) / n)


def host_consts():
    c = {}
    c["ident"] = np.eye(128, dtype=np.float32)
    s = np.arange(128)[:, None]
    t = np.arange(128)[None, :]
    c["tri_f"] = (s <= t).astype(np.float32)
    c["tri_b"] = (s >= t).astype(np.float32)
    c["mneg_f"] = np.where(s <= t, 0.0, -30000.0).astype(np.float32)
    c["mneg_b"] = np.where(s >= t, 0.0, -30000.0).astype(np.float32)
    c["tri_s"] = (s < t).astype(np.float32)
    c["ones"] = np.ones((128, 128), np.float32)
    tiles = []
    for g in range(3):
        dil = ATT_DIL[g]
        for o in range(-ATT_R[g], ATT_R[g] + 1):
            delta = (t - s) - 128 * o
            ok = (np.abs(delta) <= 64 * dil) & (delta % dil == 0)
            tiles.append(np.where(ok, np.abs(delta).astype(np.float32), 1e5))
    c["dist"] = np.ascontiguousarray(np.stack(tiles, axis=1).astype(np.float32))
    pe = np.zeros((128, 4, 2, 8), np.float32)
    for g, w in enumerate((2, 4, 8, 16)):
        h = w // 2
        for j in range(h):
            tt = j
            pe[:, g, 0, j] = 1.0 / (min(tt + h, S) - max(tt - h, 0))
            tt = S - h + j
            pe[:, g, 1, j] = 1.0 / (min(tt + h, S) - max(tt - h, 0))
    c["pool_edge"] = pe.reshape(128, 64)
    thr = np.zeros((128, NBLK, 32), np.float32)
    thr[:] = (float(BLKS) * np.arange(NBLK))[None, :, None]
    c["thr"] = thr.reshape(128, NBLK * 32)
    io = np.zeros((128, 4), np.float32)
    io[:] = 4.0 * np.arange(128)[:, None] + np.arange(4)[None, :]
    c["iow"] = io
    return c


CONST_SHAPES = {"ident": [128, 128], "tri_f": [128, 128], "tri_b": [128, 128], "mneg_f": [128, 128],
                "mneg_b": [128, 128], "tri_s": [128, 128], "ones": [128, 128], "dist": [128, 25, 128],
                "pool_edge": [128, 64], "thr": [128, NBLK * 32], "iow": [128, 4]}

WEIGHT_SHAPES = {
    "w_in": [D, D_IN], "ml_conv_w": [3, 512], "ml_gate_b": [16], "ml_norm_w": [512],
    "sg_ln_g": [512], "sg_ln_b": [512], "sg_w": [4, 128, 128], "sg_b": [4, 128],
    "pool_w": [4, 128, 128], "pool_scale": [512], "w_gate": [D, 4 * D], "b_gate": [4 * D],
    "w_branch": [4, 512, D], "w_out": [D, D], "ln1_g": [D], "ln1_b": [D],
    "w_router_group": [D, 4], "b_router_group": [4], "w_router_expert": [D, 32], "b_router_expert": [32],
    "w_exp_gate": [32, D, 512], "w_exp_up": [32, D, 512], "w_exp_down": [32, 512, D],
    "ln2_g": [D], "ln2_b": [D],
    "w_router": [128, 16 * 36], "b_router": [36],
}


def build(depth, stop_after=None, dbg=False):
    nc = bass.Bass("TRN2", target_bir_lowering=False)
    x_in = nc.dram_tensor("x", [S, D], F32, kind="ExternalInput").ap()
    W = {k: nc.dram_tensor(k, [depth] + v, F32, kind="ExternalInput").ap() for k, v in WEIGHT_SHAPES.items()}
    C = {k: nc.dram_tensor("c_" + k, v, F32, kind="ExternalInput").ap() for k, v in CONST_SHAPES.items()}
    y_out = nc.dram_tensor("y", [S, D], F32, kind="ExternalOutput").ap()
    okind = "ExternalOutput" if dbg else "Internal"
    XA = nc.dram_tensor("XA", [S, D], F32, kind=okind).ap()
    XB = nc.dram_tensor("XB", [S, D], F32, kind="Internal").ap()
    YST = nc.dram_tensor("YST", [4, 4, 128, S], BF16, kind=okind).ap()
    MT = nc.dram_tensor("MT", [16, 128, S], BF16, kind="Internal").ap()
    XS = nc.dram_tensor("XS", [NSLOT, D], BF16, kind="Internal").ap()
    YB = nc.dram_tensor("YB", [NSLOT, D], F32, kind="Internal").ap()

    st = contextlib.ExitStack()
    with st:
        def sb(name, shape, dt):
            return st.enter_context(nc.sbuf_tensor(name, shape, dt))

        xT = sb("xT", [128, 16, S], BF16)
        identb = sb("identb", [128, 128], BF16)
        identf = sb("identf", [128, 128], F32)
        onesf = sb("onesf", [128, 128], F32)
        AR = sb("AR", [128, ARN], BF16)
        logits = sb("logits", [128, NT * 36], F32)
        ps = [st.enter_context(nc.psum_tensor("ps%d" % i, [128, 512], F32)) for i in range(8)]
        p = Prog(nc)

        class Arena:
            def __init__(self):
                self.off = 0

            def reset(self):
                self.off = 0

            def f32(self, n):
                o = (self.off + 15) // 16 * 16
                self.off = o + 2 * n
                assert self.off <= ARN, self.off
                return AR[:, o:o + 2 * n].bitcast(F32)

            def bf(self, n):
                o = (self.off + 15) // 16 * 16
                self.off = o + n
                assert self.off <= ARN, self.off
                return AR[:, o:o + n]

            def i32(self, n):
                o = (self.off + 15) // 16 * 16
                self.off = o + 2 * n
                assert self.off <= ARN, self.off
                return AR[:, o:o + 2 * n].bitcast(I32)

        ar = Arena()

        def psb(i, n=512):
            return ps[i][:, 0:n // 2].bitcast(BF16)

        p.dma("pool", lambda e: e.dma_start(out=identb[:], in_=C["ident"]), writes=["identb"])
        p.dma("sync", lambda e: e.dma_start(out=identf[:], in_=C["ident"]), writes=["identf"])
        p.dma("sync", lambda e: e.dma_start(out=onesf[:], in_=C["ones"]), writes=["onesf"])

        def tile_to_xT(src, src_key, tt, pbank, hb, hb_key, lo=None):
            cp(p, "act", hb, src, reads=[src_key], writes=[hb_key])
            for half in range(2):
                bank = pbank + half
                pk = "ps%d" % bank
                for jj in range(8):
                    dc = half * 8 + jj
                    trp(p, psb(bank, 1024)[:, jj * 128:(jj + 1) * 128], hb[:, dc * 128:(dc + 1) * 128], identb[:],
                        reads=[hb_key, "identb"], writes=[pk])
                cp(p, "act" if half == 0 else "dve", xT[:, half * 8:(half + 1) * 8, tt * 128:(tt + 1) * 128],
                   psb(bank, 1024).rearrange("p (a b) -> p a b", b=128), reads=[pk], writes=[("xT", tt)])
            if lo is not None:
                lob, lob_key, loT, loT_key = lo
                tt_(p, "dve", lob, src, hb, ALU.subtract, reads=[src_key, hb_key], writes=[lob_key])
                for half in range(2):
                    bank = pbank + half
                    pk = "ps%d" % bank
                    for jj in range(8):
                        dc = half * 8 + jj
                        trp(p, psb(bank, 1024)[:, jj * 128:(jj + 1) * 128], lob[:, dc * 128:(dc + 1) * 128], identb[:],
                            reads=[lob_key, "identb"], writes=[pk])
                    cp(p, "act" if half == 0 else "dve", loT[:, half * 8:(half + 1) * 8, :],
                       psb(bank, 1024).rearrange("p (a b) -> p a b", b=128), reads=[pk], writes=[loT_key])

        XT_ALL = [("xT", tt) for tt in range(NT)]

        def load_w_fm(dst, key, wap, lo, ncols):
            p.dma("pool", lambda e: e.dma_start(
                out=dst, in_=wap[:, lo:lo + ncols].rearrange("(dc q) c -> q dc c", q=128)), writes=[key])

        def bcast_load(dst, key, vec_ap, n):
            for c0 in range(0, n, 512):
                c1 = min(n, c0 + 512)
                p.dma("sync", lambda e, c0=c0, c1=c1: e.dma_start(out=dst[:, c0:c1], in_=vec_ap[c0:c1].partition_broadcast(128)), writes=[key])

        def layer_norm_rows(src, src_key, dst, dst_key, n, gt, bt, gb_keys, stats, mv, tmpk):
            nch = max(1, n // 512)
            w = n // nch
            for c in range(nch):
                p.op("dve", lambda e, c=c: e.bn_stats(out=stats[:, c * 6:(c + 1) * 6], in_=src[:, c * w:(c + 1) * w]),
                     reads=[src_key], writes=[tmpk + "st"])
            p.op("dve", lambda e: e.bn_aggr(out=mv[:, 0:2], in_=stats[:, 0:nch * 6]), reads=[tmpk + "st"], writes=[tmpk + "mv"])
            p.op("act", lambda e: e.activation(out=mv[:, 2:3], in_=mv[:, 1:2], func=AF.Sqrt, bias=LN_EPS),
                 reads=[tmpk + "mv"], writes=[tmpk + "sd"])
            p.op("dve", lambda e: e.reciprocal(out=mv[:, 3:4], in_=mv[:, 2:3]), reads=[tmpk + "sd"], writes=[tmpk + "rs"])
            p.op("dve", lambda e: e.tensor_scalar(out=dst, in0=src, scalar1=mv[:, 0:1], scalar2=mv[:, 3:4],
                                                  op0=ALU.subtract, op1=ALU.mult),
                 reads=[src_key, tmpk + "mv", tmpk + "rs"], writes=[dst_key])
            if gt is not None:
                p.op("dve", lambda e: e.tensor_tensor(out=dst, in0=dst, in1=gt, op=ALU.mult),
                     reads=[dst_key, gb_keys[0]], writes=[dst_key])
                p.op("dve", lambda e: e.tensor_tensor(out=dst, in0=dst, in1=bt, op=ALU.add),
                     reads=[dst_key, gb_keys[1]], writes=[dst_key])

        cur_x = x_in
        for l in range(depth):
            last = (l == depth - 1)
            wl = {k: v[l] for k, v in W.items()}
            wl["_l"] = l
            wl["_W"] = W
            if l == 0:
                p.barrier()
                ar.reset()
                xt_tiles = [ar.f32(2048) for _ in range(2)]
                hbA = [ar.bf(2048) for _ in range(2)]
                for tt in range(NT):
                    b = tt % 2
                    p.dma("sync", lambda e, tt=tt, b=b, cx=cur_x: e.dma_start(out=xt_tiles[b], in_=cx[tt * 128:(tt + 1) * 128, :]),
                          writes=[("xld", b)])
                    tile_to_xT(xt_tiles[b], ("xld", b), tt, 0, hbA[b], ("hbA", b))

            if stop_after == "A":
                break
            STOP[0] = stop_after
            stage_mlstm(nc, p, ar, ps, psb, xT, XT_ALL, identb, identf, onesf, wl, C, YST, load_w_fm, bcast_load,
                        layer_norm_rows)
            if stop_after in ("mlstm", "ml1", "ml2"):
                break
            stage_attn(nc, p, ar, ps, psb, xT, XT_ALL, identb, wl, C, YST, load_w_fm)
            if stop_after == "attn":
                break
            stage_sg(nc, p, ar, ps, psb, xT, XT_ALL, identb, identf, wl, C, YST, load_w_fm, bcast_load, layer_norm_rows)
            stage_pool(nc, p, ar, ps, psb, xT, XT_ALL, wl, C, YST, load_w_fm)
            if stop_after == "branches":
                break
            stage_merge(nc, p, ar, ps, psb, xT, XT_ALL, identf, wl, C, YST, MT, cur_x, XA, load_w_fm, bcast_load,
                        layer_norm_rows, tile_to_xT, logits=(None if os.environ.get("NOROUTER") else logits))
            if stop_after in ("ln1", "mg1"):
                break
            dst = y_out if last else XB
            stage_moe(nc, p, ar, ps, psb, xT, XT_ALL, identb, identf, onesf, wl, C, XA, XS, YB, dst, bcast_load,
                      layer_norm_rows, tile_to_xT, logits, first=(l == 0), last=last)
            cur_x = XB
        p.barrier()
        p.emit()
    return nc


def mm(p, out, lhsT, rhs, start=True, stop=True, reads=(), writes=()):
    return p.op("pe", lambda e: e.matmul(out, lhsT=lhsT, rhs=rhs, start=start, stop=stop), reads, writes)


def trp(p, out, in_, ident, reads=(), writes=()):
    return p.op("pe", lambda e: e.transpose(out=out, in_=in_, identity=ident), reads, writes)


def act(p, out, in_, func, reads=(), writes=(), bias=None, scale=None):
    kw = {}
    if bias is not None:
        kw["bias"] = bias
    if scale is not None:
        kw["scale"] = scale
    return p.op("act", lambda e: e.activation(out=out, in_=in_, func=func, **kw), reads, writes)


def ts(p, eng, out, in0, s1, s2, op0, op1=None, reads=(), writes=()):
    if op1 is None:
        return p.op(eng, lambda e: e.tensor_scalar(out=out, in0=in0, scalar1=s1, scalar2=None, op0=op0), reads, writes)
    return p.op(eng, lambda e: e.tensor_scalar(out=out, in0=in0, scalar1=s1, scalar2=s2, op0=op0, op1=op1), reads, writes)


def stt(p, out, in0, scalar, in1, op0, op1, reads=(), writes=()):
    return p.op("dve", lambda e: e.scalar_tensor_tensor(out=out, in0=in0, scalar=scalar, in1=in1, op0=op0, op1=op1),
                reads, writes)


def tt_(p, eng, out, in0, in1, op, reads=(), writes=()):
    return p.op(eng, lambda e: e.tensor_tensor(out=out, in0=in0, in1=in1, op=op), reads, writes)


def cp(p, eng, out, in_, reads=(), writes=()):
    if eng == "act":
        return p.op("act", lambda e: e.activation(out=out, in_=in_, func=AF.Copy), reads, writes)
    return p.op(eng, lambda e: e.tensor_copy(out=out, in_=in_), reads, writes)


def ms(p, eng, ap, val, writes=()):
    return p.op(eng, lambda e: e.memset(ap, val), (), writes)


def dm(p, eng, out, in_, reads=(), writes=(), **kw):
    return p.dma(eng, lambda e: e.dma_start(out=out, in_=in_, **kw), reads, writes)


def xt_keys(t0, n):
    return [("xT", t) for t in range(t0, t0 + n)]


def proj_fm(p, ps, xT, wt, wkey, ncols, evac):
    for q in range(4):
        b = q % 2
        pk = "ps%d" % b
        for dc in range(16):
            mm(p, ps[b][0:ncols, :], wt[:, dc, :], xT[:, dc, q * 512:(q + 1) * 512], dc == 0, dc == 15,
               reads=[wkey] + xt_keys(q * 4, 4), writes=[pk])
        evac(q, ps[b][0:ncols, :], pk)


def proj_tm(p, ps, xT, wt, wkey, ncols, tile, bank):
    pk = "ps%d" % bank
    for dc in range(16):
        mm(p, ps[bank][:, 0:ncols], xT[:, dc, tile * 128:(tile + 1) * 128], wt[:, dc, :], dc == 0, dc == 15,
           reads=[wkey, ("xT", tile)], writes=[pk])
    return ps[bank][:, 0:ncols], pk


def stage_mlstm(nc, p, ar, ps, psb, xT, XT_ALL, identb, identf, onesf, wl, C, YST, load_w_fm, bcast_load,
                layer_norm_rows):
    p.barrier()
    ar.reset()
    w_in = wl["w_in"]
    qkT = ar.bf(4 * S).rearrange("p (c t) -> p c t", t=S)
    ktok = ar.bf(NT * 256).rearrange("p (a b) -> p a b", b=256)
    vaug = ar.bf(NT * 4 * 129).rearrange("p (a h v) -> p a h v", h=4, v=129)
    convw = ar.f32(12)
    gb = ar.f32(16)
    normw = ar.f32(512)
    tri = [ar.f32(128), ar.f32(128)]
    mneg = [ar.f32(128), ar.f32(128)]
    gtok = ar.f32(NT * 16).rearrange("p (a b) -> p a b", b=16)
    G = {k: ar.f32(128) for k in ("lf", "ig", "b", "tot", "imb", "wk", "dec", "ebt", "tmp")}
    mark = ar.off
    for j in range(3):
        dm(p, "sync", convw[:, j * 4:(j + 1) * 4], wl["ml_conv_w"][j].rearrange("(c q) -> q c", q=128),
           writes=["convw"], allow_slow_non_contiguous=True)
    bcast_load(gb, "gb", wl["ml_gate_b"], 16)
    bcast_load(normw, "normw", wl["ml_norm_w"], 512)
    dm(p, "sync", tri[0], C["tri_f"], writes=["tri0"])
    dm(p, "sync", tri[1], C["tri_b"], writes=["tri1"])
    dm(p, "sync", mneg[0], C["mneg_f"], writes=["mneg0"])
    dm(p, "sync", mneg[1], C["mneg_b"], writes=["mneg1"])
    wqk = ar.bf(16 * 128).rearrange("p (a b) -> p a b", b=128)
    zqk = ar.f32(2050)
    ctmp = ar.f32(2048)
    wv = ar.bf(16 * 512).rearrange("p (a b) -> p a b", b=512)
    wg = ar.bf(16 * 16).rearrange("p (a b) -> p a b", b=16)
    ms(p, "pool", zqk[:, 0:1], 0.0, writes=["zqk"])
    ms(p, "pool", zqk[:, 2049:2050], 0.0, writes=["zqk"])
    for ch in range(4):
        load_w_fm(wqk, "wqk", w_in, OFF_MQ + ch * 128, 128)

        def evac(q, pap, pk):
            cp(p, "act", zqk[:, 1 + q * 512:1 + (q + 1) * 512], pap, reads=[pk], writes=["zqk"])
        proj_fm(p, ps, xT, wqk, "wqk", 128, evac)
        ts(p, "dve", ctmp, zqk[:, 0:2048], convw[:, ch:ch + 1], None, ALU.mult, reads=["zqk", "convw"], writes=["ctmp"])
        stt(p, ctmp, zqk[:, 1:2049], convw[:, 4 + ch:5 + ch], ctmp, ALU.mult, ALU.add, reads=["zqk", "convw", "ctmp"], writes=["ctmp"])
        stt(p, ctmp, zqk[:, 2:2050], convw[:, 8 + ch:9 + ch], ctmp, ALU.mult, ALU.add, reads=["zqk", "convw", "ctmp"], writes=["ctmp"])
        act(p, qkT[:, ch, :], ctmp, AF.Silu, reads=["ctmp"], writes=[("qkT", ch)])
        if ch < 2:
            ts(p, "pool", qkT[:, ch, :], qkT[:, ch, :], 0.125, None, ALU.mult, reads=[("qkT", ch)], writes=[("qkT", ch)])
    for tile in range(NT):
        b = 2 + tile % 2
        pk = "ps%d" % b
        for kc in range(2):
            trp(p, psb(b)[:, kc * 128:(kc + 1) * 128], qkT[:, 2 + kc, tile * 128:(tile + 1) * 128], identb[:],
                reads=[("qkT", 2 + kc), "identb"], writes=[pk])
        cp(p, "dve", ktok[:, tile, :], psb(b)[:, 0:256], reads=[pk], writes=["ktok"])
    load_w_fm(wv, "wv", w_in, OFF_MV, 512)
    load_w_fm(wg, "wg", w_in, OFF_MG, 16)
    ms(p, "pool", vaug[:, :, :, 128:129], 1.0, writes=["vaug"])
    for tile in range(NT):
        b = 4 + tile % 2
        pap, pk = proj_tm(p, ps, xT, wv, "wv", 512, tile, b)
        cp(p, "act", vaug[:, tile, :, 0:128], pap.rearrange("p (h v) -> p h v", v=128), reads=[pk], writes=["vaug"])
        b2 = 6 + tile % 2
        pap2, pk2 = proj_tm(p, ps, xT, wg, "wg", 16, tile, b2)
        tt_(p, "dve", gtok[:, tile, :], pap2, gb, ALU.add, reads=[pk2, "gb"], writes=["gtok"])
    gv = gtok.rearrange("p a (d y h) -> p d a y h", d=2, y=2, h=4)

    def g4(t):
        return t.rearrange("p (d a h) -> p d a h", d=2, h=4)
    cp(p, "dve", g4(G["ig"]), gv[:, :, :, 0, :], reads=["gtok"], writes=["ig"])
    act(p, g4(G["tmp"]), gv[:, :, :, 1, :], AF.Exp, reads=["gtok"], writes=["gtmp"], scale=-1.0)
    act(p, G["tmp"], G["tmp"], AF.Ln, reads=["gtmp"], writes=["gtmp"], bias=1.0)
    ts(p, "dve", G["lf"], G["tmp"], -1.0, None, ALU.mult, reads=["gtmp"], writes=["lf"])
    for d in range(2):
        mm(p, ps[0][:, d * 64:(d + 1) * 64], tri[d], G["lf"][:, d * 64:(d + 1) * 64], True, True,
           reads=["tri%d" % d, "lf"], writes=["ps0"])
    cp(p, "dve", G["b"], ps[0][:, 0:128], reads=["ps0"], writes=["gb_"])
    mm(p, ps[1][:, 0:128], onesf[:], G["lf"], True, True, reads=["onesf", "lf"], writes=["ps1"])
    cp(p, "dve", G["tot"], ps[1][:, 0:128], reads=["ps1"], writes=["tot"])
    tt_(p, "dve", G["imb"], G["ig"], G["b"], ALU.subtract, reads=["ig", "gb_"], writes=["imb"])
    tt_(p, "dve", G["tmp"], G["tot"], G["imb"], ALU.add, reads=["tot", "imb", "gtmp"], writes=["gtmp"])
    act(p, G["wk"], G["tmp"], AF.Exp, reads=["gtmp"], writes=["wk"])
    act(p, G["dec"], G["tot"], AF.Exp, reads=["tot"], writes=["dec"])
    act(p, G["ebt"], G["b"], AF.Exp, reads=["gb_"], writes=["ebt"])

    if STOP[0] == "ml1":
        return
    p.barrier()
    ar.off = mark
    hsum = ar.f32(NT * 512).rearrange("p (a h v) -> p a h v", h=4, v=128)
    mark2 = ar.off
    CT = ar.f32(8 * 129).rearrange("p (u v) -> p u v", v=129)
    CTb = ar.bf(8 * 130).rearrange("p (u v) -> p u v", v=130)
    NS = 2
    Rt = [ar.f32(128) for _ in range(NS)]
    AT = [ar.f32(128) for _ in range(NS)]
    ST = [ar.bf(128) for _ in range(NS)]
    nsb = [ar.f32(132) for _ in range(NS)]
    ddt = [ar.f32(4) for _ in range(NS)]
    kw = [[ar.bf(128) for _ in range(2)] for _ in range(NS)]
    for sidx in range(NS):
        for hh in range(2):
            ms(p, "pool", kw[sidx][hh], 0.0, writes=[("kw", sidx, hh)])
    unit = 0
    import os
    for d in range(2):
        order = list(range(NT)) if d == 0 else list(range(NT - 1, -1, -1))
        for ci, c in enumerate(order):
            for h in range(4):
                if unit >= int(os.environ.get("MLU", "100000")):
                    continue
                sl = unit % NS
                unit += 1
                b0, b1, b2 = 4 * sl, 4 * sl + 1, 4 * sl + 2
                k0, k1, k2 = "ps%d" % b0, "ps%d" % b1, "ps%d" % b2
                col = d * 64 + c * 4 + h
                hh, pc = h % 2, h // 2
                rows = slice(hh * 64, (hh + 1) * 64)
                u = d * 4 + h
                tsl = slice(c * 128, (c + 1) * 128)
                ts(p, "pool", Rt[sl], tri[d], G["lf"][:, col:col + 1], None, ALU.mult, reads=["tri%d" % d, "lf"], writes=[("Rt", sl)])
                mm(p, ps[b0][:, 0:128], onesf[:], Rt[sl], True, False, reads=["onesf", ("Rt", sl)], writes=[k0])
                mm(p, ps[b0][:, 0:128], identf[:], mneg[d], False, True, reads=["identf", "mneg%d" % d], writes=[k0])
                act(p, AT[sl], ps[b0][:, 0:128], AF.Exp, reads=[k0, "imb"], writes=[("AT", sl)], bias=G["imb"][:, col:col + 1])
                mm(p, ps[b1][:, 0:128], qkT[rows, 2 + pc, tsl], qkT[rows, pc, tsl], True, True,
                   reads=[("qkT", 2 + pc), ("qkT", pc)], writes=[k1])
                tt_(p, "dve", ST[sl], ps[b1][:, 0:128], AT[sl], ALU.mult, reads=[k1, ("AT", sl)], writes=[("ST", sl)])
                mm(p, ps[b2][:, 0:129], ST[sl], vaug[:, c, h, :], True, True, reads=[("ST", sl), "vaug"], writes=[k2])
                cp(p, "act", nsb[sl][:, 0:129], ps[b2][:, 0:129], reads=[k2], writes=[("nsb", sl)])
                if ci > 0:
                    mm(p, ps[b0][:, 256:385], qkT[rows, pc, tsl], CTb[rows, u, 0:129], True, True,
                       reads=[("qkT", pc), ("CTb", u)], writes=[k0])
                    stt(p, nsb[sl][:, 0:129], ps[b0][:, 256:385], G["ebt"][:, col:col + 1], nsb[sl][:, 0:129], ALU.mult, ALU.add,
                        reads=[k0, "ebt", ("nsb", sl)], writes=[("nsb", sl)])
                dd = ddt[sl]
                stt(p, dd[:, 0:1], nsb[sl][:, 128:129], -1.0, nsb[sl][:, 128:129], ALU.mult, ALU.max, reads=[("nsb", sl)], writes=[("dd", sl)])
                ts(p, "dve", dd[:, 1:2], dd[:, 0:1], 1.0, None, ALU.max, reads=[("dd", sl)], writes=[("dd", sl)])
                p.op("dve", lambda e, dd=dd: e.reciprocal(out=dd[:, 2:3], in_=dd[:, 1:2]), reads=[("dd", sl)], writes=[("dd", sl)])
                if d == 0:
                    ts(p, "dve", hsum[:, c, h, :], nsb[sl][:, 0:128], dd[:, 2:3], None, ALU.mult,
                       reads=[("nsb", sl), ("dd", sl)], writes=[("hsum", c, h)])
                else:
                    stt(p, hsum[:, c, h, :], nsb[sl][:, 0:128], dd[:, 2:3], hsum[:, c, h, :], ALU.mult, ALU.add,
                        reads=[("nsb", sl), ("dd", sl), ("hsum", c, h)], writes=[("hsum", c, h)])
                if ci < NT - 1:
                    ts(p, "pool", kw[sl][hh][:, rows], ktok[:, c, h * 64:(h + 1) * 64], G["wk"][:, col:col + 1], None, ALU.mult,
                       reads=["ktok", "wk"], writes=[("kw", sl, hh)])
                    mm(p, ps[b1][:, 256:385], kw[sl][hh], vaug[:, c, h, :], True, True, reads=[("kw", sl, hh), "vaug"], writes=[k1])
                    if ci == 0:
                        cp(p, "dve", CT[rows, u, :], ps[b1][rows, 256:385], reads=[k1], writes=[("CT", u)])
                    else:
                        stt(p, CT[rows, u, :], CT[rows, u, :], G["dec"][rows, col:col + 1], ps[b1][rows, 256:385], ALU.mult, ALU.add,
                            reads=[k1, "dec", ("CT", u)], writes=[("CT", u)])
                    cp(p, "act", CTb[rows, u, 0:129], CT[rows, u, :], reads=[("CT", u)], writes=[("CTb", u)])
    if STOP[0] == "ml2":
        return
    p.barrier()
    ar.off = mark2
    wo = ar.bf(16 * 512).rearrange("p (a b) -> p a b", b=512)
    yT = ar.bf(4 * S).rearrange("p (c t) -> p c t", t=S)
    osig = [ar.f32(512) for _ in range(2)]
    hn = [ar.f32(512) for _ in range(2)]
    ybf = [ar.bf(512) for _ in range(2)]
    stats = ar.f32(24)
    mv = ar.f32(16)
    load_w_fm(wo, "wo", w_in, OFF_MO, 512)
    for tile in range(NT):
        s2 = tile % 2
        pap, pk = proj_tm(p, ps, xT, wo, "wo", 512, tile, s2)
        act(p, osig[s2], pap, AF.Sigmoid, reads=[pk], writes=[("osig", s2)])
        for h in range(4):
            layer_norm_rows(hsum[:, tile, h, :], ("hsum", tile, h), hn[s2][:, h * 128:(h + 1) * 128], ("hn", s2), 128,
                            None, None, None, stats[:, h * 6:(h + 1) * 6], mv[:, h * 4:(h + 1) * 4], "mlln%d" % h)
        tt_(p, "pool", hn[s2], hn[s2], normw, ALU.mult, reads=[("hn", s2), "normw"], writes=[("hn", s2)])
        tt_(p, "pool", ybf[s2], hn[s2], osig[s2], ALU.mult, reads=[("hn", s2), ("osig", s2)], writes=[("ybf", s2)])
        b = 2 + s2
        pk2 = "ps%d" % b
        for c4 in range(4):
            trp(p, psb(b)[:, c4 * 128:(c4 + 1) * 128], ybf[s2][:, c4 * 128:(c4 + 1) * 128], identb[:],
                reads=[("ybf", s2), "identb"], writes=[pk2])
        cp(p, "act", yT[:, :, tile * 128:(tile + 1) * 128], psb(b)[:, 0:512].rearrange("p (c t) -> p c t", t=128),
           reads=[pk2], writes=["yT"])
    for c4 in range(4):
        dm(p, "sync", YST[0, c4], yT[:, c4, :], reads=["yT"], writes=["YST0"])


def stage_attn(nc, p, ar, ps, psb, xT, XT_ALL, identb, wl, C, YST, load_w_fm):
    p.barrier()
    ar.reset()
    w_in = wl["w_in"]
    dist = ar.f32(25 * 128).rearrange("p (a b) -> p a b", b=128)
    dm(p, "sync", dist, C["dist"], writes=["dist"])
    qT = ar.bf(3 * S).rearrange("p (g t) -> p g t", t=S)
    kT = ar.bf(3 * S).rearrange("p (g t) -> p g t", t=S)
    vat = ar.bf(NT * 3 * 2 * 65).rearrange("p (a g h v) -> p a g h v", g=3, h=2, v=65)
    wch = [ar.bf(16 * 128).rearrange("p (a b) -> p a b", b=128) for _ in range(2)]
    wv3 = ar.bf(16 * 384).rearrange("p (a b) -> p a b", b=384)
    yT = ar.bf(S)
    LAG = 3
    NL = LAG + 2
    Lt = [ar.f32(512) for _ in range(NL)]
    Pt = [ar.bf(512) for _ in range(NL)]
    ytile = [ar.bf(128) for _ in range(2)]
    rd = [ar.f32(2) for _ in range(2)]
    base = (0, 3, 8)
    ms(p, "pool", vat[:, :, :, :, 64:65], 1.0, writes=["vat"])
    wi = 0
    for hp in range(4):
        for g in range(3):
            for which, dstT, off in (("q", qT, OFF_AQ), ("k", kT, OFF_AK)):
                wb = wch[wi % 2]
                wk_ = ("wch", wi % 2)
                wi += 1
                load_w_fm(wb, wk_, w_in, off + (g * 4 + hp) * 128, 128)

                def evac(q, pap, pk, dstT=dstT, g=g, which=which):
                    cp(p, "act", dstT[:, g, q * 512:(q + 1) * 512], pap, reads=[pk], writes=[(which + "T", g)])
                proj_fm(p, ps, xT, wb, wk_, 128, evac)
            p.dma("pool", lambda e, g=g, hp=hp: e.dma_start(
                out=wv3[:, :, g * 128:(g + 1) * 128],
                in_=w_in[:, OFF_AV + (g * 8 + 2 * hp) * 64:OFF_AV + (g * 8 + 2 * hp) * 64 + 128].rearrange("(dc q) c -> q dc c", q=128)),
                writes=["wv3"])
        for tile in range(NT):
            b = 2 + tile % 2
            pap, pk = proj_tm(p, ps, xT, wv3, "wv3", 384, tile, b)
            cp(p, "dve", vat[:, tile, :, :, 0:64], pap.rearrange("p (g h v) -> p g h v", g=3, h=2), reads=[pk], writes=["vat"])
        batches = []
        for qt in range(NT):
            for h2 in range(2):
                units = []
                for g in range(3):
                    kbs = list(range(max(0, qt - ATT_R[g]), min(NT - 1, qt + ATT_R[g]) + 1))
                    for i0 in range(0, len(kbs), 4):
                        units.append((g, kbs[i0:i0 + 4]))
                for ui, (g, kbs) in enumerate(units):
                    batches.append((qt, h2, g, kbs, ui == 0, ui == len(units) - 1))

        def front(i):
            qt, h2, g, kbs, first, last = batches[i]
            qs = slice(qt * 128, (qt + 1) * 128)
            head = 2 * hp + h2
            rows = slice(h2 * 64, (h2 + 1) * 64)
            n = len(kbs)
            sb_ = i % 4
            sk = "ps%d" % sb_
            sl = i % NL
            for j, kb in enumerate(kbs):
                mm(p, ps[sb_][:, j * 128:(j + 1) * 128], kT[rows, g, kb * 128:(kb + 1) * 128], qT[rows, g, qs], True, True,
                   reads=[("kT", g), ("qT", g)], writes=[sk])
            idx0 = base[g] + (kbs[0] - qt) + ATT_R[g]
            stt(p, Lt[sl][:, 0:n * 128], dist[:, idx0:idx0 + n, :].rearrange("p a b -> p (a b)"),
                -8.0 * alibi_slope(g, head), ps[sb_][:, 0:n * 128], ALU.mult, ALU.add,
                reads=["dist", sk], writes=[("Lt", sl)])
            act(p, Pt[sl][:, 0:n * 128], Lt[sl][:, 0:n * 128], AF.Exp, reads=[("Lt", sl)], writes=[("Pt", sl)], scale=0.125)

        def back(i):
            qt, h2, g, kbs, first, last = batches[i]
            qs = slice(qt * 128, (qt + 1) * 128)
            sl = i % NL
            ab = 6 + h2
            ak = "ps%d" % ab
            n = len(kbs)
            for j, kb in enumerate(kbs):
                mm(p, ps[ab][:, 0:65], Pt[sl][:, j * 128:(j + 1) * 128], vat[:, kb, g, h2, :], first and j == 0, last and j == n - 1,
                   reads=[("Pt", sl), "vat"], writes=[ak])
            if last:
                p.op("dve", lambda e, ab=ab, h2=h2: e.reciprocal(out=rd[h2][:, 0:1], in_=ps[ab][:, 64:65]), reads=[ak], writes=[("rd", h2)])
                ts(p, "dve", ytile[qt % 2][:, h2 * 64:(h2 + 1) * 64], ps[ab][:, 0:64], rd[h2][:, 0:1], None, ALU.mult,
                   reads=[ak, ("rd", h2)], writes=[("ytile", qt % 2)])
                if h2 == 1:
                    tb = 4 + qt % 2
                    trp(p, psb(tb)[:, 0:128], ytile[qt % 2], identb[:], reads=[("ytile", qt % 2), "identb"], writes=["ps%d" % tb])
                    cp(p, "act", yT[:, qs], psb(tb)[:, 0:128], reads=["ps%d" % tb], writes=["yTa"])

        nb_ = len(batches)
        for i in range(nb_ + LAG):
            if i < nb_:
                front(i)
            if i >= LAG:
                back(i - LAG)
        dm(p, "sync", YST[1, hp], yT, reads=["yTa"], writes=["YST1"])


def stage_sg(nc, p, ar, ps, psb, xT, XT_ALL, identb, identf, wl, C, YST, load_w_fm, bcast_load, layer_norm_rows):
    p.barrier()
    ar.reset()
    w_in = wl["w_in"]
    wu = ar.bf(16 * 512).rearrange("p (a b) -> p a b", b=512)
    wv = ar.bf(16 * 512).rearrange("p (a b) -> p a b", b=512)
    lng = ar.f32(512)
    lnb = ar.f32(512)
    wsf = ar.f32(512).rearrange("p (g s) -> p g s", s=128)
    wsT = ar.bf(512).rearrange("p (g t) -> p g t", t=128)
    bs = ar.f32(4)
    yT = ar.bf(4 * S).rearrange("p (c t) -> p c t", t=S)
    u = [ar.f32(512) for _ in range(2)]
    v = [ar.f32(512) for _ in range(2)]
    vn = [ar.bf(512) for _ in range(2)]
    ysg = [ar.bf(512) for _ in range(2)]
    stats = ar.f32(8)
    mv = ar.f32(4)
    load_w_fm(wu, "wu", w_in, OFF_SU, 512)
    load_w_fm(wv, "wv", w_in, OFF_SV, 512)
    bcast_load(lng, "lng", wl["sg_ln_g"], 512)
    bcast_load(lnb, "lnb", wl["sg_ln_b"], 512)
    dm(p, "sync", wsf, wl["sg_w"].rearrange("g t s -> t g s"), writes=["wsf"])
    dm(p, "sync", bs, wl["sg_b"].rearrange("g t -> t g"), writes=["bs"], allow_slow_non_contiguous=True)
    for g in range(4):
        trp(p, ps[7][:, g * 128:(g + 1) * 128], wsf[:, g, :], identf[:], reads=["wsf", "identf"], writes=["ps7"])
    cp(p, "dve", wsT.rearrange("p g t -> p (g t)"), ps[7][:, 0:512], reads=["ps7"], writes=["wsT"])
    for tile in range(NT):
        s2 = tile % 2
        pap, pk = proj_tm(p, ps, xT, wu, "wu", 512, tile, 0 + s2)
        act(p, u[s2], pap, AF.Gelu, reads=[pk], writes=[("u", s2)])
        pap, pk = proj_tm(p, ps, xT, wv, "wv", 512, tile, 2 + s2)
        act(p, v[s2], pap, AF.Gelu, reads=[pk], writes=[("v", s2)])
        layer_norm_rows(v[s2], ("v", s2), v[s2], ("v", s2), 512, lng, lnb, ("lng", "lnb"), stats, mv, "sgln")
        cp(p, "pool", vn[s2], v[s2], reads=[("v", s2)], writes=[("vn", s2)])
        b = 4 + s2
        pk = "ps%d" % b
        for g in range(4):
            mm(p, ps[b][:, g * 128:(g + 1) * 128], wsT[:, g, :], vn[s2][:, g * 128:(g + 1) * 128], True, True,
               reads=["wsT", ("vn", s2)], writes=[pk])
        for g in range(4):
            stt(p, ysg[s2][:, g * 128:(g + 1) * 128], ps[b][:, g * 128:(g + 1) * 128], bs[:, g:g + 1], u[s2][:, g * 128:(g + 1) * 128],
                ALU.add, ALU.mult, reads=[pk, "bs", ("u", s2)], writes=[("ysg", s2)])
        b2 = 6
        for c4 in range(4):
            trp(p, psb(b2)[:, c4 * 128:(c4 + 1) * 128], ysg[s2][:, c4 * 128:(c4 + 1) * 128], identb[:], reads=[("ysg", s2), "identb"], writes=["ps6"])
        cp(p, "act", yT[:, :, tile * 128:(tile + 1) * 128], psb(b2)[:, 0:512].rearrange("p (c t) -> p c t", t=128), reads=["ps6"], writes=["yT"])
    for c4 in range(4):
        dm(p, "sync", YST[2, c4], yT[:, c4, :], reads=["yT"], writes=["YST2"])


def stage_pool(nc, p, ar, ps, psb, xT, XT_ALL, wl, C, YST, load_w_fm):
    p.barrier()
    ar.reset()
    w_in = wl["w_in"]
    PADW = S + 32
    wch = [ar.bf(16 * 128).rearrange("p (a b) -> p a b", b=128) for _ in range(2)]
    wp = ar.bf(512).rearrange("p (g d) -> p g d", d=128)
    psc = ar.f32(4)
    edge = ar.f32(64).rearrange("p (g s j) -> p g s j", s=2, j=8)
    pp = ar.f32(PADW)
    A = [ar.f32(PADW) for _ in range(2)]
    dT = ar.bf(S)
    yT = ar.bf(S)
    p.dma("pool", lambda e: e.dma_start(out=wp, in_=wl["pool_w"].rearrange("g c d -> c g d")), writes=["wp"])
    dm(p, "sync", psc, wl["pool_scale"].rearrange("(g q) -> q g", q=128), writes=["psc"], allow_slow_non_contiguous=True)
    dm(p, "sync", edge.rearrange("p g s j -> p (g s j)"), C["pool_edge"], writes=["edge"])
    ms(p, "pool", pp, 0.0, writes=["pp"])
    ms(p, "pool", A[0], 0.0, writes=["A0"])
    ms(p, "pool", A[1], 0.0, writes=["A1"])
    O = 16
    E0, EN = O - 8, S + 16
    for g in range(4):
        wb = wch[g % 2]
        wk_ = ("wch", g % 2)
        load_w_fm(wb, wk_, w_in, OFF_PP + g * 128, 128)

        def evac(q, pap, pk):
            cp(p, "act", pp[:, O + q * 512:O + (q + 1) * 512], pap, reads=[pk], writes=["pp"])
        proj_fm(p, ps, xT, wb, wk_, 128, evac)
        tt_(p, "pool", A[0][:, E0:E0 + EN], pp[:, E0 - 1:E0 - 1 + EN], pp[:, E0:E0 + EN], ALU.add, reads=["pp", "A0"], writes=["A0"])
        cur = 0
        sh = 1
        for step in range(g):
            nxt = 1 - cur
            tt_(p, "pool", A[nxt][:, E0:E0 + EN], A[cur][:, E0 - sh:E0 - sh + EN], A[cur][:, E0 + sh:E0 + sh + EN], ALU.add,
                reads=["A%d" % cur, "A%d" % nxt], writes=["A%d" % nxt])
            cur = nxt
            sh *= 2
        w = (2, 4, 8, 16)[g]
        hw = w // 2
        stt(p, dT[:, :], A[cur][:, O:O + S], 1.0 / w, pp[:, O:O + S], ALU.mult, ALU.subtract, reads=["A%d" % cur, "pp"], writes=["dT"])
        for side, c0 in ((0, 0), (1, S - hw)):
            tt_(p, "dve", A[cur][:, O + c0:O + c0 + hw], A[cur][:, O + c0:O + c0 + hw], edge[:, g, side, 0:hw], ALU.mult,
                reads=["A%d" % cur, "edge", "dT"], writes=["A%d" % cur])
            tt_(p, "dve", dT[:, c0:c0 + hw], A[cur][:, O + c0:O + c0 + hw], pp[:, O + c0:O + c0 + hw], ALU.subtract,
                reads=["A%d" % cur, "pp"], writes=["dT"])
        for q in range(4):
            b = 2 + q % 2
            pk = "ps%d" % b
            mm(p, ps[b][:, :], wp[:, g, :], dT[:, q * 512:(q + 1) * 512], True, True, reads=["wp", "dT"], writes=[pk])
            act(p, yT[:, q * 512:(q + 1) * 512], ps[b][:, :], AF.Identity, reads=[pk, "psc"], writes=["yT"], scale=psc[:, g:g + 1])
        dm(p, "sync", YST[3, g], yT, reads=["yT"], writes=["YST3"])


def stage_merge(nc, p, ar, ps, psb, xT, XT_ALL, identf, wl, C, YST, MT, cur_x, XA, load_w_fm, bcast_load,
                layer_norm_rows, tile_to_xT, logits=None):
    p.barrier()
    ar.reset()
    ystb = [ar.bf(4 * S).rearrange("p (k t) -> p k t", k=4) for _ in range(2)]
    wg = [ar.bf(16 * 512).rearrange("p (k c) -> p k c", c=512) for _ in range(2)]
    wb = [ar.bf(4 * 512).rearrange("p (k c) -> p k c", c=512) for _ in range(2)]
    bg = ar.f32(64)
    gsb = [ar.f32(512) for _ in range(2)]
    macc = [[ar.f32(512) for _ in range(4)] for _ in range(4)]
    tmpm = [ar.f32(512) for _ in range(2)]
    mTb = [ar.bf(512) for _ in range(2)]
    bgr = ar.f32(128)
    dm(p, "sync", bgr[0:64, :], wl["b_gate"].rearrange("(c q) -> c q", q=128), writes=["bgr"])
    trp(p, ps[7][:, 0:64], bgr[0:64, :], identf[0:64, 0:64], reads=["bgr", "identf"], writes=["ps7"])
    cp(p, "dve", bg, ps[7][:, 0:64], reads=["ps7"], writes=["bg"])
    w_gate, w_branch = wl["w_gate"], wl["w_branch"]
    it = 0
    for dcg in range(4):
        for n in range(4):
            b = it % 2
            it += 1
            c0 = n * D + dcg * 512
            p.dma("pool", lambda e, c0=c0, b=b: e.dma_start(
                out=wg[b], in_=w_gate[:, c0:c0 + 512].rearrange("(k q) c -> q k c", q=128)), writes=[("wg", b)])
            p.dma("pool", lambda e, n=n, dcg=dcg, b=b: e.dma_start(
                out=wb[b], in_=w_branch[n, :, dcg * 512:(dcg + 1) * 512].rearrange("(k q) c -> q k c", q=128)), writes=[("wb", b)])
            for k in range(4):
                dm(p, "sync", ystb[b][:, k, :], YST[n, k], writes=[("yst", b)])
            for dl in range(4):
                dc = dcg * 4 + dl
                cs = slice(dl * 128, (dl + 1) * 128)
                for q in range(4):
                    tq = slice(q * 512, (q + 1) * 512)
                    ga = (dl * 4 + q) % 2
                    gk = "ps%d" % ga
                    for k in range(16):
                        mm(p, ps[ga][:, :], wg[b][:, k, cs], xT[:, k, tq], k == 0, k == 15, reads=[("wg", b)] + xt_keys(q * 4, 4), writes=[gk])
                    act(p, gsb[ga], ps[ga][:, :], AF.Sigmoid, reads=[gk, "bg"], writes=[("gsb", ga)], bias=bg[:, n * 16 + dc:n * 16 + dc + 1])
                    pa = 2 + ga
                    pk = "ps%d" % pa
                    for k in range(4):
                        mm(p, ps[pa][:, :], wb[b][:, k, cs], ystb[b][:, k, tq], k == 0, k == 3, reads=[("wb", b), ("yst", b)], writes=[pk])
                    mk = ("macc", dl, q)
                    if n == 0:
                        tt_(p, "dve", macc[dl][q], ps[pa][:, :], gsb[ga], ALU.mult, reads=[pk, ("gsb", ga)], writes=[mk])
                    else:
                        tt_(p, "dve", tmpm[ga], ps[pa][:, :], gsb[ga], ALU.mult, reads=[pk, ("gsb", ga)], writes=[("tmpm", ga)])
                        if n < 3:
                            tt_(p, "pool", macc[dl][q], macc[dl][q], tmpm[ga], ALU.add, reads=[mk, ("tmpm", ga)], writes=[mk])
                        else:
                            tt_(p, "pool", mTb[ga], macc[dl][q], tmpm[ga], ALU.add, reads=[mk, ("tmpm", ga)], writes=[("mTb", ga)])
                            dm(p, "sync", MT[dc, :, tq], mTb[ga], reads=[("mTb", ga)], writes=["MT"])

    if STOP[0] == "mg1":
        return
    p.barrier()
    ar.reset()
    wout = ar.bf(16 * D).rearrange("p (k c) -> p k c", c=D)
    lng = ar.f32(D)
    lnb = ar.f32(D)
    mt = [ar.bf(16 * 128).rearrange("p (k t) -> p k t", t=128) for _ in range(2)]
    xres = [ar.f32(D) for _ in range(2)]
    sres = xres
    stats = ar.f32(24)
    mv = ar.f32(4)
    wr = ar.f32(16 * 36).rearrange("p (k c) -> p k c", c=36)
    whb = ar.bf(16 * 36).rearrange("p (k c) -> p k c", c=36)
    wlb = ar.bf(16 * 36).rearrange("p (k c) -> p k c", c=36)
    wtmp = ar.f32(16 * 36).rearrange("p (k c) -> p k c", c=36)
    brb = ar.f32(36)
    hb = [ar.bf(D) for _ in range(2)]
    lob = ar.bf(D)
    loT = ar.bf(16 * 128).rearrange("p (k t) -> p k t", t=128)
    for oc in range(4):
        p.dma("pool", lambda e, oc=oc: e.dma_start(out=wout[:, :, oc * 512:(oc + 1) * 512],
                                                   in_=wl["w_out"][:, oc * 512:(oc + 1) * 512].rearrange("(k q) c -> q k c", q=128)),
              writes=["wout"])
    bcast_load(lng, "lng", wl["ln1_g"], D)
    bcast_load(lnb, "lnb", wl["ln1_b"], D)
    dm(p, "sync", wr.rearrange("p k c -> p (k c)"), wl["w_router"], writes=["wr"])
    bcast_load(brb, "brb", wl["b_router"], 36)
    cp(p, "dve", whb, wr, reads=["wr"], writes=["whb"])
    tt_(p, "dve", wtmp, wr, whb, ALU.subtract, reads=["wr", "whb"], writes=["wtmp"])
    cp(p, "dve", wlb, wtmp, reads=["wtmp"], writes=["wlb"])
    P2 = int(os.environ.get("P2STOP", "99"))
    for tile in range(NT):
        b = tile % 2
        rs = slice(tile * 128, (tile + 1) * 128)
        for k in range(16):
            dm(p, "sync", mt[b][:, k, :], MT[k, :, rs], reads=["MT"], writes=[("mt", b)])
        dm(p, "sync", xres[b], cur_x[rs, :], writes=[("sres", b)])
        for oc in range(4):
            ok = "ps%d" % oc
            for k in range(16):
                mm(p, ps[oc][:, :], mt[b][:, k, :], wout[:, k, oc * 512:(oc + 1) * 512], k == 0, k == 15, reads=[("mt", b), "wout"], writes=[ok])
            stt(p, sres[b][:, oc * 512:(oc + 1) * 512], xres[b][:, oc * 512:(oc + 1) * 512], ALPHA, ps[oc][:, :], ALU.mult, ALU.add,
                reads=[ok, ("sres", b)], writes=[("sres", b)])
        layer_norm_rows(sres[b], ("sres", b), sres[b], ("sres", b), D, lng, lnb, ("lng", "lnb"), stats, mv, "ln1")
        dm(p, "sync", XA[rs, :], sres[b], reads=[("sres", b)], writes=["XA"])
        if P2 <= 4:
            continue
        tsl = slice(tile * 128, (tile + 1) * 128)
        tile_to_xT(sres[b], ("sres", b), tile, 4, hb[b], ("hb", b), lo=((lob, "lob", loT, "loT") if logits is not None else None))
        if P2 <= 5:
            continue
        if logits is not None:
            n = 0
            for k in range(16):
                for a_, ak_, w_, wk_ in ((xT[:, k, tsl], ("xT", tile), whb, "whb"), (xT[:, k, tsl], ("xT", tile), wlb, "wlb"),
                                         (loT[:, k, :], "loT", whb, "whb")):
                    mm(p, ps[6][:, 0:36], a_, w_[:, k, :], n == 0, n == 47, reads=[ak_, wk_], writes=["ps6"])
                    n += 1
            tt_(p, "dve", logits[:, tile * 36:(tile + 1) * 36], ps[6][:, 0:36], brb, ALU.add, reads=["ps6", "brb"], writes=["logits"])


def make_in_map(x_b, weights, consts):
    m = {"x": np.ascontiguousarray(x_b, dtype=np.float32)}
    weights = dict(weights)
    wr = np.concatenate([np.asarray(weights["w_router_group"]), np.asarray(weights["w_router_expert"])], axis=-1)
    L_ = wr.shape[0]
    weights["w_router"] = wr.reshape(L_, 16, 128, 36).transpose(0, 2, 1, 3)
    weights["b_router"] = np.concatenate([np.asarray(weights["b_router_group"]), np.asarray(weights["b_router_expert"])], axis=-1)
    for k, shp in WEIGHT_SHAPES.items():
        w = np.asarray(weights[k], dtype=np.float32)
        m[k] = np.ascontiguousarray(w.reshape([w.shape[0]] + shp))
    for k, v in consts.items():
        m["c_" + k] = np.ascontiguousarray(v.reshape(CONST_SHAPES[k]))
    return m


def bc(ap, axis, shape):
    return ap.unsqueeze(axis).to_broadcast(shape)


def red(p, eng, out, in_, op, axis, reads=(), writes=()):
    return p.op(eng, lambda e: e.tensor_reduce(out=out, in_=in_, axis=axis, op=op), reads, writes)


def stage_moe(nc, p, ar, ps, psb, xT, XT_ALL, identb, identf, onesf, wl, C, XA, XS, YB, dst, bcast_load,
              layer_norm_rows, tile_to_xT, logits, first, last):
    p.barrier()
    ar.reset()
    IOA = bass.IndirectOffsetOnAxis
    desti = ar.i32(32)
    idxW = ar.i32(NBLK * 4).rearrange("p (b q) -> p b q", q=4)
    idxW2 = ar.i32(NBLK * 4).rearrange("p (b q) -> p b q", q=4)
    wts = ar.f32(32).rearrange("p (a k) -> p a k", k=2)
    mark = ar.off
    L3 = logits.rearrange("p (a c) -> p a c", c=36)
    lg = L3[:, :, 0:4]
    le = L3[:, :, 4:36]
    mx = ar.f32(16)
    ohg = ar.f32(64).rearrange("p (a g) -> p a g", g=4)
    eg = ar.f32(64).rearrange("p (a g) -> p a g", g=4)
    se = ar.f32(16)
    psel = ar.f32(16)
    pen = ar.f32(64).rearrange("p (a g) -> p a g", g=4)
    lem = ar.f32(512).rearrange("p (a g e) -> p a g e", g=4, e=8)
    lem2 = ar.f32(512).rearrange("p (a c) -> p a c", c=32)
    m1 = ar.f32(16)
    m2 = ar.f32(16)
    E = ar.f32(1024).rearrange("p (k a c) -> p k a c", k=2, c=32)
    tot = ar.f32(1024).rearrange("p (j c) -> p j c", c=32)
    off = ar.f32(1024).rearrange("p (j c) -> p j c", c=32)
    Sm = ar.f32(1024).rearrange("p (j c) -> p j c", c=32)
    cnt = ar.f32(32)
    cnti = ar.i32(32)
    padv = ar.f32(32)
    pst = ar.f32(33)
    pend = ar.f32(32)
    destf = ar.f32(32)
    thr = ar.f32(NBLK * 32).rearrange("p (b c) -> p b c", c=32)
    cmpv = ar.f32(NBLK * 32).rearrange("p (b c) -> p b c", c=32)
    bex = ar.f32(NBLK)
    iow = ar.f32(4)
    idxf = ar.f32(NBLK * 4).rearrange("p (b q) -> p b q", q=4)
    tris = ar.f32(128)
    e21 = ar.f32(16)
    rden = ar.f32(16)
    dm(p, "sync", thr.rearrange("p b c -> p (b c)"), C["thr"], writes=["thr"])
    dm(p, "sync", iow, C["iow"], writes=["iow"])
    dm(p, "sync", tris, C["tri_s"], writes=["tris"])
    lemf = lem.rearrange("p a g e -> p a (g e)")
    red(p, "dve", mx, lg, ALU.max, AX.X, reads=["logits"], writes=["mx"])
    tt_(p, "dve", ohg, lg, bc(mx, 2, [128, NT, 4]), ALU.is_equal, reads=["logits", "mx"], writes=["ohg"])
    tt_(p, "dve", eg, lg, bc(mx, 2, [128, NT, 4]), ALU.subtract, reads=["logits", "mx"], writes=["eg"])
    act(p, eg, eg, AF.Exp, reads=["eg"], writes=["eg"])
    red(p, "dve", se, eg, ALU.add, AX.X, reads=["eg"], writes=["se"])
    p.op("dve", lambda e: e.reciprocal(out=psel, in_=se), reads=["se"], writes=["psel"])
    ts(p, "dve", pen, ohg, -1.0, 1e30, ALU.add, ALU.mult, reads=["ohg"], writes=["pen"])
    tt_(p, "dve", lem, le.rearrange("p a (g e) -> p a g e", e=8), bc(pen, 3, [128, NT, 4, 8]), ALU.add, reads=["logits", "pen"], writes=["lem"])
    red(p, "dve", m1, lemf, ALU.max, AX.X, reads=["lem"], writes=["m1"])
    tt_(p, "dve", E[:, 0], lemf, bc(m1, 2, [128, NT, 32]), ALU.is_equal, reads=["lem", "m1"], writes=["E"])
    stt(p, lem2, E[:, 0], -1e30, lemf, ALU.mult, ALU.add, reads=["E", "lem"], writes=["lem2"])
    red(p, "dve", m2, lem2, ALU.max, AX.X, reads=["lem2"], writes=["m2"])
    tt_(p, "dve", E[:, 1], lem2, bc(m2, 2, [128, NT, 32]), ALU.is_equal, reads=["lem2", "m2", "E"], writes=["E"])
    tt_(p, "dve", e21, m2, m1, ALU.subtract, reads=["m1", "m2"], writes=["e21"])
    act(p, e21, e21, AF.Exp, reads=["e21"], writes=["e21"])
    ts(p, "dve", rden, e21, 1.0, None, ALU.add, reads=["e21"], writes=["rden"])
    p.op("dve", lambda e: e.reciprocal(out=rden, in_=rden), reads=["rden"], writes=["rden"])
    tt_(p, "dve", rden, rden, psel, ALU.mult, reads=["rden", "psel"], writes=["rden"])
    cp(p, "dve", wts[:, :, 0], rden, reads=["rden"], writes=["wts"])
    tt_(p, "dve", wts[:, :, 1], rden, e21, ALU.mult, reads=["rden", "e21", "wts"], writes=["wts"])
    Ef = E.rearrange("p k a c -> p (k a c)")
    for hf in range(2):
        mm(p, ps[hf][:, :], tris, Ef[:, hf * 512:(hf + 1) * 512], True, True, reads=["tris", "E"], writes=["ps%d" % hf])
        mm(p, ps[2 + hf][:, :], onesf[:], Ef[:, hf * 512:(hf + 1) * 512], True, True, reads=["onesf", "E"], writes=["ps%d" % (2 + hf)])
        cp(p, "dve", tot.rearrange("p j c -> p (j c)")[:, hf * 512:(hf + 1) * 512], ps[2 + hf][:, :], reads=["ps%d" % (2 + hf)], writes=["tot"])
    ms(p, "dve", off[:, 0, :], 0.0, writes=["off"])
    for j in range(31):
        tt_(p, "dve", off[:, j + 1, :], off[:, j, :], tot[:, j, :], ALU.add, reads=["off", "tot"], writes=["off"])
    tt_(p, "dve", cnt, off[:, 31, :], tot[:, 31, :], ALU.add, reads=["off", "tot"], writes=["cnt"])
    ts(p, "dve", cnt, cnt, float(BLKS - 1), None, ALU.add, reads=["cnt"], writes=["cnt"])
    cp(p, "dve", cnti, cnt, reads=["cnt"], writes=["cnti"])
    p.op("dve", lambda e: e.tensor_single_scalar(out=cnti, in_=cnti, scalar=8, op=ALU.arith_shift_right), reads=["cnti"], writes=["cnti"])
    cp(p, "dve", padv, cnti, reads=["cnti"], writes=["padv"])
    ts(p, "dve", padv, padv, float(BLKS), None, ALU.mult, reads=["padv"], writes=["padv"])
    ms(p, "dve", pst[:, 0:1], 0.0, writes=["pst"])
    for e_ in range(32):
        tt_(p, "dve", pst[:, e_ + 1:e_ + 2], pst[:, e_:e_ + 1], padv[:, e_:e_ + 1], ALU.add, reads=["pst", "padv"], writes=["pst"])
    cp(p, "dve", pend, pst[:, 1:33], reads=["pst"], writes=["pend"])
    tt_(p, "dve", Sm, off, bc(pst[:, 0:32], 1, [128, 32, 32]), ALU.add, reads=["off", "pst"], writes=["Sm"])
    Smf = Sm.rearrange("p j c -> p (j c)")
    for hf in range(2):
        tt_(p, "dve", Smf[:, hf * 512:(hf + 1) * 512], ps[hf][:, :], Smf[:, hf * 512:(hf + 1) * 512], ALU.add, reads=["ps%d" % hf, "Sm"], writes=["Sm"])
    tt_(p, "dve", Smf, Smf, Ef, ALU.mult, reads=["Sm", "E"], writes=["Sm"])
    red(p, "dve", destf, Sm, ALU.add, AX.X, reads=["Sm"], writes=["destf"])
    cp(p, "dve", desti, destf, reads=["destf"], writes=["desti"])
    tt_(p, "dve", cmpv, thr, bc(pend, 1, [128, NBLK, 32]), ALU.is_ge, reads=["thr", "pend"], writes=["cmpv"])
    red(p, "dve", bex, cmpv, ALU.add, AX.X, reads=["cmpv"], writes=["bex"])
    bigt = ar.f32(NBLK)
    ts(p, "dve", bigt, bex, 31.5, (1.0e6 if SKIP_UNUSED else 0.0), ALU.is_ge, ALU.mult, reads=["bex"], writes=["bigt"])
    ts(p, "dve", bex, bex, 31.0, None, ALU.min, reads=["bex", "bigt"], writes=["bex"])
    cp(p, "dve", idxf, bc(iow, 1, [128, NBLK, 4]), reads=["iow"], writes=["idxf"])
    stt(p, idxf, bc(bex, 2, [128, NBLK, 4]), 512.0, idxf, ALU.mult, ALU.add, reads=["bex", "idxf"], writes=["idxf"])
    ts(p, "dve", idxf, idxf, float(wl["_l"] * 16384), None, ALU.add, reads=["idxf"], writes=["idxf"])
    cp(p, "dve", idxW2, idxf, reads=["idxf"], writes=["idxW2"])
    tt_(p, "dve", idxf, idxf, bc(bigt, 2, [128, NBLK, 4]), ALU.add, reads=["bigt", "idxf", "idxW2"], writes=["idxf"])
    cp(p, "dve", idxW, idxf, reads=["idxf"], writes=["idxW"])

    p.barrier()
    ar.off = mark
    xb = [ar.bf(D) for _ in range(2)]
    if first:
        ms(p, "pool", xb[0], 0.0, writes=[("xb", 0)])
        for b in range(NSLOT // 128):
            dm(p, "sync", XS[b * 128:(b + 1) * 128, :], xb[0], reads=[("xb", 0)], writes=["XS"])
        p.barrier()
    for tile in range(NT):
        b = tile % 2
        p.dma("pool", lambda e, tile=tile, b=b: e.dma_start(out=xb[b], in_=XA[tile * 128:(tile + 1) * 128, :]), writes=[("xb", b)])
        for k in range(2):
            j = k * NT + tile
            p.dma("pool", lambda e, j=j, b=b: e.indirect_dma_start(
                out=XS[:, :], out_offset=IOA(ap=desti[:, j:j + 1], axis=0), in_=xb[b], in_offset=None),
                reads=[("xb", b), "desti"], writes=["XSs"])
    p.barrier()
    ar.off = mark
    w1b = [ar.bf(16 * 512).rearrange("p (j f) -> p j f", f=512) for _ in range(2)]
    w3b = [ar.bf(16 * 512).rearrange("p (j f) -> p j f", f=512) for _ in range(2)]
    w2b = [ar.bf(4 * D).rearrange("p (j c) -> p j c", c=D) for _ in range(2)]
    xs = [ar.bf(D) for _ in range(2)]
    XsT = [ar.bf(16 * 128).rearrange("p (j s) -> p j s", s=128) for _ in range(2)]
    hs = [ar.f32(512) for _ in range(2)]
    Hb = [ar.bf(512) for _ in range(2)]
    HT = [ar.bf(512).rearrange("p (j s) -> p j s", s=128) for _ in range(2)]
    ybs = [ar.f32(D) for _ in range(2)]
    W1 = wl["_W"]["w_exp_gate"].rearrange("l e (r a) f -> (l e r) (a f)", a=4)
    W3 = wl["_W"]["w_exp_up"].rearrange("l e (r a) f -> (l e r) (a f)", a=4)
    W2 = wl["_W"]["w_exp_down"].rearrange("l e f c -> (l e f) c")
    nrows = int(W1.shape[0])
    _rh = {}

    def bkw_(e):
        if not SKIP_UNUSED:
            return {}
        if "r" not in _rh:
            r = e.alloc_register("moe_bound_%d" % wl["_l"])
            e.reg_mov(r, nrows - 1)
            _rh["r"] = r
        return dict(bounds_check=_rh["r"], oob_is_err=False)
    for b in range(NBLK):
        s2 = b % 2
        for q in range(4):
            p.dma("pool", lambda e, b=b, q=q, s2=s2: e.indirect_dma_start(
                out=w1b[s2][:, 4 * q:4 * q + 4, :].rearrange("p j f -> p (j f)"), out_offset=None, in_=W1,
                in_offset=IOA(ap=idxW[:, b, q:q + 1], axis=0), **bkw_(e)), reads=["idxW"], writes=[("w1b", s2)])
            p.dma("pool", lambda e, b=b, q=q, s2=s2: e.indirect_dma_start(
                out=w3b[s2][:, 4 * q:4 * q + 4, :].rearrange("p j f -> p (j f)"), out_offset=None, in_=W3,
                in_offset=IOA(ap=idxW[:, b, q:q + 1], axis=0), **bkw_(e)), reads=["idxW"], writes=[("w3b", s2)])
        for q in range(4):
            p.dma("pool", lambda e, b=b, q=q, s2=s2: e.indirect_dma_start(
                out=w2b[s2].rearrange("p j c -> p (j c)")[:, q * D:(q + 1) * D], out_offset=None, in_=W2,
                in_offset=IOA(ap=idxW2[:, b, q:q + 1], axis=0)), reads=["idxW2"], writes=[("w2b", s2)])
        for sub in range(2):
            r0 = b * BLKS + sub * 128
            dm(p, "sync", xs[sub], XS[r0:r0 + 128, :], reads=["XSs"], writes=[("xs", sub)])
            xv = xs[sub].rearrange("s (q j) -> s j q", j=16)
            for hf in range(2):
                for jj in range(8):
                    j = hf * 8 + jj
                    trp(p, psb(hf, 1024)[:, jj * 128:(jj + 1) * 128], xv[:, j, :], identb[:], reads=[("xs", sub), "identb"], writes=["ps%d" % hf])
                cp(p, "act" if hf == 0 else "dve", XsT[sub][:, hf * 8:(hf + 1) * 8, :].rearrange("p j s -> p (j s)"), psb(hf, 1024),
                   reads=["ps%d" % hf], writes=[("XsT", sub)])
        for sub in range(2):
            for j in range(16):
                mm(p, ps[2][:, :], XsT[sub][:, j, :], w1b[s2][:, j, :], j == 0, j == 15, reads=[("XsT", sub), ("w1b", s2)], writes=["ps2"])
            for j in range(16):
                mm(p, ps[3][:, :], XsT[sub][:, j, :], w3b[s2][:, j, :], j == 0, j == 15, reads=[("XsT", sub), ("w3b", s2)], writes=["ps3"])
            act(p, hs[sub], ps[2][:, :], AF.Silu, reads=["ps2"], writes=[("hs", sub)])
            tt_(p, "dve", Hb[sub], ps[3][:, :], hs[sub], ALU.mult, reads=["ps3", ("hs", sub)], writes=[("Hb", sub)])
        for sub in range(2):
            hv = Hb[sub].rearrange("s (q j) -> s j q", j=4)
            for j in range(4):
                trp(p, psb(sub, 1024)[:, j * 128:(j + 1) * 128], hv[:, j, :], identb[:], reads=[("Hb", sub), "identb"], writes=["ps%d" % sub])
            cp(p, "act", HT[sub].rearrange("p j s -> p (j s)"), psb(sub, 1024)[:, 0:512], reads=["ps%d" % sub], writes=[("HT", sub)])
        for sub in range(2):
            r0 = b * BLKS + sub * 128
            for oc in range(4):
                for j in range(4):
                    mm(p, ps[4 + oc][:, :], HT[sub][:, j, :], w2b[s2][:, j, oc * 512:(oc + 1) * 512], j == 0, j == 3, reads=[("HT", sub), ("w2b", s2)], writes=["ps%d" % (4 + oc)])
                cp(p, "act" if oc % 2 == 0 else "dve", ybs[sub][:, oc * 512:(oc + 1) * 512], ps[4 + oc][:, :], reads=["ps%d" % (4 + oc)], writes=[("ybs", sub)])
            dm(p, "sync", YB[r0:r0 + 128, :], ybs[sub], reads=[("ybs", sub)], writes=["YB"])
    p.barrier()
    ar.off = mark
    lng = ar.f32(D)
    lnb = ar.f32(D)
    y0 = [ar.f32(D) for _ in range(2)]
    y1 = [ar.f32(D) for _ in range(2)]
    x1 = [ar.f32(D) for _ in range(2)]
    hb2 = [ar.bf(D) for _ in range(2)]
    stats = ar.f32(24)
    mv = ar.f32(4)
    bcast_load(lng, "lng", wl["ln2_g"], D)
    bcast_load(lnb, "lnb", wl["ln2_b"], D)
    for tile in range(NT):
        b = tile % 2
        rs = slice(tile * 128, (tile + 1) * 128)
        for k, yk in ((0, y0), (1, y1)):
            j = k * NT + tile
            p.dma("pool", lambda e, j=j, yk=yk, b=b: e.indirect_dma_start(
                out=yk[b], out_offset=None, in_=YB[:, :], in_offset=IOA(ap=desti[:, j:j + 1], axis=0)),
                reads=["desti", "YB"], writes=[("y%d" % k, b)])
        dm(p, "sync", x1[b], XA[rs, :], writes=[("x1", b)])
        ts(p, "dve", y0[b], y0[b], wts[:, tile, 0:1], None, ALU.mult, reads=[("y0", b), "wts"], writes=[("y0", b)])
        stt(p, y0[b], y1[b], wts[:, tile, 1:2], y0[b], ALU.mult, ALU.add, reads=[("y0", b), ("y1", b), "wts"], writes=[("y0", b)])
        stt(p, x1[b], x1[b], ALPHA, y0[b], ALU.mult, ALU.add, reads=[("y0", b), ("x1", b)], writes=[("x1", b)])
        layer_norm_rows(x1[b], ("x1", b), x1[b], ("x1", b), D, lng, lnb, ("lng", "lnb"), stats, mv, "ln2")
        dm(p, "sync", dst[rs, :], x1[b], reads=[("x1", b)], writes=["dst"])
        if not last:
            tile_to_xT(x1[b], ("x1", b), tile, 0, hb2[b], ("hb2", b))


DEPTH = 4
_NC_CACHE = {}


def kernel(**inputs):
    x = np.asarray(inputs["x"], dtype=np.float32)
    weights = {k: inputs[k] for k in WEIGHT_SHAPES if k in inputs}
    consts = host_consts()
    if "nc" not in _NC_CACHE:
        _NC_CACHE["nc"] = build(DEPTH)
    nc = _NC_CACHE["nc"]
    base = make_in_map(x[0], weights, consts)
    in_maps = []
    for c in range(8):
        m = dict(base)
        m["x"] = np.ascontiguousarray(x[c])
        in_maps.append(m)
    res = run_bass_kernel_spmd(nc, in_maps, core_ids=list(range(8)))
    return np.stack([np.asarray(r["y"], dtype=np.float32) for r in res.results], axis=0)
```

```python
import contextlib
import math
import os
import numpy as np
import concourse.bass as bass
import concourse.mybir as mybir
from concourse.bass_utils import run_bass_kernel_spmd

F32 = mybir.dt.float32
BF16 = mybir.dt.bfloat16
I32 = mybir.dt.int32
AF = mybir.ActivationFunctionType
ALU = mybir.AluOpType
AX = mybir.AxisListType

S = 2048
D = 2048
NT = 16
D_IN = 7696
OFF_MQ, OFF_MK, OFF_MV, OFF_MO, OFF_MG = 0, 256, 512, 1024, 1536
OFF_AQ, OFF_AK, OFF_AV = 1552, 3088, 4624
OFF_SU, OFF_SV, OFF_PP = 6160, 6672, 7184
ALPHA = 8.0 ** 0.25
LN_EPS = 1e-5
ATT_R = (1, 2, 8)
ATT_DIL = (1, 4, 16)
NBLK = 48
BLKS = 256
NSLOT = NBLK * BLKS
ARN = 71000

ENGS = ("sync", "act", "dve", "pool", "pe")
N_DMA_SEMS = 16


class Prog:
    def __init__(self, nc):
        self.nc = nc
        self.ops = {e: [] for e in ENGS}
        self.cnt = {e: 0 for e in ENGS}
        self.dcnt = {e: 0 for e in ENGS}
        self.res = {}
        self.last = {}
        self.bar = {e: set() for e in ENGS}

    def _deps(self, eng, reads, writes):
        deps = set(self.bar[eng])
        self.bar[eng] = set()
        for k in reads:
            r = self.res.get(k)
            if r and r[0] is not None:
                deps.add(r[0])
        for k in writes:
            r = self.res.get(k)
            if r:
                if r[0] is not None:
                    deps.add(r[0])
                deps.update(r[1])
        return deps

    def _commit(self, tok, reads, writes):
        self.last[tok[0]] = tok
        for k in reads:
            r = self.res.setdefault(k, [None, []])
            r[1].append(tok)
        for k in writes:
            self.res[k] = [tok, []]

    def op(self, eng, fn, reads=(), writes=()):
        deps = self._deps(eng, reads, writes)
        self.cnt[eng] += 1
        tok = (("c", eng), self.cnt[eng], eng)
        self.ops[eng].append((fn, deps, tok, 1))
        self._commit(tok, reads, writes)
        return tok

    def dma(self, eng, fn, reads=(), writes=()):
        deps = self._deps(eng, reads, writes)
        i = self.dcnt[eng]
        self.dcnt[eng] += 1
        tok = (("d", eng, i % N_DMA_SEMS), 16 * (i // N_DMA_SEMS + 1), "dma_" + eng)
        prev = self.last.get(tok[0])
        if prev is not None:
            deps.add(prev)
        self.ops[eng].append((fn, deps, tok, 16))
        self._commit(tok, reads, writes)
        return tok

    def barrier(self):
        toks = set(self.last.values())
        for e in ENGS:
            self.bar[e] |= toks
        self.res = {}

    def emit(self):
        nc = self.nc
        finals = set(self.last.values())
        with contextlib.ExitStack() as st:
            sems = {}
            for e in ("act", "dve", "pool", "pe"):
                sems[("c", e)] = st.enter_context(nc.semaphore("c_" + e))
            for e in ("sync", "act", "pool"):
                for j in range(N_DMA_SEMS):
                    sems[("d", e, j)] = st.enter_context(nc.semaphore("d_%s_%d" % (e, j)))
            block = st.enter_context(nc.Block())
            ops = self.ops

            def run(engname, engobj, fin=()):
                waited = {}

                def waits(deps):
                    for (sk, val, deng) in sorted(deps, key=str):
                        if deng == "pe" and engname == "pe":
                            continue
                        if waited.get(sk, 0) >= val:
                            continue
                        engobj.wait_ge(sems[sk], val)
                        waited[sk] = val

                for fn, deps, tok, inc in ops[engname]:
                    waits(deps)
                    ins = fn(engobj)
                    ins.then_inc(sems[tok[0]], inc)
                waits([f for f in fin if not (f[2] == "pe" and engname == "pe")])

            @block.sync
            def _(sync):
                run("sync", sync, finals)

            @block.scalar
            def _(scalar):
                run("act", scalar)

            @block.vector
            def _(vector):
                run("dve", vector)

            @block.gpsimd
            def _(gpsimd):
                run("pool", gpsimd)

            @block.tensor
            def _(tensor):
                run("pe", tensor)


STOP = [None]
SKIP_UNUSED = bool(int(os.environ.get("SKIP_UNUSED", "1")))


def alibi_slope(g, h):
    n = 24
    return 2.0 ** (-8.0 * (g * 8 + h + 1) / n)


def host_consts():
    c = {}
    c["ident"] = np.eye(128, dtype=np.float32)
    s = np.arange(128)[:, None]
    t = np.arange(128)[None, :]
    c["tri_f"] = (s <= t).astype(np.float32)
    c["tri_b"] = (s >= t).astype(np.float32)
    c["mneg_f"] = np.where(s <= t, 0.0, -30000.0).astype(np.float32)
    c["mneg_b"] = np.where(s >= t, 0.0, -30000.0).astype(np.float32)
    c["tri_s"] = (s < t).astype(np.float32)
    c["ones"] = np.ones((128, 128), np.float32)
    tiles = []
    for g in range(3):
        dil = ATT_DIL[g]
        for o in range(-ATT_R[g], ATT_R[g] + 1):
            delta = (t - s) - 128 * o
            ok = (np.abs(delta) <= 64 * dil) & (delta % dil == 0)
            tiles.append(np.where(ok, np.abs(delta).astype(np.float32), 1e5))
    c["dist"] = np.ascontiguousarray(np.stack(tiles, axis=1).astype(np.float32))
    pe = np.zeros((128, 4, 2, 8), np.float32)
    for g, w in enumerate((2, 4, 8, 16)):
        h = w // 2
        for j in range(h):
            tt = j
            pe[:, g, 0, j] = 1.0 / (min(tt + h, S) - max(tt - h, 0))
            tt = S - h + j
            pe[:, g, 1, j] = 1.0 / (min(tt + h, S) - max(tt - h, 0))
    c["pool_edge"] = pe.reshape(128, 64)
    thr = np.zeros((128, NBLK, 32), np.float32)
    thr[:] = (float(BLKS) * np.arange(NBLK))[None, :, None]
    c["thr"] = thr.reshape(128, NBLK * 32)
    io = np.zeros((128, 4), np.float32)
    io[:] = 4.0 * np.arange(128)[:, None] + np.arange(4)[None, :]
    c["iow"] = io
    return c


CONST_SHAPES = {"ident": [128, 128], "tri_f": [128, 128], "tri_b": [128, 128], "mneg_f": [128, 128],
                "mneg_b": [128, 128], "tri_s": [128, 128], "ones": [128, 128], "dist": [128, 25, 128],
                "pool_edge": [128, 64], "thr": [128, NBLK * 32], "iow": [128, 4]}

WEIGHT_SHAPES = {
    "w_in": [D, D_IN], "ml_conv_w": [3, 512], "ml_gate_b": [16], "ml_norm_w": [512],
    "sg_ln_g": [512], "sg_ln_b": [512], "sg_w": [4, 128, 128], "sg_b": [4, 128],
    "pool_w": [4, 128, 128], "pool_scale": [512], "w_gate": [D, 4 * D], "b_gate": [4 * D],
    "w_branch": [4, 512, D], "w_out": [D, D], "ln1_g": [D], "ln1_b": [D],
    "w_router_group": [D, 4], "b_router_group": [4], "w_router_expert": [D, 32], "b_router_expert": [32],
    "w_exp_gate": [32, D, 512], "w_exp_up": [32, D, 512], "w_exp_down": [32, 512, D],
    "ln2_g": [D], "ln2_b": [D],
    "w_router": [128, 16 * 36], "b_router": [36],
}


def build(depth, stop_after=None, dbg=False):
    nc = bass.Bass("TRN2", target_bir_lowering=False)
    x_in = nc.dram_tensor("x", [S, D], F32, kind="ExternalInput").ap()
    W = {k: nc.dram_tensor(k, [depth] + v, F32, kind="ExternalInput").ap() for k, v in WEIGHT_SHAPES.items()}
    C = {k: nc.dram_tensor("c_" + k, v, F32, kind="ExternalInput").ap() for k, v in CONST_SHAPES.items()}
    y_out = nc.dram_tensor("y", [S, D], F32, kind="ExternalOutput").ap()
    okind = "ExternalOutput" if dbg else "Internal"
    XA = nc.dram_tensor("XA", [S, D], F32, kind=okind).ap()
    XB = nc.dram_tensor("XB", [S, D], F32, kind="Internal").ap()
    YST = nc.dram_tensor("YST", [4, 4, 128, S], BF16, kind=okind).ap()
    MT = nc.dram_tensor("MT", [16, 128, S], BF16, kind="Internal").ap()
    XS = nc.dram_tensor("XS", [NSLOT, D], BF16, kind="Internal").ap()
    YB = nc.dram_tensor("YB", [NSLOT, D], F32, kind="Internal").ap()

    st = contextlib.ExitStack()
    with st:
        def sb(name, shape, dt):
            return st.enter_context(nc.sbuf_tensor(name, shape, dt))

        xT = sb("xT", [128, 16, S], BF16)
        identb = sb("identb", [128, 128], BF16)
        identf = sb("identf", [128, 128], F32)
        onesf = sb("onesf", [128, 128], F32)
        AR = sb("AR", [128, ARN], BF16)
        logits = sb("logits", [128, NT * 36], F32)
        ps = [st.enter_context(nc.psum_tensor("ps%d" % i, [128, 512], F32)) for i in range(8)]
        p = Prog(nc)

        class Arena:
            def __init__(self):
                self.off = 0

            def reset(self):
                self.off = 0

            def f32(self, n):
                o = (self.off + 15) // 16 * 16
                self.off = o + 2 * n
                assert self.off <= ARN, self.off
                return AR[:, o:o + 2 * n].bitcast(F32)

            def bf(self, n):
                o = (self.off + 15) // 16 * 16
                self.off = o + n
                assert self.off <= ARN, self.off
                return AR[:, o:o + n]

            def i32(self, n):
                o = (self.off + 15) // 16 * 16
                self.off = o + 2 * n
                assert self.off <= ARN, self.off
                return AR[:, o:o + 2 * n].bitcast(I32)

        ar = Arena()

        def psb(i, n=512):
            return ps[i][:, 0:n // 2].bitcast(BF16)

        p.dma("pool", lambda e: e.dma_start(out=identb[:], in_=C["ident"]), writes=["identb"])
        p.dma("sync", lambda e: e.dma_start(out=identf[:], in_=C["ident"]), writes=["identf"])
        p.dma("sync", lambda e: e.dma_start(out=onesf[:], in_=C["ones"]), writes=["onesf"])

        def tile_to_xT(src, src_key, tt, pbank, hb, hb_key, lo=None):
            cp(p, "act", hb, src, reads=[src_key], writes=[hb_key])
            for half in range(2):
                bank = pbank + half
                pk = "ps%d" % bank
                for jj in range(8):
                    dc = half * 8 + jj
                    trp(p, psb(bank, 1024)[:, jj * 128:(jj + 1) * 128], hb[:, dc * 128:(dc + 1) * 128], identb[:],
                        reads=[hb_key, "identb"], writes=[pk])
                cp(p, "act" if half == 0 else "dve", xT[:, half * 8:(half + 1) * 8, tt * 128:(tt + 1) * 128],
                   psb(bank, 1024).rearrange("p (a b) -> p a b", b=128), reads=[pk], writes=[("xT", tt)])
            if lo is not None:
                lob, lob_key, loT, loT_key = lo
                tt_(p, "dve", lob, src, hb, ALU.subtract, reads=[src_key, hb_key], writes=[lob_key])
                for half in range(2):
                    bank = pbank + half
                    pk = "ps%d" % bank
                    for jj in range(8):
                        dc = half * 8 + jj
                        trp(p, psb(bank, 1024)[:, jj * 128:(jj + 1) * 128], lob[:, dc * 128:(dc + 1) * 128], identb[:],
                            reads=[lob_key, "identb"], writes=[pk])
                    cp(p, "act" if half == 0 else "dve", loT[:, half * 8:(half + 1) * 8, :],
                       psb(bank, 1024).rearrange("p (a b) -> p a b", b=128), reads=[pk], writes=[loT_key])

        XT_ALL = [("xT", tt) for tt in range(NT)]

        def load_w_fm(dst, key, wap, lo, ncols):
            p.dma("pool", lambda e: e.dma_start(
                out=dst, in_=wap[:, lo:lo + ncols].rearrange("(dc q) c -> q dc c", q=128)), writes=[key])

        def bcast_load(dst, key, vec_ap, n):
            for c0 in range(0, n, 512):
                c1 = min(n, c0 + 512)
                p.dma("sync", lambda e, c0=c0, c1=c1: e.dma_start(out=dst[:, c0:c1], in_=vec_ap[c0:c1].partition_broadcast(128)), writes=[key])

        def layer_norm_rows(src, src_key, dst, dst_key, n, gt, bt, gb_keys, stats, mv, tmpk):
            nch = max(1, n // 512)
            w = n // nch
            for c in range(nch):
                p.op("dve", lambda e, c=c: e.bn_stats(out=stats[:, c * 6:(c + 1) * 6], in_=src[:, c * w:(c + 1) * w]),
                     reads=[src_key], writes=[tmpk + "st"])
            p.op("dve", lambda e: e.bn_aggr(out=mv[:, 0:2], in_=stats[:, 0:nch * 6]), reads=[tmpk + "st"], writes=[tmpk + "mv"])
            p.op("act", lambda e: e.activation(out=mv[:, 2:3], in_=mv[:, 1:2], func=AF.Sqrt, bias=LN_EPS),
                 reads=[tmpk + "mv"], writes=[tmpk + "sd"])
            p.op("dve", lambda e: e.reciprocal(out=mv[:, 3:4], in_=mv[:, 2:3]), reads=[tmpk + "sd"], writes=[tmpk + "rs"])
            p.op("dve", lambda e: e.tensor_scalar(out=dst, in0=src, scalar1=mv[:, 0:1], scalar2=mv[:, 3:4],
                                                  op0=ALU.subtract, op1=ALU.mult),
                 reads=[src_key, tmpk + "mv", tmpk + "rs"], writes=[dst_key])
            if gt is not None:
                p.op("dve", lambda e: e.tensor_tensor(out=dst, in0=dst, in1=gt, op=ALU.mult),
                     reads=[dst_key, gb_keys[0]], writes=[dst_key])
                p.op("dve", lambda e: e.tensor_tensor(out=dst, in0=dst, in1=bt, op=ALU.add),
                     reads=[dst_key, gb_keys[1]], writes=[dst_key])

        cur_x = x_in
        for l in range(depth):
            last = (l == depth - 1)
            wl = {k: v[l] for k, v in W.items()}
            wl["_l"] = l
            wl["_W"] = W
            if l == 0:
                p.barrier()
                ar.reset()
                xt_tiles = [ar.f32(2048) for _ in range(2)]
                hbA = [ar.bf(2048) for _ in range(2)]
                for tt in range(NT):
                    b = tt % 2
                    p.dma("sync", lambda e, tt=tt, b=b, cx=cur_x: e.dma_start(out=xt_tiles[b], in_=cx[tt * 128:(tt + 1) * 128, :]),
                          writes=[("xld", b)])
                    tile_to_xT(xt_tiles[b], ("xld", b), tt, 0, hbA[b], ("hbA", b))

            if stop_after == "A":
                break
            STOP[0] = stop_after
            stage_mlstm(nc, p, ar, ps, psb, xT, XT_ALL, identb, identf, onesf, wl, C, YST, load_w_fm, bcast_load,
                        layer_norm_rows)
            if stop_after in ("mlstm", "ml1", "ml2"):
                break
            stage_attn(nc, p, ar, ps, psb, xT, XT_ALL, identb, wl, C, YST, load_w_fm)
            if stop_after == "attn":
                break
            stage_sg(nc, p, ar, ps, psb, xT, XT_ALL, identb, identf, wl, C, YST, load_w_fm, bcast_load, layer_norm_rows)
            stage_pool(nc, p, ar, ps, psb, xT, XT_ALL, wl, C, YST, load_w_fm)
            if stop_after == "branches":
                break
            stage_merge(nc, p, ar, ps, psb, xT, XT_ALL, identf, wl, C, YST, MT, cur_x, XA, load_w_fm, bcast_load,
                        layer_norm_rows, tile_to_xT, logits=(None if os.environ.get("NOROUTER") else logits))
            if stop_after in ("ln1", "mg1"):
                break
            dst = y_out if last else XB
            stage_moe(nc, p, ar, ps, psb, xT, XT_ALL, identb, identf, onesf, wl, C, XA, XS, YB, dst, bcast_load,
                      layer_norm_rows, tile_to_xT, logits, first=(l == 0), last=last)
            cur_x = XB
        p.barrier()
        p.emit()
    return nc


def mm(p, out, lhsT, rhs, start=True, stop=True, reads=(), writes=()):
    return p.op("pe", lambda e: e.matmul(out, lhsT=lhsT, rhs=rhs, start=start, stop=stop), reads, writes)


def trp(p, out, in_, ident, reads=(), writes=()):
    return p.op("pe", lambda e: e.transpose(out=out, in_=in_, identity=ident), reads, writes)


def act(p, out, in_, func, reads=(), writes=(), bias=None, scale=None):
    kw = {}
    if bias is not None:
        kw["bias"] = bias
    if scale is not None:
        kw["scale"] = scale
    return p.op("act", lambda e: e.activation(out=out, in_=in_, func=func, **kw), reads, writes)


def ts(p, eng, out, in0, s1, s2, op0, op1=None, reads=(), writes=()):
    if op1 is None:
        return p.op(eng, lambda e: e.tensor_scalar(out=out, in0=in0, scalar1=s1, scalar2=None, op0=op0), reads, writes)
    return p.op(eng, lambda e: e.tensor_scalar(out=out, in0=in0, scalar1=s1, scalar2=s2, op0=op0, op1=op1), reads, writes)


def stt(p, out, in0, scalar, in1, op0, op1, reads=(), writes=()):
    return p.op("dve", lambda e: e.scalar_tensor_tensor(out=out, in0=in0, scalar=scalar, in1=in1, op0=op0, op1=op1),
                reads, writes)


def tt_(p, eng, out, in0, in1, op, reads=(), writes=()):
    return p.op(eng, lambda e: e.tensor_tensor(out=out, in0=in0, in1=in1, op=op), reads, writes)


def cp(p, eng, out, in_, reads=(), writes=()):
    if eng == "act":
        return p.op("act", lambda e: e.activation(out=out, in_=in_, func=AF.Copy), reads, writes)
    return p.op(eng, lambda e: e.tensor_copy(out=out, in_=in_), reads, writes)


def ms(p, eng, ap, val, writes=()):
    return p.op(eng, lambda e: e.memset(ap, val), (), writes)


def dm(p, eng, out, in_, reads=(), writes=(), **kw):
    return p.dma(eng, lambda e: e.dma_start(out=out, in_=in_, **kw), reads, writes)


def xt_keys(t0, n):
    return [("xT", t) for t in range(t0, t0 + n)]


def proj_fm(p, ps, xT, wt, wkey, ncols, evac):
    for q in range(4):
        b = q % 2
        pk = "ps%d" % b
        for dc in range(16):
            mm(p, ps[b][0:ncols, :], wt[:, dc, :], xT[:, dc, q * 512:(q + 1) * 512], dc == 0, dc == 15,
               reads=[wkey] + xt_keys(q * 4, 4), writes=[pk])
        evac(q, ps[b][0:ncols, :], pk)


def proj_tm(p, ps, xT, wt, wkey, ncols, tile, bank):
    pk = "ps%d" % bank
    for dc in range(16):
        mm(p, ps[bank][:, 0:ncols], xT[:, dc, tile * 128:(tile + 1) * 128], wt[:, dc, :], dc == 0, dc == 15,
           reads=[wkey, ("xT", tile)], writes=[pk])
    return ps[bank][:, 0:ncols], pk


def stage_mlstm(nc, p, ar, ps, psb, xT, XT_ALL, identb, identf, onesf, wl, C, YST, load_w_fm, bcast_load,
                layer_norm_rows):
    p.barrier()
    ar.reset()
    w_in = wl["w_in"]
    qkT = ar.bf(4 * S).rearrange("p (c t) -> p c t", t=S)
    ktok = ar.bf(NT * 256).rearrange("p (a b) -> p a b", b=256)
    vaug = ar.bf(NT * 4 * 129).rearrange("p (a h v) -> p a h v", h=4, v=129)
    convw = ar.f32(12)
    gb = ar.f32(16)
    normw = ar.f32(512)
    tri = [ar.f32(128), ar.f32(128)]
    mneg = [ar.f32(128), ar.f32(128)]
    gtok = ar.f32(NT * 16).rearrange("p (a b) -> p a b", b=16)
    G = {k: ar.f32(128) for k in ("lf", "ig", "b", "tot", "imb", "wk", "dec", "ebt", "tmp")}
    mark = ar.off
    for j in range(3):
        dm(p, "sync", convw[:, j * 4:(j + 1) * 4], wl["ml_conv_w"][j].rearrange("(c q) -> q c", q=128),
           writes=["convw"], allow_slow_non_contiguous=True)
    bcast_load(gb, "gb", wl["ml_gate_b"], 16)
    bcast_load(normw, "normw", wl["ml_norm_w"], 512)
    dm(p, "sync", tri[0], C["tri_f"], writes=["tri0"])
    dm(p, "sync", tri[1], C["tri_b"], writes=["tri1"])
    dm(p, "sync", mneg[0], C["mneg_f"], writes=["mneg0"])
    dm(p, "sync", mneg[1], C["mneg_b"], writes=["mneg1"])
    wqk = ar.bf(16 * 128).rearrange("p (a b) -> p a b", b=128)
    zqk = ar.f32(2050)
    ctmp = ar.f32(2048)
    wv = ar.bf(16 * 512).rearrange("p (a b) -> p a b", b=512)
    wg = ar.bf(16 * 16).rearrange("p (a b) -> p a b", b=16)
    ms(p, "pool", zqk[:, 0:1], 0.0, writes=["zqk"])
    ms(p, "pool", zqk[:, 2049:2050], 0.0, writes=["zqk"])
    for ch in range(4):
        load_w_fm(wqk, "wqk", w_in, OFF_MQ + ch * 128, 128)

        def evac(q, pap, pk):
            cp(p, "act", zqk[:, 1 + q * 512:1 + (q + 1) * 512], pap, reads=[pk], writes=["zqk"])
        proj_fm(p, ps, xT, wqk, "wqk", 128, evac)
        ts(p, "dve", ctmp, zqk[:, 0:2048], convw[:, ch:ch + 1], None, ALU.mult, reads=["zqk", "convw"], writes=["ctmp"])
        stt(p, ctmp, zqk[:, 1:2049], convw[:, 4 + ch:5 + ch], ctmp, ALU.mult, ALU.add, reads=["zqk", "convw", "ctmp"], writes=["ctmp"])
        stt(p, ctmp, zqk[:, 2:2050], convw[:, 8 + ch:9 + ch], ctmp, ALU.mult, ALU.add, reads=["zqk", "convw", "ctmp"], writes=["ctmp"])
        act(p, qkT[:, ch, :], ctmp, AF.Silu, reads=["ctmp"], writes=[("qkT", ch)])
        if ch < 2:
            ts(p, "pool", qkT[:, ch, :], qkT[:, ch, :], 0.125, None, ALU.mult, reads=[("qkT", ch)], writes=[("qkT", ch)])
    for tile in range(NT):
        b = 2 + tile % 2
        pk = "ps%d" % b
        for kc in range(2):
            trp(p, psb(b)[:, kc * 128:(kc + 1) * 128], qkT[:, 2 + kc, tile * 128:(tile + 1) * 128], identb[:],
                reads=[("qkT", 2 + kc), "identb"], writes=[pk])
        cp(p, "dve", ktok[:, tile, :], psb(b)[:, 0:256], reads=[pk], writes=["ktok"])
    load_w_fm(wv, "wv", w_in, OFF_MV, 512)
    load_w_fm(wg, "wg", w_in, OFF_MG, 16)
    ms(p, "pool", vaug[:, :, :, 128:129], 1.0, writes=["vaug"])
    for tile in range(NT):
        b = 4 + tile % 2
        pap, pk = proj_tm(p, ps, xT, wv, "wv", 512, tile, b)
        cp(p, "act", vaug[:, tile, :, 0:128], pap.rearrange("p (h v) -> p h v", v=128), reads=[pk], writes=["vaug"])
        b2 = 6 + tile % 2
        pap2, pk2 = proj_tm(p, ps, xT, wg, "wg", 16, tile, b2)
        tt_(p, "dve", gtok[:, tile, :], pap2, gb, ALU.add, reads=[pk2, "gb"], writes=["gtok"])
    gv = gtok.rearrange("p a (d y h) -> p d a y h", d=2, y=2, h=4)

    def g4(t):
        return t.rearrange("p (d a h) -> p d a h", d=2, h=4)
    cp(p, "dve", g4(G["ig"]), gv[:, :, :, 0, :], reads=["gtok"], writes=["ig"])
    act(p, g4(G["tmp"]), gv[:, :, :, 1, :], AF.Exp, reads=["gtok"], writes=["gtmp"], scale=-1.0)
    act(p, G["tmp"], G["tmp"], AF.Ln, reads=["gtmp"], writes=["gtmp"], bias=1.0)
    ts(p, "dve", G["lf"], G["tmp"], -1.0, None, ALU.mult, reads=["gtmp"], writes=["lf"])
    for d in range(2):
        mm(p, ps[0][:, d * 64:(d + 1) * 64], tri[d], G["lf"][:, d * 64:(d + 1) * 64], True, True,
           reads=["tri%d" % d, "lf"], writes=["ps0"])
    cp(p, "dve", G["b"], ps[0][:, 0:128], reads=["ps0"], writes=["gb_"])
    mm(p, ps[1][:, 0:128], onesf[:], G["lf"], True, True, reads=["onesf", "lf"], writes=["ps1"])
    cp(p, "dve", G["tot"], ps[1][:, 0:128], reads=["ps1"], writes=["tot"])
    tt_(p, "dve", G["imb"], G["ig"], G["b"], ALU.subtract, reads=["ig", "gb_"], writes=["imb"])
    tt_(p, "dve", G["tmp"], G["tot"], G["imb"], ALU.add, reads=["tot", "imb", "gtmp"], writes=["gtmp"])
    act(p, G["wk"], G["tmp"], AF.Exp, reads=["gtmp"], writes=["wk"])
    act(p, G["dec"], G["tot"], AF.Exp, reads=["tot"], writes=["dec"])
    act(p, G["ebt"], G["b"], AF.Exp, reads=["gb_"], writes=["ebt"])

    if STOP[0] == "ml1":
        return
    p.barrier()
    ar.off = mark
    hsum = ar.f32(NT * 512).rearrange("p (a h v) -> p a h v", h=4, v=128)
    mark2 = ar.off
    CT = ar.f32(8 * 129).rearrange("p (u v) -> p u v", v=129)
    CTb = ar.bf(8 * 130).rearrange("p (u v) -> p u v", v=130)
    LAGM = 2
    NS = LAGM + 1
    Rt = [ar.f32(128) for _ in range(NS)]
    AT = [ar.f32(128) for _ in range(NS)]
    ST = [ar.bf(128) for _ in range(NS)]
    nsb = [ar.f32(132) for _ in range(2)]
    ddt = [ar.f32(4) for _ in range(2)]
    kw = [[ar.bf(128) for _ in range(2)] for _ in range(2)]
    for sidx in range(2):
        for hh in range(2):
            ms(p, "pool", kw[sidx][hh], 0.0, writes=[("kw", sidx, hh)])
    units = []
    for d in range(2):
        order = list(range(NT)) if d == 0 else list(range(NT - 1, -1, -1))
        for ci, c in enumerate(order):
            for h in range(4):
                units.append((d, ci, c, h))
    units = units[:int(os.environ.get("MLU", "100000"))]

    def mfront(i):
        d, ci, c, h = units[i]
        sl = i % NS
        bD, bS = 2 * sl, 2 * sl + 1
        kD, kS = "ps%d" % bD, "ps%d" % bS
        col = d * 64 + c * 4 + h
        hh, pc = h % 2, h // 2
        rows = slice(hh * 64, (hh + 1) * 64)
        tsl = slice(c * 128, (c + 1) * 128)
        ts(p, "pool", Rt[sl], tri[d], G["lf"][:, col:col + 1], None, ALU.mult, reads=["tri%d" % d, "lf"], writes=[("Rt", sl)])
        mm(p, ps[bD][:, 0:128], onesf[:], Rt[sl], True, False, reads=["onesf", ("Rt", sl)], writes=[kD])
        mm(p, ps[bD][:, 0:128], identf[:], mneg[d], False, True, reads=["identf", "mneg%d" % d], writes=[kD])
        act(p, AT[sl], ps[bD][:, 0:128], AF.Exp, reads=[kD, "imb"], writes=[("AT", sl)], bias=G["imb"][:, col:col + 1])
        mm(p, ps[bS][:, 0:128], qkT[rows, 2 + pc, tsl], qkT[rows, pc, tsl], True, True,
           reads=[("qkT", 2 + pc), ("qkT", pc)], writes=[kS])
        tt_(p, "dve", ST[sl], ps[bS][:, 0:128], AT[sl], ALU.mult, reads=[kS, ("AT", sl)], writes=[("ST", sl)])

    def mback(i):
        d, ci, c, h = units[i]
        sl = i % NS
        s2 = i % 2
        col = d * 64 + c * 4 + h
        hh, pc = h % 2, h // 2
        rows = slice(hh * 64, (hh + 1) * 64)
        u = d * 4 + h
        tsl = slice(c * 128, (c + 1) * 128)
        mm(p, ps[6][:, 0:129], ST[sl], vaug[:, c, h, :], True, True, reads=[("ST", sl), "vaug"], writes=["ps6"])
        cp(p, "act", nsb[s2][:, 0:129], ps[6][:, 0:129], reads=["ps6"], writes=[("nsb", s2)])
        if ci > 0:
            mm(p, ps[7][:, 0:129], qkT[rows, pc, tsl], CTb[rows, u, 0:129], True, True,
               reads=[("qkT", pc), ("CTb", u)], writes=["ps7"])
            stt(p, nsb[s2][:, 0:129], ps[7][:, 0:129], G["ebt"][:, col:col + 1], nsb[s2][:, 0:129], ALU.mult, ALU.add,
                reads=["ps7", "ebt", ("nsb", s2)], writes=[("nsb", s2)])
        dd = ddt[s2]
        stt(p, dd[:, 0:1], nsb[s2][:, 128:129], -1.0, nsb[s2][:, 128:129], ALU.mult, ALU.max, reads=[("nsb", s2)], writes=[("dd", s2)])
        ts(p, "dve", dd[:, 1:2], dd[:, 0:1], 1.0, None, ALU.max, reads=[("dd", s2)], writes=[("dd", s2)])
        p.op("dve", lambda e, dd=dd: e.reciprocal(out=dd[:, 2:3], in_=dd[:, 1:2]), reads=[("dd", s2)], writes=[("dd", s2)])
        if d == 0:
            ts(p, "dve", hsum[:, c, h, :], nsb[s2][:, 0:128], dd[:, 2:3], None, ALU.mult,
               reads=[("nsb", s2), ("dd", s2)], writes=[("hsum", c, h)])
        else:
            stt(p, hsum[:, c, h, :], nsb[s2][:, 0:128], dd[:, 2:3], hsum[:, c, h, :], ALU.mult, ALU.add,
                reads=[("nsb", s2), ("dd", s2), ("hsum", c, h)], writes=[("hsum", c, h)])
        if ci < NT - 1:
            ts(p, "pool", kw[s2][hh][:, rows], ktok[:, c, h * 64:(h + 1) * 64], G["wk"][:, col:col + 1], None, ALU.mult,
               reads=["ktok", "wk"], writes=[("kw", s2, hh)])
            mm(p, ps[6][:, 256:385], kw[s2][hh], vaug[:, c, h, :], True, True, reads=[("kw", s2, hh), "vaug"], writes=["ps6"])
            if ci == 0:
                cp(p, "dve", CT[rows, u, :], ps[6][rows, 256:385], reads=["ps6"], writes=[("CT", u)])
            else:
                stt(p, CT[rows, u, :], CT[rows, u, :], G["dec"][rows, col:col + 1], ps[6][rows, 256:385], ALU.mult, ALU.add,
                    reads=["ps6", "dec", ("CT", u)], writes=[("CT", u)])
            cp(p, "act", CTb[rows, u, 0:129], CT[rows, u, :], reads=[("CT", u)], writes=[("CTb", u)])

    nu_ = len(units)
    for i in range(nu_ + LAGM):
        if i < nu_:
            mfront(i)
        if i >= LAGM:
            mback(i - LAGM)
    if STOP[0] == "ml2":
        return
    p.barrier()
    ar.off = mark2
    wo = ar.bf(16 * 512).rearrange("p (a b) -> p a b", b=512)
    yT = ar.bf(4 * S).rearrange("p (c t) -> p c t", t=S)
    osig = [ar.f32(512) for _ in range(2)]
    hn = [ar.f32(512) for _ in range(2)]
    ybf = [ar.bf(512) for _ in range(2)]
    stats = ar.f32(24)
    mv = ar.f32(16)
    load_w_fm(wo, "wo", w_in, OFF_MO, 512)
    for tile in range(NT):
        s2 = tile % 2
        pap, pk = proj_tm(p, ps, xT, wo, "wo", 512, tile, s2)
        act(p, osig[s2], pap, AF.Sigmoid, reads=[pk], writes=[("osig", s2)])
        for h in range(4):
            layer_norm_rows(hsum[:, tile, h, :], ("hsum", tile, h), hn[s2][:, h * 128:(h + 1) * 128], ("hn", s2), 128,
                            None, None, None, stats[:, h * 6:(h + 1) * 6], mv[:, h * 4:(h + 1) * 4], "mlln%d" % h)
        tt_(p, "pool", hn[s2], hn[s2], normw, ALU.mult, reads=[("hn", s2), "normw"], writes=[("hn", s2)])
        tt_(p, "pool", ybf[s2], hn[s2], osig[s2], ALU.mult, reads=[("hn", s2), ("osig", s2)], writes=[("ybf", s2)])
        b = 2 + s2
        pk2 = "ps%d" % b
        for c4 in range(4):
            trp(p, psb(b)[:, c4 * 128:(c4 + 1) * 128], ybf[s2][:, c4 * 128:(c4 + 1) * 128], identb[:],
                reads=[("ybf", s2), "identb"], writes=[pk2])
        cp(p, "act", yT[:, :, tile * 128:(tile + 1) * 128], psb(b)[:, 0:512].rearrange("p (c t) -> p c t", t=128),
           reads=[pk2], writes=["yT"])
    for c4 in range(4):
        dm(p, "sync", YST[0, c4], yT[:, c4, :], reads=["yT"], writes=["YST0"])


def stage_attn(nc, p, ar, ps, psb, xT, XT_ALL, identb, wl, C, YST, load_w_fm):
    p.barrier()
    ar.reset()
    w_in = wl["w_in"]
    dist = ar.f32(25 * 128).rearrange("p (a b) -> p a b", b=128)
    dm(p, "sync", dist, C["dist"], writes=["dist"])
    qT = ar.bf(3 * S).rearrange("p (g t) -> p g t", t=S)
    kT = ar.bf(3 * S).rearrange("p (g t) -> p g t", t=S)
    vat = ar.bf(NT * 3 * 2 * 65).rearrange("p (a g h v) -> p a g h v", g=3, h=2, v=65)
    wch = [ar.bf(16 * 128).rearrange("p (a b) -> p a b", b=128) for _ in range(2)]
    wv3 = ar.bf(16 * 384).rearrange("p (a b) -> p a b", b=384)
    yT = ar.bf(S)
    LAG = 3
    NL = LAG + 2
    Lt = [ar.f32(512) for _ in range(NL)]
    Pt = [ar.bf(512) for _ in range(NL)]
    ytile = [ar.bf(128) for _ in range(2)]
    rd = [ar.f32(2) for _ in range(2)]
    base = (0, 3, 8)
    ms(p, "pool", vat[:, :, :, :, 64:65], 1.0, writes=["vat"])
    wi = 0
    for hp in range(4):
        for g in range(3):
            for which, dstT, off in (("q", qT, OFF_AQ), ("k", kT, OFF_AK)):
                wb = wch[wi % 2]
                wk_ = ("wch", wi % 2)
                wi += 1
                load_w_fm(wb, wk_, w_in, off + (g * 4 + hp) * 128, 128)

                def evac(q, pap, pk, dstT=dstT, g=g, which=which):
                    cp(p, "act", dstT[:, g, q * 512:(q + 1) * 512], pap, reads=[pk], writes=[(which + "T", g)])
                proj_fm(p, ps, xT, wb, wk_, 128, evac)
            p.dma("pool", lambda e, g=g, hp=hp: e.dma_start(
                out=wv3[:, :, g * 128:(g + 1) * 128],
                in_=w_in[:, OFF_AV + (g * 8 + 2 * hp) * 64:OFF_AV + (g * 8 + 2 * hp) * 64 + 128].rearrange("(dc q) c -> q dc c", q=128)),
                writes=["wv3"])
        for tile in range(NT):
            b = 2 + tile % 2
            pap, pk = proj_tm(p, ps, xT, wv3, "wv3", 384, tile, b)
            cp(p, "dve", vat[:, tile, :, :, 0:64], pap.rearrange("p (g h v) -> p g h v", g=3, h=2), reads=[pk], writes=["vat"])
        batches = []
        for qt in range(NT):
            for h2 in range(2):
                units = []
                for g in range(3):
                    kbs = list(range(max(0, qt - ATT_R[g]), min(NT - 1, qt + ATT_R[g]) + 1))
                    for i0 in range(0, len(kbs), 4):
                        units.append((g, kbs[i0:i0 + 4]))
                for ui, (g, kbs) in enumerate(units):
                    batches.append((qt, h2, g, kbs, ui == 0, ui == len(units) - 1))

        def front(i):
            qt, h2, g, kbs, first, last = batches[i]
            qs = slice(qt * 128, (qt + 1) * 128)
            head = 2 * hp + h2
            rows = slice(h2 * 64, (h2 + 1) * 64)
            n = len(kbs)
            sb_ = i % 4
            sk = "ps%d" % sb_
            sl = i % NL
            for j, kb in enumerate(kbs):
                mm(p, ps[sb_][:, j * 128:(j + 1) * 128], kT[rows, g, kb * 128:(kb + 1) * 128], qT[rows, g, qs], True, True,
                   reads=[("kT", g), ("qT", g)], writes=[sk])
            idx0 = base[g] + (kbs[0] - qt) + ATT_R[g]
            stt(p, Lt[sl][:, 0:n * 128], dist[:, idx0:idx0 + n, :].rearrange("p a b -> p (a b)"),
                -8.0 * alibi_slope(g, head), ps[sb_][:, 0:n * 128], ALU.mult, ALU.add,
                reads=["dist", sk], writes=[("Lt", sl)])
            act(p, Pt[sl][:, 0:n * 128], Lt[sl][:, 0:n * 128], AF.Exp, reads=[("Lt", sl)], writes=[("Pt", sl)], scale=0.125)

        def back(i):
            qt, h2, g, kbs, first, last = batches[i]
            qs = slice(qt * 128, (qt + 1) * 128)
            sl = i % NL
            ab = 6 + h2
            ak = "ps%d" % ab
            n = len(kbs)
            for j, kb in enumerate(kbs):
                mm(p, ps[ab][:, 0:65], Pt[sl][:, j * 128:(j + 1) * 128], vat[:, kb, g, h2, :], first and j == 0, last and j == n - 1,
                   reads=[("Pt", sl), "vat"], writes=[ak])
            if last:
                p.op("dve", lambda e, ab=ab, h2=h2: e.reciprocal(out=rd[h2][:, 0:1], in_=ps[ab][:, 64:65]), reads=[ak], writes=[("rd", h2)])
                ts(p, "dve", ytile[qt % 2][:, h2 * 64:(h2 + 1) * 64], ps[ab][:, 0:64], rd[h2][:, 0:1], None, ALU.mult,
                   reads=[ak, ("rd", h2)], writes=[("ytile", qt % 2)])
                if h2 == 1:
                    tb = 4 + qt % 2
                    trp(p, psb(tb)[:, 0:128], ytile[qt % 2], identb[:], reads=[("ytile", qt % 2), "identb"], writes=["ps%d" % tb])
                    cp(p, "act", yT[:, qs], psb(tb)[:, 0:128], reads=["ps%d" % tb], writes=["yTa"])

        nb_ = len(batches)
        for i in range(nb_ + LAG):
            if i < nb_:
                front(i)
            if i >= LAG:
                back(i - LAG)
        dm(p, "sync", YST[1, hp], yT, reads=["yTa"], writes=["YST1"])


def stage_sg(nc, p, ar, ps, psb, xT, XT_ALL, identb, identf, wl, C, YST, load_w_fm, bcast_load, layer_norm_rows):
    p.barrier()
    ar.reset()
    w_in = wl["w_in"]
    wu = ar.bf(16 * 512).rearrange("p (a b) -> p a b", b=512)
    wv = ar.bf(16 * 512).rearrange("p (a b) -> p a b", b=512)
    lng = ar.f32(512)
    lnb = ar.f32(512)
    wsf = ar.f32(512).rearrange("p (g s) -> p g s", s=128)
    wsT = ar.bf(512).rearrange("p (g t) -> p g t", t=128)
    bs = ar.f32(4)
    yT = ar.bf(4 * S).rearrange("p (c t) -> p c t", t=S)
    u = [ar.f32(512) for _ in range(2)]
    v = [ar.f32(512) for _ in range(2)]
    vn = [ar.bf(512) for _ in range(2)]
    ysg = [ar.bf(512) for _ in range(2)]
    stats = ar.f32(8)
    mv = ar.f32(4)
    load_w_fm(wu, "wu", w_in, OFF_SU, 512)
    load_w_fm(wv, "wv", w_in, OFF_SV, 512)
    bcast_load(lng, "lng", wl["sg_ln_g"], 512)
    bcast_load(lnb, "lnb", wl["sg_ln_b"], 512)
    dm(p, "sync", wsf, wl["sg_w"].rearrange("g t s -> t g s"), writes=["wsf"])
    dm(p, "sync", bs, wl["sg_b"].rearrange("g t -> t g"), writes=["bs"], allow_slow_non_contiguous=True)
    for g in range(4):
        trp(p, ps[7][:, g * 128:(g + 1) * 128], wsf[:, g, :], identf[:], reads=["wsf", "identf"], writes=["ps7"])
    cp(p, "dve", wsT.rearrange("p g t -> p (g t)"), ps[7][:, 0:512], reads=["ps7"], writes=["wsT"])
    for tile in range(NT):
        s2 = tile % 2
        pap, pk = proj_tm(p, ps, xT, wu, "wu", 512, tile, 0 + s2)
        act(p, u[s2], pap, AF.Gelu, reads=[pk], writes=[("u", s2)])
        pap, pk = proj_tm(p, ps, xT, wv, "wv", 512, tile, 2 + s2)
        act(p, v[s2], pap, AF.Gelu, reads=[pk], writes=[("v", s2)])
        layer_norm_rows(v[s2], ("v", s2), v[s2], ("v", s2), 512, lng, lnb, ("lng", "lnb"), stats, mv, "sgln")
        cp(p, "pool", vn[s2], v[s2], reads=[("v", s2)], writes=[("vn", s2)])
        b = 4 + s2
        pk = "ps%d" % b
        for g in range(4):
            mm(p, ps[b][:, g * 128:(g + 1) * 128], wsT[:, g, :], vn[s2][:, g * 128:(g + 1) * 128], True, True,
               reads=["wsT", ("vn", s2)], writes=[pk])
        for g in range(4):
            stt(p, ysg[s2][:, g * 128:(g + 1) * 128], ps[b][:, g * 128:(g + 1) * 128], bs[:, g:g + 1], u[s2][:, g * 128:(g + 1) * 128],
                ALU.add, ALU.mult, reads=[pk, "bs", ("u", s2)], writes=[("ysg", s2)])
        b2 = 6
        for c4 in range(4):
            trp(p, psb(b2)[:, c4 * 128:(c4 + 1) * 128], ysg[s2][:, c4 * 128:(c4 + 1) * 128], identb[:], reads=[("ysg", s2), "identb"], writes=["ps6"])
        cp(p, "act", yT[:, :, tile * 128:(tile + 1) * 128], psb(b2)[:, 0:512].rearrange("p (c t) -> p c t", t=128), reads=["ps6"], writes=["yT"])
    for c4 in range(4):
        dm(p, "sync", YST[2, c4], yT[:, c4, :], reads=["yT"], writes=["YST2"])


def stage_pool(nc, p, ar, ps, psb, xT, XT_ALL, wl, C, YST, load_w_fm):
    p.barrier()
    ar.reset()
    w_in = wl["w_in"]
    PADW = S + 32
    wch = [ar.bf(16 * 128).rearrange("p (a b) -> p a b", b=128) for _ in range(2)]
    wp = ar.bf(512).rearrange("p (g d) -> p g d", d=128)
    psc = ar.f32(4)
    edge = ar.f32(64).rearrange("p (g s j) -> p g s j", s=2, j=8)
    pp = ar.f32(PADW)
    A = [ar.f32(PADW) for _ in range(2)]
    dT = ar.bf(S)
    yT = ar.bf(S)
    p.dma("pool", lambda e: e.dma_start(out=wp, in_=wl["pool_w"].rearrange("g c d -> c g d")), writes=["wp"])
    dm(p, "sync", psc, wl["pool_scale"].rearrange("(g q) -> q g", q=128), writes=["psc"], allow_slow_non_contiguous=True)
    dm(p, "sync", edge.rearrange("p g s j -> p (g s j)"), C["pool_edge"], writes=["edge"])
    ms(p, "pool", pp, 0.0, writes=["pp"])
    ms(p, "pool", A[0], 0.0, writes=["A0"])
    ms(p, "pool", A[1], 0.0, writes=["A1"])
    O = 16
    E0, EN = O - 8, S + 16
    for g in range(4):
        wb = wch[g % 2]
        wk_ = ("wch", g % 2)
        load_w_fm(wb, wk_, w_in, OFF_PP + g * 128, 128)

        def evac(q, pap, pk):
            cp(p, "act", pp[:, O + q * 512:O + (q + 1) * 512], pap, reads=[pk], writes=["pp"])
        proj_fm(p, ps, xT, wb, wk_, 128, evac)
        tt_(p, "pool", A[0][:, E0:E0 + EN], pp[:, E0 - 1:E0 - 1 + EN], pp[:, E0:E0 + EN], ALU.add, reads=["pp", "A0"], writes=["A0"])
        cur = 0
        sh = 1
        for step in range(g):
            nxt = 1 - cur
            tt_(p, "pool", A[nxt][:, E0:E0 + EN], A[cur][:, E0 - sh:E0 - sh + EN], A[cur][:, E0 + sh:E0 + sh + EN], ALU.add,
                reads=["A%d" % cur, "A%d" % nxt], writes=["A%d" % nxt])
            cur = nxt
            sh *= 2
        w = (2, 4, 8, 16)[g]
        hw = w // 2
        stt(p, dT[:, :], A[cur][:, O:O + S], 1.0 / w, pp[:, O:O + S], ALU.mult, ALU.subtract, reads=["A%d" % cur, "pp"], writes=["dT"])
        for side, c0 in ((0, 0), (1, S - hw)):
            tt_(p, "dve", A[cur][:, O + c0:O + c0 + hw], A[cur][:, O + c0:O + c0 + hw], edge[:, g, side, 0:hw], ALU.mult,
                reads=["A%d" % cur, "edge", "dT"], writes=["A%d" % cur])
            tt_(p, "dve", dT[:, c0:c0 + hw], A[cur][:, O + c0:O + c0 + hw], pp[:, O + c0:O + c0 + hw], ALU.subtract,
                reads=["A%d" % cur, "pp"], writes=["dT"])
        for q in range(4):
            b = 2 + q % 2
            pk = "ps%d" % b
            mm(p, ps[b][:, :], wp[:, g, :], dT[:, q * 512:(q + 1) * 512], True, True, reads=["wp", "dT"], writes=[pk])
            act(p, yT[:, q * 512:(q + 1) * 512], ps[b][:, :], AF.Identity, reads=[pk, "psc"], writes=["yT"], scale=psc[:, g:g + 1])
        dm(p, "sync", YST[3, g], yT, reads=["yT"], writes=["YST3"])


def stage_merge(nc, p, ar, ps, psb, xT, XT_ALL, identf, wl, C, YST, MT, cur_x, XA, load_w_fm, bcast_load,
                layer_norm_rows, tile_to_xT, logits=None):
    p.barrier()
    ar.reset()
    ystb = [ar.bf(4 * S).rearrange("p (k t) -> p k t", k=4) for _ in range(2)]
    wg = [ar.bf(16 * 512).rearrange("p (k c) -> p k c", c=512) for _ in range(2)]
    wb = [ar.bf(4 * 512).rearrange("p (k c) -> p k c", c=512) for _ in range(2)]
    bg = ar.f32(64)
    gsb = [ar.f32(512) for _ in range(2)]
    macc = [[ar.f32(512) for _ in range(4)] for _ in range(4)]
    tmpm = [ar.f32(512) for _ in range(2)]
    mTb = [ar.bf(512) for _ in range(2)]
    bgr = ar.f32(128)
    dm(p, "sync", bgr[0:64, :], wl["b_gate"].rearrange("(c q) -> c q", q=128), writes=["bgr"])
    trp(p, ps[7][:, 0:64], bgr[0:64, :], identf[0:64, 0:64], reads=["bgr", "identf"], writes=["ps7"])
    cp(p, "dve", bg, ps[7][:, 0:64], reads=["ps7"], writes=["bg"])
    w_gate, w_branch = wl["w_gate"], wl["w_branch"]
    it = 0
    for dcg in range(4):
        for n in range(4):
            b = it % 2
            it += 1
            c0 = n * D + dcg * 512
            p.dma("pool", lambda e, c0=c0, b=b: e.dma_start(
                out=wg[b], in_=w_gate[:, c0:c0 + 512].rearrange("(k q) c -> q k c", q=128)), writes=[("wg", b)])
            p.dma("pool", lambda e, n=n, dcg=dcg, b=b: e.dma_start(
                out=wb[b], in_=w_branch[n, :, dcg * 512:(dcg + 1) * 512].rearrange("(k q) c -> q k c", q=128)), writes=[("wb", b)])
            for k in range(4):
                dm(p, "sync", ystb[b][:, k, :], YST[n, k], writes=[("yst", b)])
            for dl in range(4):
                dc = dcg * 4 + dl
                cs = slice(dl * 128, (dl + 1) * 128)
                for q in range(4):
                    tq = slice(q * 512, (q + 1) * 512)
                    ga = (dl * 4 + q) % 2
                    gk = "ps%d" % ga
                    for k in range(16):
                        mm(p, ps[ga][:, :], wg[b][:, k, cs], xT[:, k, tq], k == 0, k == 15, reads=[("wg", b)] + xt_keys(q * 4, 4), writes=[gk])
                    act(p, gsb[ga], ps[ga][:, :], AF.Sigmoid, reads=[gk, "bg"], writes=[("gsb", ga)], bias=bg[:, n * 16 + dc:n * 16 + dc + 1])
                    pa = 2 + ga
                    pk = "ps%d" % pa
                    for k in range(4):
                        mm(p, ps[pa][:, :], wb[b][:, k, cs], ystb[b][:, k, tq], k == 0, k == 3, reads=[("wb", b), ("yst", b)], writes=[pk])
                    mk = ("macc", dl, q)
                    if n == 0:
                        tt_(p, "dve", macc[dl][q], ps[pa][:, :], gsb[ga], ALU.mult, reads=[pk, ("gsb", ga)], writes=[mk])
                    else:
                        tt_(p, "dve", tmpm[ga], ps[pa][:, :], gsb[ga], ALU.mult, reads=[pk, ("gsb", ga)], writes=[("tmpm", ga)])
                        if n < 3:
                            tt_(p, "pool", macc[dl][q], macc[dl][q], tmpm[ga], ALU.add, reads=[mk, ("tmpm", ga)], writes=[mk])
                        else:
                            tt_(p, "pool", mTb[ga], macc[dl][q], tmpm[ga], ALU.add, reads=[mk, ("tmpm", ga)], writes=[("mTb", ga)])
                            dm(p, "sync", MT[dc, :, tq], mTb[ga], reads=[("mTb", ga)], writes=["MT"])

    if STOP[0] == "mg1":
        return
    p.barrier()
    ar.reset()
    wout = ar.bf(16 * D).rearrange("p (k c) -> p k c", c=D)
    lng = ar.f32(D)
    lnb = ar.f32(D)
    mt = [ar.bf(16 * 128).rearrange("p (k t) -> p k t", t=128) for _ in range(2)]
    xres = [ar.f32(D) for _ in range(2)]
    sres = xres
    stats = ar.f32(24)
    mv = ar.f32(4)
    wr = ar.f32(16 * 36).rearrange("p (k c) -> p k c", c=36)
    whb = ar.bf(16 * 36).rearrange("p (k c) -> p k c", c=36)
    wlb = ar.bf(16 * 36).rearrange("p (k c) -> p k c", c=36)
    wtmp = ar.f32(16 * 36).rearrange("p (k c) -> p k c", c=36)
    brb = ar.f32(36)
    hb = [ar.bf(D) for _ in range(2)]
    lob = ar.bf(D)
    loT = ar.bf(16 * 128).rearrange("p (k t) -> p k t", t=128)
    for oc in range(4):
        p.dma("pool", lambda e, oc=oc: e.dma_start(out=wout[:, :, oc * 512:(oc + 1) * 512],
                                                   in_=wl["w_out"][:, oc * 512:(oc + 1) * 512].rearrange("(k q) c -> q k c", q=128)),
              writes=["wout"])
    bcast_load(lng, "lng", wl["ln1_g"], D)
    bcast_load(lnb, "lnb", wl["ln1_b"], D)
    dm(p, "sync", wr.rearrange("p k c -> p (k c)"), wl["w_router"], writes=["wr"])
    bcast_load(brb, "brb", wl["b_router"], 36)
    cp(p, "dve", whb, wr, reads=["wr"], writes=["whb"])
    tt_(p, "dve", wtmp, wr, whb, ALU.subtract, reads=["wr", "whb"], writes=["wtmp"])
    cp(p, "dve", wlb, wtmp, reads=["wtmp"], writes=["wlb"])
    P2 = int(os.environ.get("P2STOP", "99"))
    for tile in range(NT):
        b = tile % 2
        rs = slice(tile * 128, (tile + 1) * 128)
        for k in range(16):
            dm(p, "sync", mt[b][:, k, :], MT[k, :, rs], reads=["MT"], writes=[("mt", b)])
        dm(p, "sync", xres[b], cur_x[rs, :], writes=[("sres", b)])
        for oc in range(4):
            ok = "ps%d" % oc
            for k in range(16):
                mm(p, ps[oc][:, :], mt[b][:, k, :], wout[:, k, oc * 512:(oc + 1) * 512], k == 0, k == 15, reads=[("mt", b), "wout"], writes=[ok])
            stt(p, sres[b][:, oc * 512:(oc + 1) * 512], xres[b][:, oc * 512:(oc + 1) * 512], ALPHA, ps[oc][:, :], ALU.mult, ALU.add,
                reads=[ok, ("sres", b)], writes=[("sres", b)])
        layer_norm_rows(sres[b], ("sres", b), sres[b], ("sres", b), D, lng, lnb, ("lng", "lnb"), stats, mv, "ln1")
        dm(p, "sync", XA[rs, :], sres[b], reads=[("sres", b)], writes=["XA"])
        if P2 <= 4:
            continue
        tsl = slice(tile * 128, (tile + 1) * 128)
        tile_to_xT(sres[b], ("sres", b), tile, 4, hb[b], ("hb", b), lo=((lob, "lob", loT, "loT") if logits is not None else None))
        if P2 <= 5:
            continue
        if logits is not None:
            n = 0
            for k in range(16):
                for a_, ak_, w_, wk_ in ((xT[:, k, tsl], ("xT", tile), whb, "whb"), (xT[:, k, tsl], ("xT", tile), wlb, "wlb"),
                                         (loT[:, k, :], "loT", whb, "whb")):
                    mm(p, ps[6][:, 0:36], a_, w_[:, k, :], n == 0, n == 47, reads=[ak_, wk_], writes=["ps6"])
                    n += 1
            tt_(p, "dve", logits[:, tile * 36:(tile + 1) * 36], ps[6][:, 0:36], brb, ALU.add, reads=["ps6", "brb"], writes=["logits"])


def make_in_map(x_b, weights, consts):
    m = {"x": np.ascontiguousarray(x_b, dtype=np.float32)}
    weights = dict(weights)
    wr = np.concatenate([np.asarray(weights["w_router_group"]), np.asarray(weights["w_router_expert"])], axis=-1)
    L_ = wr.shape[0]
    weights["w_router"] = wr.reshape(L_, 16, 128, 36).transpose(0, 2, 1, 3)
    weights["b_router"] = np.concatenate([np.asarray(weights["b_router_group"]), np.asarray(weights["b_router_expert"])], axis=-1)
    for k, shp in WEIGHT_SHAPES.items():
        w = np.asarray(weights[k], dtype=np.float32)
        m[k] = np.ascontiguousarray(w.reshape([w.shape[0]] + shp))
    for k, v in consts.items():
        m["c_" + k] = np.ascontiguousarray(v.reshape(CONST_SHAPES[k]))
    return m


def bc(ap, axis, shape):
    return ap.unsqueeze(axis).to_broadcast(shape)


def red(p, eng, out, in_, op, axis, reads=(), writes=()):
    return p.op(eng, lambda e: e.tensor_reduce(out=out, in_=in_, axis=axis, op=op), reads, writes)


def stage_moe(nc, p, ar, ps, psb, xT, XT_ALL, identb, identf, onesf, wl, C, XA, XS, YB, dst, bcast_load,
              layer_norm_rows, tile_to_xT, logits, first, last):
    p.barrier()
    ar.reset()
    IOA = bass.IndirectOffsetOnAxis
    desti = ar.i32(32)
    idxW = ar.i32(NBLK * 4).rearrange("p (b q) -> p b q", q=4)
    idxW2 = ar.i32(NBLK * 4).rearrange("p (b q) -> p b q", q=4)
    wts = ar.f32(32).rearrange("p (a k) -> p a k", k=2)
    mark = ar.off
    L3 = logits.rearrange("p (a c) -> p a c", c=36)
    lg = L3[:, :, 0:4]
    le = L3[:, :, 4:36]
    mx = ar.f32(16)
    ohg = ar.f32(64).rearrange("p (a g) -> p a g", g=4)
    eg = ar.f32(64).rearrange("p (a g) -> p a g", g=4)
    se = ar.f32(16)
    psel = ar.f32(16)
    pen = ar.f32(64).rearrange("p (a g) -> p a g", g=4)
    lem = ar.f32(512).rearrange("p (a g e) -> p a g e", g=4, e=8)
    lem2 = ar.f32(512).rearrange("p (a c) -> p a c", c=32)
    m1 = ar.f32(16)
    m2 = ar.f32(16)
    E = ar.f32(1024).rearrange("p (k a c) -> p k a c", k=2, c=32)
    tot = ar.f32(1024).rearrange("p (j c) -> p j c", c=32)
    off = ar.f32(1024).rearrange("p (j c) -> p j c", c=32)
    Sm = ar.f32(1024).rearrange("p (j c) -> p j c", c=32)
    cnt = ar.f32(32)
    cnti = ar.i32(32)
    padv = ar.f32(32)
    pst = ar.f32(33)
    pend = ar.f32(32)
    destf = ar.f32(32)
    thr = ar.f32(NBLK * 32).rearrange("p (b c) -> p b c", c=32)
    cmpv = ar.f32(NBLK * 32).rearrange("p (b c) -> p b c", c=32)
    bex = ar.f32(NBLK)
    iow = ar.f32(4)
    idxf = ar.f32(NBLK * 4).rearrange("p (b q) -> p b q", q=4)
    tris = ar.f32(128)
    e21 = ar.f32(16)
    rden = ar.f32(16)
    dm(p, "sync", thr.rearrange("p b c -> p (b c)"), C["thr"], writes=["thr"])
    dm(p, "sync", iow, C["iow"], writes=["iow"])
    dm(p, "sync", tris, C["tri_s"], writes=["tris"])
    lemf = lem.rearrange("p a g e -> p a (g e)")
    red(p, "dve", mx, lg, ALU.max, AX.X, reads=["logits"], writes=["mx"])
    tt_(p, "dve", ohg, lg, bc(mx, 2, [128, NT, 4]), ALU.is_equal, reads=["logits", "mx"], writes=["ohg"])
    tt_(p, "dve", eg, lg, bc(mx, 2, [128, NT, 4]), ALU.subtract, reads=["logits", "mx"], writes=["eg"])
    act(p, eg, eg, AF.Exp, reads=["eg"], writes=["eg"])
    red(p, "dve", se, eg, ALU.add, AX.X, reads=["eg"], writes=["se"])
    p.op("dve", lambda e: e.reciprocal(out=psel, in_=se), reads=["se"], writes=["psel"])
    ts(p, "dve", pen, ohg, -1.0, 1e30, ALU.add, ALU.mult, reads=["ohg"], writes=["pen"])
    tt_(p, "dve", lem, le.rearrange("p a (g e) -> p a g e", e=8), bc(pen, 3, [128, NT, 4, 8]), ALU.add, reads=["logits", "pen"], writes=["lem"])
    red(p, "dve", m1, lemf, ALU.max, AX.X, reads=["lem"], writes=["m1"])
    tt_(p, "dve", E[:, 0], lemf, bc(m1, 2, [128, NT, 32]), ALU.is_equal, reads=["lem", "m1"], writes=["E"])
    stt(p, lem2, E[:, 0], -1e30, lemf, ALU.mult, ALU.add, reads=["E", "lem"], writes=["lem2"])
    red(p, "dve", m2, lem2, ALU.max, AX.X, reads=["lem2"], writes=["m2"])
    tt_(p, "dve", E[:, 1], lem2, bc(m2, 2, [128, NT, 32]), ALU.is_equal, reads=["lem2", "m2", "E"], writes=["E"])
    tt_(p, "dve", e21, m2, m1, ALU.subtract, reads=["m1", "m2"], writes=["e21"])
    act(p, e21, e21, AF.Exp, reads=["e21"], writes=["e21"])
    ts(p, "dve", rden, e21, 1.0, None, ALU.add, reads=["e21"], writes=["rden"])
    p.op("dve", lambda e: e.reciprocal(out=rden, in_=rden), reads=["rden"], writes=["rden"])
    tt_(p, "dve", rden, rden, psel, ALU.mult, reads=["rden", "psel"], writes=["rden"])
    cp(p, "dve", wts[:, :, 0], rden, reads=["rden"], writes=["wts"])
    tt_(p, "dve", wts[:, :, 1], rden, e21, ALU.mult, reads=["rden", "e21", "wts"], writes=["wts"])
    Ef = E.rearrange("p k a c -> p (k a c)")
    for hf in range(2):
        mm(p, ps[hf][:, :], tris, Ef[:, hf * 512:(hf + 1) * 512], True, True, reads=["tris", "E"], writes=["ps%d" % hf])
        mm(p, ps[2 + hf][:, :], onesf[:], Ef[:, hf * 512:(hf + 1) * 512], True, True, reads=["onesf", "E"], writes=["ps%d" % (2 + hf)])
        cp(p, "dve", tot.rearrange("p j c -> p (j c)")[:, hf * 512:(hf + 1) * 512], ps[2 + hf][:, :], reads=["ps%d" % (2 + hf)], writes=["tot"])
    ms(p, "dve", off[:, 0, :], 0.0, writes=["off"])
    for j in range(31):
        tt_(p, "dve", off[:, j + 1, :], off[:, j, :], tot[:, j, :], ALU.add, reads=["off", "tot"], writes=["off"])
    tt_(p, "dve", cnt, off[:, 31, :], tot[:, 31, :], ALU.add, reads=["off", "tot"], writes=["cnt"])
    ts(p, "dve", cnt, cnt, float(BLKS - 1), None, ALU.add, reads=["cnt"], writes=["cnt"])
    cp(p, "dve", cnti, cnt, reads=["cnt"], writes=["cnti"])
    p.op("dve", lambda e: e.tensor_single_scalar(out=cnti, in_=cnti, scalar=8, op=ALU.arith_shift_right), reads=["cnti"], writes=["cnti"])
    cp(p, "dve", padv, cnti, reads=["cnti"], writes=["padv"])
    ts(p, "dve", padv, padv, float(BLKS), None, ALU.mult, reads=["padv"], writes=["padv"])
    ms(p, "dve", pst[:, 0:1], 0.0, writes=["pst"])
    for e_ in range(32):
        tt_(p, "dve", pst[:, e_ + 1:e_ + 2], pst[:, e_:e_ + 1], padv[:, e_:e_ + 1], ALU.add, reads=["pst", "padv"], writes=["pst"])
    cp(p, "dve", pend, pst[:, 1:33], reads=["pst"], writes=["pend"])
    tt_(p, "dve", Sm, off, bc(pst[:, 0:32], 1, [128, 32, 32]), ALU.add, reads=["off", "pst"], writes=["Sm"])
    Smf = Sm.rearrange("p j c -> p (j c)")
    for hf in range(2):
        tt_(p, "dve", Smf[:, hf * 512:(hf + 1) * 512], ps[hf][:, :], Smf[:, hf * 512:(hf + 1) * 512], ALU.add, reads=["ps%d" % hf, "Sm"], writes=["Sm"])
    tt_(p, "dve", Smf, Smf, Ef, ALU.mult, reads=["Sm", "E"], writes=["Sm"])
    red(p, "dve", destf, Sm, ALU.add, AX.X, reads=["Sm"], writes=["destf"])
    cp(p, "dve", desti, destf, reads=["destf"], writes=["desti"])
    tt_(p, "dve", cmpv, thr, bc(pend, 1, [128, NBLK, 32]), ALU.is_ge, reads=["thr", "pend"], writes=["cmpv"])
    red(p, "dve", bex, cmpv, ALU.add, AX.X, reads=["cmpv"], writes=["bex"])
    bigt = ar.f32(NBLK)
    ts(p, "dve", bigt, bex, 31.5, (1.0e6 if SKIP_UNUSED else 0.0), ALU.is_ge, ALU.mult, reads=["bex"], writes=["bigt"])
    ts(p, "dve", bex, bex, 31.0, None, ALU.min, reads=["bex", "bigt"], writes=["bex"])
    cp(p, "dve", idxf, bc(iow, 1, [128, NBLK, 4]), reads=["iow"], writes=["idxf"])
    stt(p, idxf, bc(bex, 2, [128, NBLK, 4]), 512.0, idxf, ALU.mult, ALU.add, reads=["bex", "idxf"], writes=["idxf"])
    ts(p, "dve", idxf, idxf, float(wl["_l"] * 16384), None, ALU.add, reads=["idxf"], writes=["idxf"])
    cp(p, "dve", idxW2, idxf, reads=["idxf"], writes=["idxW2"])
    tt_(p, "dve", idxf, idxf, bc(bigt, 2, [128, NBLK, 4]), ALU.add, reads=["bigt", "idxf", "idxW2"], writes=["idxf"])
    cp(p, "dve", idxW, idxf, reads=["idxf"], writes=["idxW"])

    p.barrier()
    ar.off = mark
    xb = [ar.bf(D) for _ in range(2)]
    if first:
        ms(p, "pool", xb[0], 0.0, writes=[("xb", 0)])
        for b in range(NSLOT // 128):
            dm(p, "sync", XS[b * 128:(b + 1) * 128, :], xb[0], reads=[("xb", 0)], writes=["XS"])
        p.barrier()
    for tile in range(NT):
        b = tile % 2
        p.dma("pool", lambda e, tile=tile, b=b: e.dma_start(out=xb[b], in_=XA[tile * 128:(tile + 1) * 128, :]), writes=[("xb", b)])
        for k in range(2):
            j = k * NT + tile
            p.dma("pool", lambda e, j=j, b=b: e.indirect_dma_start(
                out=XS[:, :], out_offset=IOA(ap=desti[:, j:j + 1], axis=0), in_=xb[b], in_offset=None),
                reads=[("xb", b), "desti"], writes=["XSs"])
    p.barrier()
    ar.off = mark
    w1b = [ar.bf(16 * 512).rearrange("p (j f) -> p j f", f=512) for _ in range(2)]
    w3b = [ar.bf(16 * 512).rearrange("p (j f) -> p j f", f=512) for _ in range(2)]
    w2b = [ar.bf(4 * D).rearrange("p (j c) -> p j c", c=D) for _ in range(2)]
    xs = [ar.bf(D) for _ in range(2)]
    XsT = [ar.bf(16 * 128).rearrange("p (j s) -> p j s", s=128) for _ in range(2)]
    hs = [ar.f32(512) for _ in range(2)]
    Hb = [ar.bf(512) for _ in range(2)]
    HT = [ar.bf(512).rearrange("p (j s) -> p j s", s=128) for _ in range(2)]
    ybs = [ar.f32(D) for _ in range(2)]
    W1 = wl["_W"]["w_exp_gate"].rearrange("l e (r a) f -> (l e r) (a f)", a=4)
    W3 = wl["_W"]["w_exp_up"].rearrange("l e (r a) f -> (l e r) (a f)", a=4)
    W2 = wl["_W"]["w_exp_down"].rearrange("l e f c -> (l e f) c")
    nrows = int(W1.shape[0])
    _rh = {}

    def bkw_(e):
        if not SKIP_UNUSED:
            return {}
        if "r" not in _rh:
            r = e.alloc_register("moe_bound_%d" % wl["_l"])
            e.reg_mov(r, nrows - 1)
            _rh["r"] = r
        return dict(bounds_check=_rh["r"], oob_is_err=False)
    for b in range(NBLK):
        s2 = b % 2
        for q in range(4):
            p.dma("pool", lambda e, b=b, q=q, s2=s2: e.indirect_dma_start(
                out=w1b[s2][:, 4 * q:4 * q + 4, :].rearrange("p j f -> p (j f)"), out_offset=None, in_=W1,
                in_offset=IOA(ap=idxW[:, b, q:q + 1], axis=0), **bkw_(e)), reads=["idxW"], writes=[("w1b", s2)])
            p.dma("pool", lambda e, b=b, q=q, s2=s2: e.indirect_dma_start(
                out=w3b[s2][:, 4 * q:4 * q + 4, :].rearrange("p j f -> p (j f)"), out_offset=None, in_=W3,
                in_offset=IOA(ap=idxW[:, b, q:q + 1], axis=0), **bkw_(e)), reads=["idxW"], writes=[("w3b", s2)])
        for q in range(4):
            p.dma("pool", lambda e, b=b, q=q, s2=s2: e.indirect_dma_start(
                out=w2b[s2].rearrange("p j c -> p (j c)")[:, q * D:(q + 1) * D], out_offset=None, in_=W2,
                in_offset=IOA(ap=idxW[:, b, q:q + 1], axis=0), **bkw_(e)), reads=["idxW"], writes=[("w2b", s2)])
        for sub in range(2):
            r0 = b * BLKS + sub * 128
            dm(p, "sync", xs[sub], XS[r0:r0 + 128, :], reads=["XSs"], writes=[("xs", sub)])
            xv = xs[sub].rearrange("s (q j) -> s j q", j=16)
            for hf in range(2):
                for jj in range(8):
                    j = hf * 8 + jj
                    trp(p, psb(hf, 1024)[:, jj * 128:(jj + 1) * 128], xv[:, j, :], identb[:], reads=[("xs", sub), "identb"], writes=["ps%d" % hf])
                cp(p, "act" if hf == 0 else "dve", XsT[sub][:, hf * 8:(hf + 1) * 8, :].rearrange("p j s -> p (j s)"), psb(hf, 1024),
                   reads=["ps%d" % hf], writes=[("XsT", sub)])
        for sub in range(2):
            for j in range(16):
                mm(p, ps[2][:, :], XsT[sub][:, j, :], w1b[s2][:, j, :], j == 0, j == 15, reads=[("XsT", sub), ("w1b", s2)], writes=["ps2"])
            for j in range(16):
                mm(p, ps[3][:, :], XsT[sub][:, j, :], w3b[s2][:, j, :], j == 0, j == 15, reads=[("XsT", sub), ("w3b", s2)], writes=["ps3"])
            act(p, hs[sub], ps[2][:, :], AF.Silu, reads=["ps2"], writes=[("hs", sub)])
            tt_(p, "dve", Hb[sub], ps[3][:, :], hs[sub], ALU.mult, reads=["ps3", ("hs", sub)], writes=[("Hb", sub)])
        for sub in range(2):
            hv = Hb[sub].rearrange("s (q j) -> s j q", j=4)
            for j in range(4):
                trp(p, psb(sub, 1024)[:, j * 128:(j + 1) * 128], hv[:, j, :], identb[:], reads=[("Hb", sub), "identb"], writes=["ps%d" % sub])
            cp(p, "act", HT[sub].rearrange("p j s -> p (j s)"), psb(sub, 1024)[:, 0:512], reads=["ps%d" % sub], writes=[("HT", sub)])
        for sub in range(2):
            r0 = b * BLKS + sub * 128
            for oc in range(4):
                for j in range(4):
                    mm(p, ps[4 + oc][:, :], HT[sub][:, j, :], w2b[s2][:, j, oc * 512:(oc + 1) * 512], j == 0, j == 3, reads=[("HT", sub), ("w2b", s2)], writes=["ps%d" % (4 + oc)])
                cp(p, "act" if oc % 2 == 0 else "dve", ybs[sub][:, oc * 512:(oc + 1) * 512], ps[4 + oc][:, :], reads=["ps%d" % (4 + oc)], writes=[("ybs", sub)])
            dm(p, "sync", YB[r0:r0 + 128, :], ybs[sub], reads=[("ybs", sub)], writes=["YB"])
    p.barrier()
    ar.off = mark
    lng = ar.f32(D)
    lnb = ar.f32(D)
    y0 = [ar.f32(D) for _ in range(2)]
    y1 = [ar.f32(D) for _ in range(2)]
    x1 = [ar.f32(D) for _ in range(2)]
    hb2 = [ar.bf(D) for _ in range(2)]
    stats = ar.f32(24)
    mv = ar.f32(4)
    bcast_load(lng, "lng", wl["ln2_g"], D)
    bcast_load(lnb, "lnb", wl["ln2_b"], D)
    for tile in range(NT):
        b = tile % 2
        rs = slice(tile * 128, (tile + 1) * 128)
        for k, yk in ((0, y0), (1, y1)):
            j = k * NT + tile
            p.dma("pool", lambda e, j=j, yk=yk, b=b: e.indirect_dma_start(
                out=yk[b], out_offset=None, in_=YB[:, :], in_offset=IOA(ap=desti[:, j:j + 1], axis=0)),
                reads=["desti", "YB"], writes=[("y%d" % k, b)])
        dm(p, "sync", x1[b], XA[rs, :], writes=[("x1", b)])
        ts(p, "dve", y0[b], y0[b], wts[:, tile, 0:1], None, ALU.mult, reads=[("y0", b), "wts"], writes=[("y0", b)])
        stt(p, y0[b], y1[b], wts[:, tile, 1:2], y0[b], ALU.mult, ALU.add, reads=[("y0", b), ("y1", b), "wts"], writes=[("y0", b)])
        stt(p, x1[b], x1[b], ALPHA, y0[b], ALU.mult, ALU.add, reads=[("y0", b), ("x1", b)], writes=[("x1", b)])
        layer_norm_rows(x1[b], ("x1", b), x1[b], ("x1", b), D, lng, lnb, ("lng", "lnb"), stats, mv, "ln2")
        dm(p, "sync", dst[rs, :], x1[b], reads=[("x1", b)], writes=["dst"])
        if not last:
            tile_to_xT(x1[b], ("x1", b), tile, 0, hb2[b], ("hb2", b))


DEPTH = 4
_NC_CACHE = {}


def kernel(**inputs):
    x = np.asarray(inputs["x"], dtype=np.float32)
    weights = {k: inputs[k] for k in WEIGHT_SHAPES if k in inputs}
    consts = host_consts()
    if "nc" not in _NC_CACHE:
        _NC_CACHE["nc"] = build(DEPTH)
    nc = _NC_CACHE["nc"]
    base = make_in_map(x[0], weights, consts)
    in_maps = []
    for c in range(8):
        m = dict(base)
        m["x"] = np.ascontiguousarray(x[c])
        in_maps.append(m)
    res = run_bass_kernel_spmd(nc, in_maps, core_ids=list(range(8)))
    return np.stack([np.asarray(r["y"], dtype=np.float32) for r in res.results], axis=0)
```

```python
import contextlib
import math
import os
import numpy as np
import concourse.bass as bass
import concourse.mybir as mybir
from concourse.bass_utils import run_bass_kernel_spmd

F32 = mybir.dt.float32
BF16 = mybir.dt.bfloat16
I32 = mybir.dt.int32
AF = mybir.ActivationFunctionType
ALU = mybir.AluOpType
AX = mybir.AxisListType

S = 2048
D = 2048
NT = 16
D_IN = 7696
OFF_MQ, OFF_MK, OFF_MV, OFF_MO, OFF_MG = 0, 256, 512, 1024, 1536
OFF_AQ, OFF_AK, OFF_AV = 1552, 3088, 4624
OFF_SU, OFF_SV, OFF_PP = 6160, 6672, 7184
ALPHA = 8.0 ** 0.25
LN_EPS = 1e-5
ATT_R = (1, 2, 8)
ATT_DIL = (1, 4, 16)
NBLK = 48
BLKS = 256
NSLOT = NBLK * BLKS
ARN = 71000

ENGS = ("sync", "act", "dve", "pool", "pe")
N_DMA_SEMS = 16


class Prog:
    def __init__(self, nc):
        self.nc = nc
        self.ops = {e: [] for e in ENGS}
        self.cnt = {e: 0 for e in ENGS}
        self.dcnt = {e: 0 for e in ENGS}
        self.res = {}
        self.last = {}
        self.bar = {e: set() for e in ENGS}

    def _deps(self, eng, reads, writes):
        deps = set(self.bar[eng])
        self.bar[eng] = set()
        for k in reads:
            r = self.res.get(k)
            if r and r[0] is not None:
                deps.add(r[0])
        for k in writes:
            r = self.res.get(k)
            if r:
                if r[0] is not None:
                    deps.add(r[0])
                deps.update(r[1])
        return deps

    def _commit(self, tok, reads, writes):
        self.last[tok[0]] = tok
        for k in reads:
            r = self.res.setdefault(k, [None, []])
            r[1].append(tok)
        for k in writes:
            self.res[k] = [tok, []]

    def op(self, eng, fn, reads=(), writes=()):
        deps = self._deps(eng, reads, writes)
        self.cnt[eng] += 1
        tok = (("c", eng), self.cnt[eng], eng)
        self.ops[eng].append((fn, deps, tok, 1))
        self._commit(tok, reads, writes)
        return tok

    def dma(self, eng, fn, reads=(), writes=()):
        deps = self._deps(eng, reads, writes)
        i = self.dcnt[eng]
        self.dcnt[eng] += 1
        tok = (("d", eng, i % N_DMA_SEMS), 16 * (i // N_DMA_SEMS + 1), "dma_" + eng)
        prev = self.last.get(tok[0])
        if prev is not None:
            deps.add(prev)
        self.ops[eng].append((fn, deps, tok, 16))
        self._commit(tok, reads, writes)
        return tok

    def barrier(self):
        toks = set(self.last.values())
        for e in ENGS:
            self.bar[e] |= toks
        self.res = {}

    def emit(self):
        nc = self.nc
        finals = set(self.last.values())
        with contextlib.ExitStack() as st:
            sems = {}
            for e in ("act", "dve", "pool", "pe"):
                sems[("c", e)] = st.enter_context(nc.semaphore("c_" + e))
            for e in ("sync", "act", "pool"):
                for j in range(N_DMA_SEMS):
                    sems[("d", e, j)] = st.enter_context(nc.semaphore("d_%s_%d" % (e, j)))
            block = st.enter_context(nc.Block())
            ops = self.ops

            def run(engname, engobj, fin=()):
                waited = {}

                def waits(deps):
                    for (sk, val, deng) in sorted(deps, key=str):
                        if deng == "pe" and engname == "pe":
                            continue
                        if waited.get(sk, 0) >= val:
                            continue
                        engobj.wait_ge(sems[sk], val)
                        waited[sk] = val

                for fn, deps, tok, inc in ops[engname]:
                    waits(deps)
                    ins = fn(engobj)
                    ins.then_inc(sems[tok[0]], inc)
                waits([f for f in fin if not (f[2] == "pe" and engname == "pe")])

            @block.sync
            def _(sync):
                run("sync", sync, finals)

            @block.scalar
            def _(scalar):
                run("act", scalar)

            @block.vector
            def _(vector):
                run("dve", vector)

            @block.gpsimd
            def _(gpsimd):
                run("pool", gpsimd)

            @block.tensor
            def _(tensor):
                run("pe", tensor)


STOP = [None]
SKIP_UNUSED = bool(int(os.environ.get("SKIP_UNUSED", "1")))


def alibi_slope(g, h):
    n = 24
    return 2.0 ** (-8.0 * (g * 8 + h + 1) / n)


def host_consts():
    c = {}
    c["ident"] = np.eye(128, dtype=np.float32)
    s = np.arange(128)[:, None]
    t = np.arange(128)[None, :]
    c["tri_f"] = (s <= t).astype(np.float32)
    c["tri_b"] = (s >= t).astype(np.float32)
    c["mneg_f"] = np.where(s <= t, 0.0, -30000.0).astype(np.float32)
    c["mneg_b"] = np.where(s >= t, 0.0, -30000.0).astype(np.float32)
    c["tri_s"] = (s < t).astype(np.float32)
    c["ones"] = np.ones((128, 128), np.float32)
    tiles = []
    for g in range(3):
        dil = ATT_DIL[g]
        for o in range(-ATT_R[g], ATT_R[g] + 1):
            delta = (t - s) - 128 * o
            ok = (np.abs(delta) <= 64 * dil) & (delta % dil == 0)
            tiles.append(np.where(ok, np.abs(delta).astype(np.float32), 1e5))
    c["dist"] = np.ascontiguousarray(np.stack(tiles, axis=1).astype(np.float32))
    pe = np.zeros((128, 4, 2, 8), np.float32)
    for g, w in enumerate((2, 4, 8, 16)):
        h = w // 2
        for j in range(h):
            tt = j
            pe[:, g, 0, j] = 1.0 / (min(tt + h, S) - max(tt - h, 0))
            tt = S - h + j
            pe[:, g, 1, j] = 1.0 / (min(tt + h, S) - max(tt - h, 0))
    c["pool_edge"] = pe.reshape(128, 64)
    thr = np.zeros((128, NBLK, 32), np.float32)
    thr[:] = (float(BLKS) * np.arange(NBLK))[None, :, None]
    c["thr"] = thr.reshape(128, NBLK * 32)
    io = np.zeros((128, 4), np.float32)
    io[:] = 4.0 * np.arange(128)[:, None] + np.arange(4)[None, :]
    c["iow"] = io
    return c


CONST_SHAPES = {"ident": [128, 128], "tri_f": [128, 128], "tri_b": [128, 128], "mneg_f": [128, 128],
                "mneg_b": [128, 128], "tri_s": [128, 128], "ones": [128, 128], "dist": [128, 25, 128],
                "pool_edge": [128, 64], "thr": [128, NBLK * 32], "iow": [128, 4]}

WEIGHT_SHAPES = {
    "w_in": [D, D_IN], "ml_conv_w": [3, 512], "ml_gate_b": [16], "ml_norm_w": [512],
    "sg_ln_g": [512], "sg_ln_b": [512], "sg_w": [4, 128, 128], "sg_b": [4, 128],
    "pool_w": [4, 128, 128], "pool_scale": [512], "w_gate": [D, 4 * D], "b_gate": [4 * D],
    "w_branch": [4, 512, D], "w_out": [D, D], "ln1_g": [D], "ln1_b": [D],
    "w_router_group": [D, 4], "b_router_group": [4], "w_router_expert": [D, 32], "b_router_expert": [32],
    "w_exp_gate": [32, D, 512], "w_exp_up": [32, D, 512], "w_exp_down": [32, 512, D],
    "ln2_g": [D], "ln2_b": [D],
    "w_router": [128, 16 * 36], "b_router": [36],
}


def build(depth, stop_after=None, dbg=False):
    nc = bass.Bass("TRN2", target_bir_lowering=False)
    x_in = nc.dram_tensor("x", [S, D], F32, kind="ExternalInput").ap()
    W = {k: nc.dram_tensor(k, [depth] + v, F32, kind="ExternalInput").ap() for k, v in WEIGHT_SHAPES.items()}
    C = {k: nc.dram_tensor("c_" + k, v, F32, kind="ExternalInput").ap() for k, v in CONST_SHAPES.items()}
    y_out = nc.dram_tensor("y", [S, D], F32, kind="ExternalOutput").ap()
    okind = "ExternalOutput" if dbg else "Internal"
    XA = nc.dram_tensor("XA", [S, D], F32, kind=okind).ap()
    XB = nc.dram_tensor("XB", [S, D], F32, kind="Internal").ap()
    YST = nc.dram_tensor("YST", [4, 4, 128, S], BF16, kind=okind).ap()
    MT = nc.dram_tensor("MT", [16, 128, S], BF16, kind="Internal").ap()
    XS = nc.dram_tensor("XS", [NSLOT, D], BF16, kind="Internal").ap()
    YB = nc.dram_tensor("YB", [NSLOT, D], F32, kind="Internal").ap()

    st = contextlib.ExitStack()
    with st:
        def sb(name, shape, dt):
            return st.enter_context(nc.sbuf_tensor(name, shape, dt))

        xT = sb("xT", [128, 16, S], BF16)
        identb = sb("identb", [128, 128], BF16)
        identf = sb("identf", [128, 128], F32)
        onesf = sb("onesf", [128, 128], F32)
        AR = sb("AR", [128, ARN], BF16)
        logits = sb("logits", [128, NT * 36], F32)
        ps = [st.enter_context(nc.psum_tensor("ps%d" % i, [128, 512], F32)) for i in range(8)]
        p = Prog(nc)

        class Arena:
            def __init__(self):
                self.off = 0

            def reset(self):
                self.off = 0

            def f32(self, n):
                o = (self.off + 15) // 16 * 16
                self.off = o + 2 * n
                assert self.off <= ARN, self.off
                return AR[:, o:o + 2 * n].bitcast(F32)

            def bf(self, n):
                o = (self.off + 15) // 16 * 16
                self.off = o + n
                assert self.off <= ARN, self.off
                return AR[:, o:o + n]

            def i32(self, n):
                o = (self.off + 15) // 16 * 16
                self.off = o + 2 * n
                assert self.off <= ARN, self.off
                return AR[:, o:o + 2 * n].bitcast(I32)

        ar = Arena()

        def psb(i, n=512):
            return ps[i][:, 0:n // 2].bitcast(BF16)

        p.dma("pool", lambda e: e.dma_start(out=identb[:], in_=C["ident"]), writes=["identb"])
        p.dma("sync", lambda e: e.dma_start(out=identf[:], in_=C["ident"]), writes=["identf"])
        p.dma("sync", lambda e: e.dma_start(out=onesf[:], in_=C["ones"]), writes=["onesf"])

        def tile_to_xT(src, src_key, tt, pbank, hb, hb_key, lo=None):
            cp(p, "act", hb, src, reads=[src_key], writes=[hb_key])
            for half in range(2):
                bank = pbank + half
                pk = "ps%d" % bank
                for jj in range(8):
                    dc = half * 8 + jj
                    trp(p, psb(bank, 1024)[:, jj * 128:(jj + 1) * 128], hb[:, dc * 128:(dc + 1) * 128], identb[:],
                        reads=[hb_key, "identb"], writes=[pk])
                cp(p, "act" if half == 0 else "dve", xT[:, half * 8:(half + 1) * 8, tt * 128:(tt + 1) * 128],
                   psb(bank, 1024).rearrange("p (a b) -> p a b", b=128), reads=[pk], writes=[("xT", tt)])
            if lo is not None:
                lob, lob_key, loT, loT_key = lo
                tt_(p, "dve", lob, src, hb, ALU.subtract, reads=[src_key, hb_key], writes=[lob_key])
                for half in range(2):
                    bank = pbank + half
                    pk = "ps%d" % bank
                    for jj in range(8):
                        dc = half * 8 + jj
                        trp(p, psb(bank, 1024)[:, jj * 128:(jj + 1) * 128], lob[:, dc * 128:(dc + 1) * 128], identb[:],
                            reads=[lob_key, "identb"], writes=[pk])
                    cp(p, "act" if half == 0 else "dve", loT[:, half * 8:(half + 1) * 8, :],
                       psb(bank, 1024).rearrange("p (a b) -> p a b", b=128), reads=[pk], writes=[loT_key])

        XT_ALL = [("xT", tt) for tt in range(NT)]

        def load_w_fm(dst, key, wap, lo, ncols):
            p.dma("pool", lambda e: e.dma_start(
                out=dst, in_=wap[:, lo:lo + ncols].rearrange("(dc q) c -> q dc c", q=128)), writes=[key])

        def bcast_load(dst, key, vec_ap, n):
            for c0 in range(0, n, 512):
                c1 = min(n, c0 + 512)
                p.dma("sync", lambda e, c0=c0, c1=c1: e.dma_start(out=dst[:, c0:c1], in_=vec_ap[c0:c1].partition_broadcast(128)), writes=[key])

        def layer_norm_rows(src, src_key, dst, dst_key, n, gt, bt, gb_keys, stats, mv, tmpk):
            nch = max(1, n // 512)
            w = n // nch
            for c in range(nch):
                p.op("dve", lambda e, c=c: e.bn_stats(out=stats[:, c * 6:(c + 1) * 6], in_=src[:, c * w:(c + 1) * w]),
                     reads=[src_key], writes=[tmpk + "st"])
            p.op("dve", lambda e: e.bn_aggr(out=mv[:, 0:2], in_=stats[:, 0:nch * 6]), reads=[tmpk + "st"], writes=[tmpk + "mv"])
            p.op("act", lambda e: e.activation(out=mv[:, 2:3], in_=mv[:, 1:2], func=AF.Sqrt, bias=LN_EPS),
                 reads=[tmpk + "mv"], writes=[tmpk + "sd"])
            p.op("dve", lambda e: e.reciprocal(out=mv[:, 3:4], in_=mv[:, 2:3]), reads=[tmpk + "sd"], writes=[tmpk + "rs"])
            p.op("dve", lambda e: e.tensor_scalar(out=dst, in0=src, scalar1=mv[:, 0:1], scalar2=mv[:, 3:4],
                                                  op0=ALU.subtract, op1=ALU.mult),
                 reads=[src_key, tmpk + "mv", tmpk + "rs"], writes=[dst_key])
            if gt is not None:
                p.op("dve", lambda e: e.tensor_tensor(out=dst, in0=dst, in1=gt, op=ALU.mult),
                     reads=[dst_key, gb_keys[0]], writes=[dst_key])
                p.op("dve", lambda e: e.tensor_tensor(out=dst, in0=dst, in1=bt, op=ALU.add),
                     reads=[dst_key, gb_keys[1]], writes=[dst_key])

        cur_x = x_in
        for l in range(depth):
            last = (l == depth - 1)
            wl = {k: v[l] for k, v in W.items()}
            wl["_l"] = l
            wl["_W"] = W
            if l == 0:
                p.barrier()
                ar.reset()
                xt_tiles = [ar.f32(2048) for _ in range(2)]
                hbA = [ar.bf(2048) for _ in range(2)]
                for tt in range(NT):
                    b = tt % 2
                    p.dma("sync", lambda e, tt=tt, b=b, cx=cur_x: e.dma_start(out=xt_tiles[b], in_=cx[tt * 128:(tt + 1) * 128, :]),
                          writes=[("xld", b)])
                    tile_to_xT(xt_tiles[b], ("xld", b), tt, 0, hbA[b], ("hbA", b))

            if stop_after == "A":
                break
            STOP[0] = stop_after
            stage_mlstm(nc, p, ar, ps, psb, xT, XT_ALL, identb, identf, onesf, wl, C, YST, load_w_fm, bcast_load,
                        layer_norm_rows)
            if stop_after in ("mlstm", "ml1", "ml2"):
                break
            stage_attn(nc, p, ar, ps, psb, xT, XT_ALL, identb, wl, C, YST, load_w_fm)
            if stop_after == "attn":
                break
            stage_sg(nc, p, ar, ps, psb, xT, XT_ALL, identb, identf, wl, C, YST, load_w_fm, bcast_load, layer_norm_rows)
            stage_pool(nc, p, ar, ps, psb, xT, XT_ALL, wl, C, YST, load_w_fm)
            if stop_after == "branches":
                break
            stage_merge(nc, p, ar, ps, psb, xT, XT_ALL, identf, wl, C, YST, MT, cur_x, XA, load_w_fm, bcast_load,
                        layer_norm_rows, tile_to_xT, logits=(None if os.environ.get("NOROUTER") else logits))
            if stop_after in ("ln1", "mg1"):
                break
            dst = y_out if last else XB
            stage_moe(nc, p, ar, ps, psb, xT, XT_ALL, identb, identf, onesf, wl, C, XA, XS, YB, dst, bcast_load,
                      layer_norm_rows, tile_to_xT, logits, first=(l == 0), last=last)
            cur_x = XB
        p.barrier()
        p.emit()
    return nc


def mm(p, out, lhsT, rhs, start=True, stop=True, reads=(), writes=()):
    return p.op("pe", lambda e: e.matmul(out, lhsT=lhsT, rhs=rhs, start=start, stop=stop), reads, writes)


def trp(p, out, in_, ident, reads=(), writes=()):
    return p.op("pe", lambda e: e.transpose(out=out, in_=in_, identity=ident), reads, writes)


def act(p, out, in_, func, reads=(), writes=(), bias=None, scale=None):
    kw = {}
    if bias is not None:
        kw["bias"] = bias
    if scale is not None:
        kw["scale"] = scale
    return p.op("act", lambda e: e.activation(out=out, in_=in_, func=func, **kw), reads, writes)


def ts(p, eng, out, in0, s1, s2, op0, op1=None, reads=(), writes=()):
    if op1 is None:
        return p.op(eng, lambda e: e.tensor_scalar(out=out, in0=in0, scalar1=s1, scalar2=None, op0=op0), reads, writes)
    return p.op(eng, lambda e: e.tensor_scalar(out=out, in0=in0, scalar1=s1, scalar2=s2, op0=op0, op1=op1), reads, writes)


def stt(p, out, in0, scalar, in1, op0, op1, reads=(), writes=()):
    return p.op("dve", lambda e: e.scalar_tensor_tensor(out=out, in0=in0, scalar=scalar, in1=in1, op0=op0, op1=op1),
                reads, writes)


def tt_(p, eng, out, in0, in1, op, reads=(), writes=()):
    return p.op(eng, lambda e: e.tensor_tensor(out=out, in0=in0, in1=in1, op=op), reads, writes)


def cp(p, eng, out, in_, reads=(), writes=()):
    if eng == "act":
        return p.op("act", lambda e: e.activation(out=out, in_=in_, func=AF.Copy), reads, writes)
    return p.op(eng, lambda e: e.tensor_copy(out=out, in_=in_), reads, writes)


def ms(p, eng, ap, val, writes=()):
    return p.op(eng, lambda e: e.memset(ap, val), (), writes)


def dm(p, eng, out, in_, reads=(), writes=(), **kw):
    return p.dma(eng, lambda e: e.dma_start(out=out, in_=in_, **kw), reads, writes)


def xt_keys(t0, n):
    return [("xT", t) for t in range(t0, t0 + n)]


def proj_fm(p, ps, xT, wt, wkey, ncols, evac):
    for q in range(4):
        b = q % 2
        pk = "ps%d" % b
        for dc in range(16):
            mm(p, ps[b][0:ncols, :], wt[:, dc, :], xT[:, dc, q * 512:(q + 1) * 512], dc == 0, dc == 15,
               reads=[wkey] + xt_keys(q * 4, 4), writes=[pk])
        evac(q, ps[b][0:ncols, :], pk)


def proj_tm(p, ps, xT, wt, wkey, ncols, tile, bank):
    pk = "ps%d" % bank
    for dc in range(16):
        mm(p, ps[bank][:, 0:ncols], xT[:, dc, tile * 128:(tile + 1) * 128], wt[:, dc, :], dc == 0, dc == 15,
           reads=[wkey, ("xT", tile)], writes=[pk])
    return ps[bank][:, 0:ncols], pk


def stage_mlstm(nc, p, ar, ps, psb, xT, XT_ALL, identb, identf, onesf, wl, C, YST, load_w_fm, bcast_load,
                layer_norm_rows):
    p.barrier()
    ar.reset()
    w_in = wl["w_in"]
    qkT = ar.bf(4 * S).rearrange("p (c t) -> p c t", t=S)
    ktok = ar.bf(NT * 256).rearrange("p (a b) -> p a b", b=256)
    vaug = ar.bf(NT * 4 * 129).rearrange("p (a h v) -> p a h v", h=4, v=129)
    convw = ar.f32(12)
    gb = ar.f32(16)
    normw = ar.f32(512)
    tri = [ar.f32(128), ar.f32(128)]
    mneg = [ar.f32(128), ar.f32(128)]
    gtok = ar.f32(NT * 16).rearrange("p (a b) -> p a b", b=16)
    G = {k: ar.f32(128) for k in ("lf", "ig", "b", "tot", "imb", "wk", "dec", "ebt", "tmp")}
    mark = ar.off
    for j in range(3):
        dm(p, "sync", convw[:, j * 4:(j + 1) * 4], wl["ml_conv_w"][j].rearrange("(c q) -> q c", q=128),
           writes=["convw"], allow_slow_non_contiguous=True)
    bcast_load(gb, "gb", wl["ml_gate_b"], 16)
    bcast_load(normw, "normw", wl["ml_norm_w"], 512)
    dm(p, "sync", tri[0], C["tri_f"], writes=["tri0"])
    dm(p, "sync", tri[1], C["tri_b"], writes=["tri1"])
    dm(p, "sync", mneg[0], C["mneg_f"], writes=["mneg0"])
    dm(p, "sync", mneg[1], C["mneg_b"], writes=["mneg1"])
    wqk = ar.bf(16 * 128).rearrange("p (a b) -> p a b", b=128)
    zqk = ar.f32(2050)
    ctmp = ar.f32(2048)
    wv = ar.bf(16 * 512).rearrange("p (a b) -> p a b", b=512)
    wg = ar.bf(16 * 16).rearrange("p (a b) -> p a b", b=16)
    ms(p, "pool", zqk[:, 0:1], 0.0, writes=["zqk"])
    ms(p, "pool", zqk[:, 2049:2050], 0.0, writes=["zqk"])
    for ch in range(4):
        load_w_fm(wqk, "wqk", w_in, OFF_MQ + ch * 128, 128)

        def evac(q, pap, pk):
            cp(p, "act", zqk[:, 1 + q * 512:1 + (q + 1) * 512], pap, reads=[pk], writes=["zqk"])
        proj_fm(p, ps, xT, wqk, "wqk", 128, evac)
        ts(p, "dve", ctmp, zqk[:, 0:2048], convw[:, ch:ch + 1], None, ALU.mult, reads=["zqk", "convw"], writes=["ctmp"])
        stt(p, ctmp, zqk[:, 1:2049], convw[:, 4 + ch:5 + ch], ctmp, ALU.mult, ALU.add, reads=["zqk", "convw", "ctmp"], writes=["ctmp"])
        stt(p, ctmp, zqk[:, 2:2050], convw[:, 8 + ch:9 + ch], ctmp, ALU.mult, ALU.add, reads=["zqk", "convw", "ctmp"], writes=["ctmp"])
        act(p, qkT[:, ch, :], ctmp, AF.Silu, reads=["ctmp"], writes=[("qkT", ch)])
        if ch < 2:
            ts(p, "pool", qkT[:, ch, :], qkT[:, ch, :], 0.125, None, ALU.mult, reads=[("qkT", ch)], writes=[("qkT", ch)])
    for tile in range(NT):
        b = 2 + tile % 2
        pk = "ps%d" % b
        for kc in range(2):
            trp(p, psb(b)[:, kc * 128:(kc + 1) * 128], qkT[:, 2 + kc, tile * 128:(tile + 1) * 128], identb[:],
                reads=[("qkT", 2 + kc), "identb"], writes=[pk])
        cp(p, "dve", ktok[:, tile, :], psb(b)[:, 0:256], reads=[pk], writes=["ktok"])
    load_w_fm(wv, "wv", w_in, OFF_MV, 512)
    load_w_fm(wg, "wg", w_in, OFF_MG, 16)
    ms(p, "pool", vaug[:, :, :, 128:129], 1.0, writes=["vaug"])
    for tile in range(NT):
        b = 4 + tile % 2
        pap, pk = proj_tm(p, ps, xT, wv, "wv", 512, tile, b)
        cp(p, "act", vaug[:, tile, :, 0:128], pap.rearrange("p (h v) -> p h v", v=128), reads=[pk], writes=["vaug"])
        b2 = 6 + tile % 2
        pap2, pk2 = proj_tm(p, ps, xT, wg, "wg", 16, tile, b2)
        tt_(p, "dve", gtok[:, tile, :], pap2, gb, ALU.add, reads=[pk2, "gb"], writes=["gtok"])
    gv = gtok.rearrange("p a (d y h) -> p d a y h", d=2, y=2, h=4)

    def g4(t):
        return t.rearrange("p (d a h) -> p d a h", d=2, h=4)
    cp(p, "dve", g4(G["ig"]), gv[:, :, :, 0, :], reads=["gtok"], writes=["ig"])
    act(p, g4(G["tmp"]), gv[:, :, :, 1, :], AF.Exp, reads=["gtok"], writes=["gtmp"], scale=-1.0)
    act(p, G["tmp"], G["tmp"], AF.Ln, reads=["gtmp"], writes=["gtmp"], bias=1.0)
    ts(p, "dve", G["lf"], G["tmp"], -1.0, None, ALU.mult, reads=["gtmp"], writes=["lf"])
    for d in range(2):
        mm(p, ps[0][:, d * 64:(d + 1) * 64], tri[d], G["lf"][:, d * 64:(d + 1) * 64], True, True,
           reads=["tri%d" % d, "lf"], writes=["ps0"])
    cp(p, "dve", G["b"], ps[0][:, 0:128], reads=["ps0"], writes=["gb_"])
    mm(p, ps[1][:, 0:128], onesf[:], G["lf"], True, True, reads=["onesf", "lf"], writes=["ps1"])
    cp(p, "dve", G["tot"], ps[1][:, 0:128], reads=["ps1"], writes=["tot"])
    tt_(p, "dve", G["imb"], G["ig"], G["b"], ALU.subtract, reads=["ig", "gb_"], writes=["imb"])
    tt_(p, "dve", G["tmp"], G["tot"], G["imb"], ALU.add, reads=["tot", "imb", "gtmp"], writes=["gtmp"])
    act(p, G["wk"], G["tmp"], AF.Exp, reads=["gtmp"], writes=["wk"])
    act(p, G["dec"], G["tot"], AF.Exp, reads=["tot"], writes=["dec"])
    act(p, G["ebt"], G["b"], AF.Exp, reads=["gb_"], writes=["ebt"])

    if STOP[0] == "ml1":
        return
    p.barrier()
    ar.off = mark
    hsum = ar.f32(NT * 512).rearrange("p (a h v) -> p a h v", h=4, v=128)
    mark2 = ar.off
    CT = ar.f32(8 * 129).rearrange("p (u v) -> p u v", v=129)
    CTb = ar.bf(8 * 130).rearrange("p (u v) -> p u v", v=130)
    LAGM = 2
    NS = LAGM + 1
    Rt = [ar.f32(128) for _ in range(NS)]
    AT = [ar.f32(128) for _ in range(NS)]
    ST = [ar.bf(128) for _ in range(NS)]
    nsb = [ar.f32(132) for _ in range(2)]
    ddt = [ar.f32(4) for _ in range(2)]
    kw = [[ar.bf(128) for _ in range(2)] for _ in range(2)]
    for sidx in range(2):
        for hh in range(2):
            ms(p, "pool", kw[sidx][hh], 0.0, writes=[("kw", sidx, hh)])
    units = []
    for d in range(2):
        order = list(range(NT)) if d == 0 else list(range(NT - 1, -1, -1))
        for ci, c in enumerate(order):
            for h in range(4):
                units.append((d, ci, c, h))
    units = units[:int(os.environ.get("MLU", "100000"))]

    def mfront(i):
        d, ci, c, h = units[i]
        sl = i % NS
        bD, bS = 2 * sl, 2 * sl + 1
        kD, kS = "ps%d" % bD, "ps%d" % bS
        col = d * 64 + c * 4 + h
        hh, pc = h % 2, h // 2
        rows = slice(hh * 64, (hh + 1) * 64)
        tsl = slice(c * 128, (c + 1) * 128)
        ts(p, "pool", Rt[sl], tri[d], G["lf"][:, col:col + 1], None, ALU.mult, reads=["tri%d" % d, "lf"], writes=[("Rt", sl)])
        mm(p, ps[bD][:, 0:128], onesf[:], Rt[sl], True, False, reads=["onesf", ("Rt", sl)], writes=[kD])
        mm(p, ps[bD][:, 0:128], identf[:], mneg[d], False, True, reads=["identf", "mneg%d" % d], writes=[kD])
        act(p, AT[sl], ps[bD][:, 0:128], AF.Exp, reads=[kD, "imb"], writes=[("AT", sl)], bias=G["imb"][:, col:col + 1])
        mm(p, ps[bS][:, 0:128], qkT[rows, 2 + pc, tsl], qkT[rows, pc, tsl], True, True,
           reads=[("qkT", 2 + pc), ("qkT", pc)], writes=[kS])
        tt_(p, "dve", ST[sl], ps[bS][:, 0:128], AT[sl], ALU.mult, reads=[kS, ("AT", sl)], writes=[("ST", sl)])

    def mback(i):
        d, ci, c, h = units[i]
        sl = i % NS
        s2 = i % 2
        col = d * 64 + c * 4 + h
        hh, pc = h % 2, h // 2
        rows = slice(hh * 64, (hh + 1) * 64)
        u = d * 4 + h
        tsl = slice(c * 128, (c + 1) * 128)
        mm(p, ps[6][:, 0:129], ST[sl], vaug[:, c, h, :], True, True, reads=[("ST", sl), "vaug"], writes=["ps6"])
        cp(p, "act", nsb[s2][:, 0:129], ps[6][:, 0:129], reads=["ps6"], writes=[("nsb", s2)])
        if ci > 0:
            mm(p, ps[7][:, 0:129], qkT[rows, pc, tsl], CTb[rows, u, 0:129], True, True,
               reads=[("qkT", pc), ("CTb", u)], writes=["ps7"])
            stt(p, nsb[s2][:, 0:129], ps[7][:, 0:129], G["ebt"][:, col:col + 1], nsb[s2][:, 0:129], ALU.mult, ALU.add,
                reads=["ps7", "ebt", ("nsb", s2)], writes=[("nsb", s2)])
        dd = ddt[s2]
        stt(p, dd[:, 0:1], nsb[s2][:, 128:129], -1.0, nsb[s2][:, 128:129], ALU.mult, ALU.max, reads=[("nsb", s2)], writes=[("dd", s2)])
        ts(p, "dve", dd[:, 1:2], dd[:, 0:1], 1.0, None, ALU.max, reads=[("dd", s2)], writes=[("dd", s2)])
        p.op("dve", lambda e, dd=dd: e.reciprocal(out=dd[:, 2:3], in_=dd[:, 1:2]), reads=[("dd", s2)], writes=[("dd", s2)])
        if d == 0:
            ts(p, "dve", hsum[:, c, h, :], nsb[s2][:, 0:128], dd[:, 2:3], None, ALU.mult,
               reads=[("nsb", s2), ("dd", s2)], writes=[("hsum", c, h)])
        else:
            stt(p, hsum[:, c, h, :], nsb[s2][:, 0:128], dd[:, 2:3], hsum[:, c, h, :], ALU.mult, ALU.add,
                reads=[("nsb", s2), ("dd", s2), ("hsum", c, h)], writes=[("hsum", c, h)])
        if ci < NT - 1:
            ts(p, "pool", kw[s2][hh][:, rows], ktok[:, c, h * 64:(h + 1) * 64], G["wk"][:, col:col + 1], None, ALU.mult,
               reads=["ktok", "wk"], writes=[("kw", s2, hh)])
            mm(p, ps[6][:, 256:385], kw[s2][hh], vaug[:, c, h, :], True, True, reads=[("kw", s2, hh), "vaug"], writes=["ps6"])
            if ci == 0:
                cp(p, "dve", CT[rows, u, :], ps[6][rows, 256:385], reads=["ps6"], writes=[("CT", u)])
            else:
                stt(p, CT[rows, u, :], CT[rows, u, :], G["dec"][rows, col:col + 1], ps[6][rows, 256:385], ALU.mult, ALU.add,
                    reads=["ps6", "dec", ("CT", u)], writes=[("CT", u)])
            cp(p, "act", CTb[rows, u, 0:129], CT[rows, u, :], reads=[("CT", u)], writes=[("CTb", u)])

    nu_ = len(units)
    for i in range(nu_ + LAGM):
        if i < nu_:
            mfront(i)
        if i >= LAGM:
            mback(i - LAGM)
    if STOP[0] == "ml2":
        return
    p.barrier()
    ar.off = mark2
    wo = ar.bf(16 * 512).rearrange("p (a b) -> p a b", b=512)
    yT = ar.bf(4 * S).rearrange("p (c t) -> p c t", t=S)
    osig = [ar.f32(512) for _ in range(2)]
    hn = [ar.f32(512) for _ in range(2)]
    ybf = [ar.bf(512) for _ in range(2)]
    stats = ar.f32(24)
    mv = ar.f32(16)
    load_w_fm(wo, "wo", w_in, OFF_MO, 512)
    for tile in range(NT):
        s2 = tile % 2
        pap, pk = proj_tm(p, ps, xT, wo, "wo", 512, tile, s2)
        act(p, osig[s2], pap, AF.Sigmoid, reads=[pk], writes=[("osig", s2)])
        for h in range(4):
            layer_norm_rows(hsum[:, tile, h, :], ("hsum", tile, h), hn[s2][:, h * 128:(h + 1) * 128], ("hn", s2), 128,
                            None, None, None, stats[:, h * 6:(h + 1) * 6], mv[:, h * 4:(h + 1) * 4], "mlln%d" % h)
        tt_(p, "pool", hn[s2], hn[s2], normw, ALU.mult, reads=[("hn", s2), "normw"], writes=[("hn", s2)])
        tt_(p, "pool", ybf[s2], hn[s2], osig[s2], ALU.mult, reads=[("hn", s2), ("osig", s2)], writes=[("ybf", s2)])
        b = 2 + s2
        pk2 = "ps%d" % b
        for c4 in range(4):
            trp(p, psb(b)[:, c4 * 128:(c4 + 1) * 128], ybf[s2][:, c4 * 128:(c4 + 1) * 128], identb[:],
                reads=[("ybf", s2), "identb"], writes=[pk2])
        cp(p, "act", yT[:, :, tile * 128:(tile + 1) * 128], psb(b)[:, 0:512].rearrange("p (c t) -> p c t", t=128),
           reads=[pk2], writes=["yT"])
    for c4 in range(4):
        dm(p, "sync", YST[0, c4], yT[:, c4, :], reads=["yT"], writes=["YST0"])


def stage_attn(nc, p, ar, ps, psb, xT, XT_ALL, identb, wl, C, YST, load_w_fm):
    p.barrier()
    ar.reset()
    w_in = wl["w_in"]
    dist = ar.f32(25 * 128).rearrange("p (a b) -> p a b", b=128)
    dm(p, "sync", dist, C["dist"], writes=["dist"])
    qT = ar.bf(3 * S).rearrange("p (g t) -> p g t", t=S)
    kT = ar.bf(3 * S).rearrange("p (g t) -> p g t", t=S)
    vat = ar.bf(NT * 3 * 2 * 65).rearrange("p (a g h v) -> p a g h v", g=3, h=2, v=65)
    wch = [ar.bf(16 * 128).rearrange("p (a b) -> p a b", b=128) for _ in range(2)]
    wv3 = ar.bf(16 * 384).rearrange("p (a b) -> p a b", b=384)
    yT = ar.bf(S)
    LAG = 5
    NL = LAG + 2
    Lt = [ar.f32(512) for _ in range(NL)]
    Pt = [ar.bf(512) for _ in range(NL)]
    ytile = [ar.bf(128) for _ in range(2)]
    rd = [ar.f32(2) for _ in range(2)]
    base = (0, 3, 8)
    ms(p, "pool", vat[:, :, :, :, 64:65], 1.0, writes=["vat"])
    wi = 0
    for hp in range(4):
        for g in range(3):
            for which, dstT, off in (("q", qT, OFF_AQ), ("k", kT, OFF_AK)):
                wb = wch[wi % 2]
                wk_ = ("wch", wi % 2)
                wi += 1
                load_w_fm(wb, wk_, w_in, off + (g * 4 + hp) * 128, 128)

                def evac(q, pap, pk, dstT=dstT, g=g, which=which):
                    cp(p, "act", dstT[:, g, q * 512:(q + 1) * 512], pap, reads=[pk], writes=[(which + "T", g)])
                proj_fm(p, ps, xT, wb, wk_, 128, evac)
            p.dma("pool", lambda e, g=g, hp=hp: e.dma_start(
                out=wv3[:, :, g * 128:(g + 1) * 128],
                in_=w_in[:, OFF_AV + (g * 8 + 2 * hp) * 64:OFF_AV + (g * 8 + 2 * hp) * 64 + 128].rearrange("(dc q) c -> q dc c", q=128)),
                writes=["wv3"])
        for tile in range(NT):
            b = 2 + tile % 2
            pap, pk = proj_tm(p, ps, xT, wv3, "wv3", 384, tile, b)
            cp(p, "dve", vat[:, tile, :, :, 0:64], pap.rearrange("p (g h v) -> p g h v", g=3, h=2), reads=[pk], writes=["vat"])
        batches = []
        for qt in range(NT):
            for h2 in range(2):
                units = []
                for g in range(3):
                    kbs = list(range(max(0, qt - ATT_R[g]), min(NT - 1, qt + ATT_R[g]) + 1))
                    for i0 in range(0, len(kbs), 4):
                        units.append((g, kbs[i0:i0 + 4]))
                for ui, (g, kbs) in enumerate(units):
                    batches.append((qt, h2, g, kbs, ui == 0, ui == len(units) - 1))

        def front(i):
            qt, h2, g, kbs, first, last = batches[i]
            qs = slice(qt * 128, (qt + 1) * 128)
            head = 2 * hp + h2
            rows = slice(h2 * 64, (h2 + 1) * 64)
            n = len(kbs)
            sb_ = i % 4
            sk = "ps%d" % sb_
            sl = i % NL
            for j, kb in enumerate(kbs):
                mm(p, ps[sb_][:, j * 128:(j + 1) * 128], kT[rows, g, kb * 128:(kb + 1) * 128], qT[rows, g, qs], True, True,
                   reads=[("kT", g), ("qT", g)], writes=[sk])
            idx0 = base[g] + (kbs[0] - qt) + ATT_R[g]
            stt(p, Lt[sl][:, 0:n * 128], dist[:, idx0:idx0 + n, :].rearrange("p a b -> p (a b)"),
                -8.0 * alibi_slope(g, head), ps[sb_][:, 0:n * 128], ALU.mult, ALU.add,
                reads=["dist", sk], writes=[("Lt", sl)])
            act(p, Pt[sl][:, 0:n * 128], Lt[sl][:, 0:n * 128], AF.Exp, reads=[("Lt", sl)], writes=[("Pt", sl)], scale=0.125)

        def back(i):
            qt, h2, g, kbs, first, last = batches[i]
            qs = slice(qt * 128, (qt + 1) * 128)
            sl = i % NL
            ab = 6 + h2
            ak = "ps%d" % ab
            n = len(kbs)
            for j, kb in enumerate(kbs):
                mm(p, ps[ab][:, 0:65], Pt[sl][:, j * 128:(j + 1) * 128], vat[:, kb, g, h2, :], first and j == 0, last and j == n - 1,
                   reads=[("Pt", sl), "vat"], writes=[ak])
            if last:
                p.op("dve", lambda e, ab=ab, h2=h2: e.reciprocal(out=rd[h2][:, 0:1], in_=ps[ab][:, 64:65]), reads=[ak], writes=[("rd", h2)])
                ts(p, "dve", ytile[qt % 2][:, h2 * 64:(h2 + 1) * 64], ps[ab][:, 0:64], rd[h2][:, 0:1], None, ALU.mult,
                   reads=[ak, ("rd", h2)], writes=[("ytile", qt % 2)])
                if h2 == 1:
                    tb = 4 + qt % 2
                    trp(p, psb(tb)[:, 0:128], ytile[qt % 2], identb[:], reads=[("ytile", qt % 2), "identb"], writes=["ps%d" % tb])
                    cp(p, "act", yT[:, qs], psb(tb)[:, 0:128], reads=["ps%d" % tb], writes=["yTa"])

        nb_ = len(batches)
        for i in range(nb_ + LAG):
            if i < nb_:
                front(i)
            if i >= LAG:
                back(i - LAG)
        dm(p, "sync", YST[1, hp], yT, reads=["yTa"], writes=["YST1"])


def stage_sg(nc, p, ar, ps, psb, xT, XT_ALL, identb, identf, wl, C, YST, load_w_fm, bcast_load, layer_norm_rows):
    p.barrier()
    ar.reset()
    w_in = wl["w_in"]
    wu = ar.bf(16 * 512).rearrange("p (a b) -> p a b", b=512)
    wv = ar.bf(16 * 512).rearrange("p (a b) -> p a b", b=512)
    lng = ar.f32(512)
    lnb = ar.f32(512)
    wsf = ar.f32(512).rearrange("p (g s) -> p g s", s=128)
    wsT = ar.bf(512).rearrange("p (g t) -> p g t", t=128)
    bs = ar.f32(4)
    yT = ar.bf(4 * S).rearrange("p (c t) -> p c t", t=S)
    u = [ar.f32(512) for _ in range(2)]
    v = [ar.f32(512) for _ in range(2)]
    vn = [ar.bf(512) for _ in range(2)]
    ysg = [ar.bf(512) for _ in range(2)]
    stats = ar.f32(8)
    mv = ar.f32(4)
    load_w_fm(wu, "wu", w_in, OFF_SU, 512)
    load_w_fm(wv, "wv", w_in, OFF_SV, 512)
    bcast_load(lng, "lng", wl["sg_ln_g"], 512)
    bcast_load(lnb, "lnb", wl["sg_ln_b"], 512)
    dm(p, "sync", wsf, wl["sg_w"].rearrange("g t s -> t g s"), writes=["wsf"])
    dm(p, "sync", bs, wl["sg_b"].rearrange("g t -> t g"), writes=["bs"], allow_slow_non_contiguous=True)
    for g in range(4):
        trp(p, ps[7][:, g * 128:(g + 1) * 128], wsf[:, g, :], identf[:], reads=["wsf", "identf"], writes=["ps7"])
    cp(p, "dve", wsT.rearrange("p g t -> p (g t)"), ps[7][:, 0:512], reads=["ps7"], writes=["wsT"])
    for tile in range(NT):
        s2 = tile % 2
        pap, pk = proj_tm(p, ps, xT, wu, "wu", 512, tile, 0 + s2)
        act(p, u[s2], pap, AF.Gelu, reads=[pk], writes=[("u", s2)])
        pap, pk = proj_tm(p, ps, xT, wv, "wv", 512, tile, 2 + s2)
        act(p, v[s2], pap, AF.Gelu, reads=[pk], writes=[("v", s2)])
        layer_norm_rows(v[s2], ("v", s2), v[s2], ("v", s2), 512, lng, lnb, ("lng", "lnb"), stats, mv, "sgln")
        cp(p, "pool", vn[s2], v[s2], reads=[("v", s2)], writes=[("vn", s2)])
        b = 4 + s2
        pk = "ps%d" % b
        for g in range(4):
            mm(p, ps[b][:, g * 128:(g + 1) * 128], wsT[:, g, :], vn[s2][:, g * 128:(g + 1) * 128], True, True,
               reads=["wsT", ("vn", s2)], writes=[pk])
        for g in range(4):
            stt(p, ysg[s2][:, g * 128:(g + 1) * 128], ps[b][:, g * 128:(g + 1) * 128], bs[:, g:g + 1], u[s2][:, g * 128:(g + 1) * 128],
                ALU.add, ALU.mult, reads=[pk, "bs", ("u", s2)], writes=[("ysg", s2)])
        b2 = 6
        for c4 in range(4):
            trp(p, psb(b2)[:, c4 * 128:(c4 + 1) * 128], ysg[s2][:, c4 * 128:(c4 + 1) * 128], identb[:], reads=[("ysg", s2), "identb"], writes=["ps6"])
        cp(p, "act", yT[:, :, tile * 128:(tile + 1) * 128], psb(b2)[:, 0:512].rearrange("p (c t) -> p c t", t=128), reads=["ps6"], writes=["yT"])
    for c4 in range(4):
        dm(p, "sync", YST[2, c4], yT[:, c4, :], reads=["yT"], writes=["YST2"])


def stage_pool(nc, p, ar, ps, psb, xT, XT_ALL, wl, C, YST, load_w_fm):
    p.barrier()
    ar.reset()
    w_in = wl["w_in"]
    PADW = S + 32
    wch = [ar.bf(16 * 128).rearrange("p (a b) -> p a b", b=128) for _ in range(2)]
    wp = ar.bf(512).rearrange("p (g d) -> p g d", d=128)
    psc = ar.f32(4)
    edge = ar.f32(64).rearrange("p (g s j) -> p g s j", s=2, j=8)
    pp = ar.f32(PADW)
    A = [ar.f32(PADW) for _ in range(2)]
    dT = ar.bf(S)
    yT = ar.bf(S)
    p.dma("pool", lambda e: e.dma_start(out=wp, in_=wl["pool_w"].rearrange("g c d -> c g d")), writes=["wp"])
    dm(p, "sync", psc, wl["pool_scale"].rearrange("(g q) -> q g", q=128), writes=["psc"], allow_slow_non_contiguous=True)
    dm(p, "sync", edge.rearrange("p g s j -> p (g s j)"), C["pool_edge"], writes=["edge"])
    ms(p, "pool", pp, 0.0, writes=["pp"])
    ms(p, "pool", A[0], 0.0, writes=["A0"])
    ms(p, "pool", A[1], 0.0, writes=["A1"])
    O = 16
    E0, EN = O - 8, S + 16
    for g in range(4):
        wb = wch[g % 2]
        wk_ = ("wch", g % 2)
        load_w_fm(wb, wk_, w_in, OFF_PP + g * 128, 128)

        def evac(q, pap, pk):
            cp(p, "act", pp[:, O + q * 512:O + (q + 1) * 512], pap, reads=[pk], writes=["pp"])
        proj_fm(p, ps, xT, wb, wk_, 128, evac)
        tt_(p, "pool", A[0][:, E0:E0 + EN], pp[:, E0 - 1:E0 - 1 + EN], pp[:, E0:E0 + EN], ALU.add, reads=["pp", "A0"], writes=["A0"])
        cur = 0
        sh = 1
        for step in range(g):
            nxt = 1 - cur
            tt_(p, "pool", A[nxt][:, E0:E0 + EN], A[cur][:, E0 - sh:E0 - sh + EN], A[cur][:, E0 + sh:E0 + sh + EN], ALU.add,
                reads=["A%d" % cur, "A%d" % nxt], writes=["A%d" % nxt])
            cur = nxt
            sh *= 2
        w = (2, 4, 8, 16)[g]
        hw = w // 2
        stt(p, dT[:, :], A[cur][:, O:O + S], 1.0 / w, pp[:, O:O + S], ALU.mult, ALU.subtract, reads=["A%d" % cur, "pp"], writes=["dT"])
        for side, c0 in ((0, 0), (1, S - hw)):
            tt_(p, "dve", A[cur][:, O + c0:O + c0 + hw], A[cur][:, O + c0:O + c0 + hw], edge[:, g, side, 0:hw], ALU.mult,
                reads=["A%d" % cur, "edge", "dT"], writes=["A%d" % cur])
            tt_(p, "dve", dT[:, c0:c0 + hw], A[cur][:, O + c0:O + c0 + hw], pp[:, O + c0:O + c0 + hw], ALU.subtract,
                reads=["A%d" % cur, "pp"], writes=["dT"])
        for q in range(4):
            b = 2 + q % 2
            pk = "ps%d" % b
            mm(p, ps[b][:, :], wp[:, g, :], dT[:, q * 512:(q + 1) * 512], True, True, reads=["wp", "dT"], writes=[pk])
            act(p, yT[:, q * 512:(q + 1) * 512], ps[b][:, :], AF.Identity, reads=[pk, "psc"], writes=["yT"], scale=psc[:, g:g + 1])
        dm(p, "sync", YST[3, g], yT, reads=["yT"], writes=["YST3"])


def stage_merge(nc, p, ar, ps, psb, xT, XT_ALL, identf, wl, C, YST, MT, cur_x, XA, load_w_fm, bcast_load,
                layer_norm_rows, tile_to_xT, logits=None):
    p.barrier()
    ar.reset()
    ystb = [ar.bf(4 * S).rearrange("p (k t) -> p k t", k=4) for _ in range(2)]
    wg = [ar.bf(16 * 512).rearrange("p (k c) -> p k c", c=512) for _ in range(2)]
    wb = [ar.bf(4 * 512).rearrange("p (k c) -> p k c", c=512) for _ in range(2)]
    bg = ar.f32(64)
    gsb = [ar.f32(512) for _ in range(2)]
    macc = [[ar.f32(512) for _ in range(4)] for _ in range(4)]
    tmpm = [ar.f32(512) for _ in range(2)]
    mTb = [ar.bf(512) for _ in range(2)]
    bgr = ar.f32(128)
    dm(p, "sync", bgr[0:64, :], wl["b_gate"].rearrange("(c q) -> c q", q=128), writes=["bgr"])
    trp(p, ps[7][:, 0:64], bgr[0:64, :], identf[0:64, 0:64], reads=["bgr", "identf"], writes=["ps7"])
    cp(p, "dve", bg, ps[7][:, 0:64], reads=["ps7"], writes=["bg"])
    w_gate, w_branch = wl["w_gate"], wl["w_branch"]
    it = 0
    for dcg in range(4):
        for n in range(4):
            b = it % 2
            it += 1
            c0 = n * D + dcg * 512
            p.dma("pool", lambda e, c0=c0, b=b: e.dma_start(
                out=wg[b], in_=w_gate[:, c0:c0 + 512].rearrange("(k q) c -> q k c", q=128)), writes=[("wg", b)])
            p.dma("pool", lambda e, n=n, dcg=dcg, b=b: e.dma_start(
                out=wb[b], in_=w_branch[n, :, dcg * 512:(dcg + 1) * 512].rearrange("(k q) c -> q k c", q=128)), writes=[("wb", b)])
            for k in range(4):
                dm(p, "sync", ystb[b][:, k, :], YST[n, k], writes=[("yst", b)])
            for dl in range(4):
                dc = dcg * 4 + dl
                cs = slice(dl * 128, (dl + 1) * 128)
                for q in range(4):
                    tq = slice(q * 512, (q + 1) * 512)
                    ga = (dl * 4 + q) % 2
                    gk = "ps%d" % ga
                    for k in range(16):
                        mm(p, ps[ga][:, :], wg[b][:, k, cs], xT[:, k, tq], k == 0, k == 15, reads=[("wg", b)] + xt_keys(q * 4, 4), writes=[gk])
                    act(p, gsb[ga], ps[ga][:, :], AF.Sigmoid, reads=[gk, "bg"], writes=[("gsb", ga)], bias=bg[:, n * 16 + dc:n * 16 + dc + 1])
                    pa = 2 + ga
                    pk = "ps%d" % pa
                    for k in range(4):
                        mm(p, ps[pa][:, :], wb[b][:, k, cs], ystb[b][:, k, tq], k == 0, k == 3, reads=[("wb", b), ("yst", b)], writes=[pk])
                    mk = ("macc", dl, q)
                    if n == 0:
                        tt_(p, "dve", macc[dl][q], ps[pa][:, :], gsb[ga], ALU.mult, reads=[pk, ("gsb", ga)], writes=[mk])
                    else:
                        tt_(p, "dve", tmpm[ga], ps[pa][:, :], gsb[ga], ALU.mult, reads=[pk, ("gsb", ga)], writes=[("tmpm", ga)])
                        if n < 3:
                            tt_(p, "pool", macc[dl][q], macc[dl][q], tmpm[ga], ALU.add, reads=[mk, ("tmpm", ga)], writes=[mk])
                        else:
                            tt_(p, "pool", mTb[ga], macc[dl][q], tmpm[ga], ALU.add, reads=[mk, ("tmpm", ga)], writes=[("mTb", ga)])
                            dm(p, "sync", MT[dc, :, tq], mTb[ga], reads=[("mTb", ga)], writes=["MT"])

    if STOP[0] == "mg1":
        return
    p.barrier()
    ar.reset()
    wout = ar.bf(16 * D).rearrange("p (k c) -> p k c", c=D)
    lng = ar.f32(D)
    lnb = ar.f32(D)
    mt = [ar.bf(16 * 128).rearrange("p (k t) -> p k t", t=128) for _ in range(2)]
    xres = [ar.f32(D) for _ in range(2)]
    sres = xres
    stats = ar.f32(24)
    mv = ar.f32(4)
    wr = ar.f32(16 * 36).rearrange("p (k c) -> p k c", c=36)
    whb = ar.bf(16 * 36).rearrange("p (k c) -> p k c", c=36)
    wlb = ar.bf(16 * 36).rearrange("p (k c) -> p k c", c=36)
    wtmp = ar.f32(16 * 36).rearrange("p (k c) -> p k c", c=36)
    brb = ar.f32(36)
    hb = [ar.bf(D) for _ in range(2)]
    lob = ar.bf(D)
    loT = ar.bf(16 * 128).rearrange("p (k t) -> p k t", t=128)
    for oc in range(4):
        p.dma("pool", lambda e, oc=oc: e.dma_start(out=wout[:, :, oc * 512:(oc + 1) * 512],
                                                   in_=wl["w_out"][:, oc * 512:(oc + 1) * 512].rearrange("(k q) c -> q k c", q=128)),
              writes=["wout"])
    bcast_load(lng, "lng", wl["ln1_g"], D)
    bcast_load(lnb, "lnb", wl["ln1_b"], D)
    dm(p, "sync", wr.rearrange("p k c -> p (k c)"), wl["w_router"], writes=["wr"])
    bcast_load(brb, "brb", wl["b_router"], 36)
    cp(p, "dve", whb, wr, reads=["wr"], writes=["whb"])
    tt_(p, "dve", wtmp, wr, whb, ALU.subtract, reads=["wr", "whb"], writes=["wtmp"])
    cp(p, "dve", wlb, wtmp, reads=["wtmp"], writes=["wlb"])
    P2 = int(os.environ.get("P2STOP", "99"))
    for tile in range(NT):
        b = tile % 2
        rs = slice(tile * 128, (tile + 1) * 128)
        dm(p, "sync", mt[b], MT[:, :, rs].rearrange("k q t -> q k t"), reads=["MT"], writes=[("mt", b)])
        dm(p, "sync", xres[b], cur_x[rs, :], writes=[("sres", b)])
        for oc in range(4):
            ok = "ps%d" % oc
            for k in range(16):
                mm(p, ps[oc][:, :], mt[b][:, k, :], wout[:, k, oc * 512:(oc + 1) * 512], k == 0, k == 15, reads=[("mt", b), "wout"], writes=[ok])
            stt(p, sres[b][:, oc * 512:(oc + 1) * 512], xres[b][:, oc * 512:(oc + 1) * 512], ALPHA, ps[oc][:, :], ALU.mult, ALU.add,
                reads=[ok, ("sres", b)], writes=[("sres", b)])
        layer_norm_rows(sres[b], ("sres", b), sres[b], ("sres", b), D, lng, lnb, ("lng", "lnb"), stats, mv, "ln1")
        dm(p, "sync", XA[rs, :], sres[b], reads=[("sres", b)], writes=["XA"])
        if P2 <= 4:
            continue
        tsl = slice(tile * 128, (tile + 1) * 128)
        tile_to_xT(sres[b], ("sres", b), tile, 4, hb[b], ("hb", b), lo=((lob, "lob", loT, "loT") if logits is not None else None))
        if P2 <= 5:
            continue
        if logits is not None:
            n = 0
            for k in range(16):
                for a_, ak_, w_, wk_ in ((xT[:, k, tsl], ("xT", tile), whb, "whb"), (xT[:, k, tsl], ("xT", tile), wlb, "wlb"),
                                         (loT[:, k, :], "loT", whb, "whb")):
                    mm(p, ps[6][:, 0:36], a_, w_[:, k, :], n == 0, n == 47, reads=[ak_, wk_], writes=["ps6"])
                    n += 1
            tt_(p, "dve", logits[:, tile * 36:(tile + 1) * 36], ps[6][:, 0:36], brb, ALU.add, reads=["ps6", "brb"], writes=["logits"])


def make_in_map(x_b, weights, consts):
    m = {"x": np.ascontiguousarray(x_b, dtype=np.float32)}
    weights = dict(weights)
    wr = np.concatenate([np.asarray(weights["w_router_group"]), np.asarray(weights["w_router_expert"])], axis=-1)
    L_ = wr.shape[0]
    weights["w_router"] = wr.reshape(L_, 16, 128, 36).transpose(0, 2, 1, 3)
    weights["b_router"] = np.concatenate([np.asarray(weights["b_router_group"]), np.asarray(weights["b_router_expert"])], axis=-1)
    for k, shp in WEIGHT_SHAPES.items():
        w = np.asarray(weights[k], dtype=np.float32)
        m[k] = np.ascontiguousarray(w.reshape([w.shape[0]] + shp))
    for k, v in consts.items():
        m["c_" + k] = np.ascontiguousarray(v.reshape(CONST_SHAPES[k]))
    return m


def bc(ap, axis, shape):
    return ap.unsqueeze(axis).to_broadcast(shape)


def red(p, eng, out, in_, op, axis, reads=(), writes=()):
    return p.op(eng, lambda e: e.tensor_reduce(out=out, in_=in_, axis=axis, op=op), reads, writes)


def stage_moe(nc, p, ar, ps, psb, xT, XT_ALL, identb, identf, onesf, wl, C, XA, XS, YB, dst, bcast_load,
              layer_norm_rows, tile_to_xT, logits, first, last):
    p.barrier()
    ar.reset()
    IOA = bass.IndirectOffsetOnAxis
    desti = ar.i32(32)
    idxW = ar.i32(NBLK * 4).rearrange("p (b q) -> p b q", q=4)
    idxW2 = ar.i32(NBLK * 4).rearrange("p (b q) -> p b q", q=4)
    wts = ar.f32(32).rearrange("p (a k) -> p a k", k=2)
    mark = ar.off
    L3 = logits.rearrange("p (a c) -> p a c", c=36)
    lg = L3[:, :, 0:4]
    le = L3[:, :, 4:36]
    mx = ar.f32(16)
    ohg = ar.f32(64).rearrange("p (a g) -> p a g", g=4)
    eg = ar.f32(64).rearrange("p (a g) -> p a g", g=4)
    se = ar.f32(16)
    psel = ar.f32(16)
    pen = ar.f32(64).rearrange("p (a g) -> p a g", g=4)
    lem = ar.f32(512).rearrange("p (a g e) -> p a g e", g=4, e=8)
    lem2 = ar.f32(512).rearrange("p (a c) -> p a c", c=32)
    m1 = ar.f32(16)
    m2 = ar.f32(16)
    E = ar.f32(1024).rearrange("p (k a c) -> p k a c", k=2, c=32)
    tot = ar.f32(1024).rearrange("p (j c) -> p j c", c=32)
    off = ar.f32(1024).rearrange("p (j c) -> p j c", c=32)
    Sm = ar.f32(1024).rearrange("p (j c) -> p j c", c=32)
    cnt = ar.f32(32)
    cnti = ar.i32(32)
    padv = ar.f32(32)
    pst = ar.f32(33)
    pend = ar.f32(32)
    destf = ar.f32(32)
    thr = ar.f32(NBLK * 32).rearrange("p (b c) -> p b c", c=32)
    cmpv = ar.f32(NBLK * 32).rearrange("p (b c) -> p b c", c=32)
    bex = ar.f32(NBLK)
    iow = ar.f32(4)
    idxf = ar.f32(NBLK * 4).rearrange("p (b q) -> p b q", q=4)
    tris = ar.f32(128)
    e21 = ar.f32(16)
    rden = ar.f32(16)
    dm(p, "sync", thr.rearrange("p b c -> p (b c)"), C["thr"], writes=["thr"])
    dm(p, "sync", iow, C["iow"], writes=["iow"])
    dm(p, "sync", tris, C["tri_s"], writes=["tris"])
    lemf = lem.rearrange("p a g e -> p a (g e)")
    red(p, "dve", mx, lg, ALU.max, AX.X, reads=["logits"], writes=["mx"])
    tt_(p, "dve", ohg, lg, bc(mx, 2, [128, NT, 4]), ALU.is_equal, reads=["logits", "mx"], writes=["ohg"])
    tt_(p, "dve", eg, lg, bc(mx, 2, [128, NT, 4]), ALU.subtract, reads=["logits", "mx"], writes=["eg"])
    act(p, eg, eg, AF.Exp, reads=["eg"], writes=["eg"])
    red(p, "dve", se, eg, ALU.add, AX.X, reads=["eg"], writes=["se"])
    p.op("dve", lambda e: e.reciprocal(out=psel, in_=se), reads=["se"], writes=["psel"])
    ts(p, "dve", pen, ohg, -1.0, 1e30, ALU.add, ALU.mult, reads=["ohg"], writes=["pen"])
    tt_(p, "dve", lem, le.rearrange("p a (g e) -> p a g e", e=8), bc(pen, 3, [128, NT, 4, 8]), ALU.add, reads=["logits", "pen"], writes=["lem"])
    red(p, "dve", m1, lemf, ALU.max, AX.X, reads=["lem"], writes=["m1"])
    tt_(p, "dve", E[:, 0], lemf, bc(m1, 2, [128, NT, 32]), ALU.is_equal, reads=["lem", "m1"], writes=["E"])
    stt(p, lem2, E[:, 0], -1e30, lemf, ALU.mult, ALU.add, reads=["E", "lem"], writes=["lem2"])
    red(p, "dve", m2, lem2, ALU.max, AX.X, reads=["lem2"], writes=["m2"])
    tt_(p, "dve", E[:, 1], lem2, bc(m2, 2, [128, NT, 32]), ALU.is_equal, reads=["lem2", "m2", "E"], writes=["E"])
    tt_(p, "dve", e21, m2, m1, ALU.subtract, reads=["m1", "m2"], writes=["e21"])
    act(p, e21, e21, AF.Exp, reads=["e21"], writes=["e21"])
    ts(p, "dve", rden, e21, 1.0, None, ALU.add, reads=["e21"], writes=["rden"])
    p.op("dve", lambda e: e.reciprocal(out=rden, in_=rden), reads=["rden"], writes=["rden"])
    tt_(p, "dve", rden, rden, psel, ALU.mult, reads=["rden", "psel"], writes=["rden"])
    cp(p, "dve", wts[:, :, 0], rden, reads=["rden"], writes=["wts"])
    tt_(p, "dve", wts[:, :, 1], rden, e21, ALU.mult, reads=["rden", "e21", "wts"], writes=["wts"])
    Ef = E.rearrange("p k a c -> p (k a c)")
    for hf in range(2):
        mm(p, ps[hf][:, :], tris, Ef[:, hf * 512:(hf + 1) * 512], True, True, reads=["tris", "E"], writes=["ps%d" % hf])
        mm(p, ps[2 + hf][:, :], onesf[:], Ef[:, hf * 512:(hf + 1) * 512], True, True, reads=["onesf", "E"], writes=["ps%d" % (2 + hf)])
        cp(p, "dve", tot.rearrange("p j c -> p (j c)")[:, hf * 512:(hf + 1) * 512], ps[2 + hf][:, :], reads=["ps%d" % (2 + hf)], writes=["tot"])
    ms(p, "dve", off[:, 0, :], 0.0, writes=["off"])
    for j in range(31):
        tt_(p, "dve", off[:, j + 1, :], off[:, j, :], tot[:, j, :], ALU.add, reads=["off", "tot"], writes=["off"])
    tt_(p, "dve", cnt, off[:, 31, :], tot[:, 31, :], ALU.add, reads=["off", "tot"], writes=["cnt"])
    ts(p, "dve", cnt, cnt, float(BLKS - 1), None, ALU.add, reads=["cnt"], writes=["cnt"])
    cp(p, "dve", cnti, cnt, reads=["cnt"], writes=["cnti"])
    p.op("dve", lambda e: e.tensor_single_scalar(out=cnti, in_=cnti, scalar=8, op=ALU.arith_shift_right), reads=["cnti"], writes=["cnti"])
    cp(p, "dve", padv, cnti, reads=["cnti"], writes=["padv"])
    ts(p, "dve", padv, padv, float(BLKS), None, ALU.mult, reads=["padv"], writes=["padv"])
    ms(p, "dve", pst[:, 0:1], 0.0, writes=["pst"])
    for e_ in range(32):
        tt_(p, "dve", pst[:, e_ + 1:e_ + 2], pst[:, e_:e_ + 1], padv[:, e_:e_ + 1], ALU.add, reads=["pst", "padv"], writes=["pst"])
    cp(p, "dve", pend, pst[:, 1:33], reads=["pst"], writes=["pend"])
    tt_(p, "dve", Sm, off, bc(pst[:, 0:32], 1, [128, 32, 32]), ALU.add, reads=["off", "pst"], writes=["Sm"])
    Smf = Sm.rearrange("p j c -> p (j c)")
    for hf in range(2):
        tt_(p, "dve", Smf[:, hf * 512:(hf + 1) * 512], ps[hf][:, :], Smf[:, hf * 512:(hf + 1) * 512], ALU.add, reads=["ps%d" % hf, "Sm"], writes=["Sm"])
    tt_(p, "dve", Smf, Smf, Ef, ALU.mult, reads=["Sm", "E"], writes=["Sm"])
    red(p, "dve", destf, Sm, ALU.add, AX.X, reads=["Sm"], writes=["destf"])
    cp(p, "dve", desti, destf, reads=["destf"], writes=["desti"])
    tt_(p, "dve", cmpv, thr, bc(pend, 1, [128, NBLK, 32]), ALU.is_ge, reads=["thr", "pend"], writes=["cmpv"])
    red(p, "dve", bex, cmpv, ALU.add, AX.X, reads=["cmpv"], writes=["bex"])
    bigt = ar.f32(NBLK)
    ts(p, "dve", bigt, bex, 31.5, (1.0e6 if SKIP_UNUSED else 0.0), ALU.is_ge, ALU.mult, reads=["bex"], writes=["bigt"])
    ts(p, "dve", bex, bex, 31.0, None, ALU.min, reads=["bex", "bigt"], writes=["bex"])
    cp(p, "dve", idxf, bc(iow, 1, [128, NBLK, 4]), reads=["iow"], writes=["idxf"])
    stt(p, idxf, bc(bex, 2, [128, NBLK, 4]), 512.0, idxf, ALU.mult, ALU.add, reads=["bex", "idxf"], writes=["idxf"])
    ts(p, "dve", idxf, idxf, float(wl["_l"] * 16384), None, ALU.add, reads=["idxf"], writes=["idxf"])
    cp(p, "dve", idxW2, idxf, reads=["idxf"], writes=["idxW2"])
    tt_(p, "dve", idxf, idxf, bc(bigt, 2, [128, NBLK, 4]), ALU.add, reads=["bigt", "idxf", "idxW2"], writes=["idxf"])
    cp(p, "dve", idxW, idxf, reads=["idxf"], writes=["idxW"])

    p.barrier()
    ar.off = mark
    xb = [ar.bf(D) for _ in range(2)]
    if first:
        ms(p, "pool", xb[0], 0.0, writes=[("xb", 0)])
        for b in range(NSLOT // 128):
            dm(p, "sync", XS[b * 128:(b + 1) * 128, :], xb[0], reads=[("xb", 0)], writes=["XS"])
        p.barrier()
    for tile in range(NT):
        b = tile % 2
        p.dma("pool", lambda e, tile=tile, b=b: e.dma_start(out=xb[b], in_=XA[tile * 128:(tile + 1) * 128, :]), writes=[("xb", b)])
        for k in range(2):
            j = k * NT + tile
            p.dma("pool", lambda e, j=j, b=b: e.indirect_dma_start(
                out=XS[:, :], out_offset=IOA(ap=desti[:, j:j + 1], axis=0), in_=xb[b], in_offset=None),
                reads=[("xb", b), "desti"], writes=["XSs"])
    p.barrier()
    ar.off = mark
    w1b = [ar.bf(16 * 512).rearrange("p (j f) -> p j f", f=512) for _ in range(2)]
    w3b = [ar.bf(16 * 512).rearrange("p (j f) -> p j f", f=512) for _ in range(2)]
    w2b = [ar.bf(4 * D).rearrange("p (j c) -> p j c", c=D) for _ in range(2)]
    xs = [ar.bf(D) for _ in range(2)]
    XsT = [ar.bf(16 * 128).rearrange("p (j s) -> p j s", s=128) for _ in range(2)]
    hs = [ar.f32(512) for _ in range(2)]
    Hb = [ar.bf(512) for _ in range(2)]
    HT = [ar.bf(512).rearrange("p (j s) -> p j s", s=128) for _ in range(2)]
    ybs = [ar.f32(D) for _ in range(2)]
    W1 = wl["_W"]["w_exp_gate"].rearrange("l e (r a) f -> (l e r) (a f)", a=4)
    W3 = wl["_W"]["w_exp_up"].rearrange("l e (r a) f -> (l e r) (a f)", a=4)
    W2 = wl["_W"]["w_exp_down"].rearrange("l e f c -> (l e f) c")
    nrows = int(W1.shape[0])
    _rh = {}

    def bkw_(e):
        if not SKIP_UNUSED:
            return {}
        if "r" not in _rh:
            r = e.alloc_register("moe_bound_%d" % wl["_l"])
            e.reg_mov(r, nrows - 1)
            _rh["r"] = r
        return dict(bounds_check=_rh["r"], oob_is_err=False)
    for b in range(NBLK):
        s2 = b % 2
        for q in range(4):
            p.dma("pool", lambda e, b=b, q=q, s2=s2: e.indirect_dma_start(
                out=w1b[s2][:, 4 * q:4 * q + 4, :].rearrange("p j f -> p (j f)"), out_offset=None, in_=W1,
                in_offset=IOA(ap=idxW[:, b, q:q + 1], axis=0), **bkw_(e)), reads=["idxW"], writes=[("w1b", s2)])
            p.dma("pool", lambda e, b=b, q=q, s2=s2: e.indirect_dma_start(
                out=w3b[s2][:, 4 * q:4 * q + 4, :].rearrange("p j f -> p (j f)"), out_offset=None, in_=W3,
                in_offset=IOA(ap=idxW[:, b, q:q + 1], axis=0), **bkw_(e)), reads=["idxW"], writes=[("w3b", s2)])
        for q in range(4):
            p.dma("pool", lambda e, b=b, q=q, s2=s2: e.indirect_dma_start(
                out=w2b[s2].rearrange("p j c -> p (j c)")[:, q * D:(q + 1) * D], out_offset=None, in_=W2,
                in_offset=IOA(ap=idxW[:, b, q:q + 1], axis=0), **bkw_(e)), reads=["idxW"], writes=[("w2b", s2)])
        for sub in range(2):
            r0 = b * BLKS + sub * 128
            dm(p, "sync", xs[sub], XS[r0:r0 + 128, :], reads=["XSs"], writes=[("xs", sub)])
            xv = xs[sub].rearrange("s (q j) -> s j q", j=16)
            for hf in range(2):
                for jj in range(8):
                    j = hf * 8 + jj
                    trp(p, psb(hf, 1024)[:, jj * 128:(jj + 1) * 128], xv[:, j, :], identb[:], reads=[("xs", sub), "identb"], writes=["ps%d" % hf])
                cp(p, "act" if hf == 0 else "dve", XsT[sub][:, hf * 8:(hf + 1) * 8, :].rearrange("p j s -> p (j s)"), psb(hf, 1024),
                   reads=["ps%d" % hf], writes=[("XsT", sub)])
        for sub in range(2):
            for j in range(16):
                mm(p, ps[2][:, :], XsT[sub][:, j, :], w1b[s2][:, j, :], j == 0, j == 15, reads=[("XsT", sub), ("w1b", s2)], writes=["ps2"])
            for j in range(16):
                mm(p, ps[3][:, :], XsT[sub][:, j, :], w3b[s2][:, j, :], j == 0, j == 15, reads=[("XsT", sub), ("w3b", s2)], writes=["ps3"])
            act(p, hs[sub], ps[2][:, :], AF.Silu, reads=["ps2"], writes=[("hs", sub)])
            tt_(p, "dve", Hb[sub], ps[3][:, :], hs[sub], ALU.mult, reads=["ps3", ("hs", sub)], writes=[("Hb", sub)])
        for sub in range(2):
            hv = Hb[sub].rearrange("s (q j) -> s j q", j=4)
            for j in range(4):
                trp(p, psb(sub, 1024)[:, j * 128:(j + 1) * 128], hv[:, j, :], identb[:], reads=[("Hb", sub), "identb"], writes=["ps%d" % sub])
            cp(p, "act", HT[sub].rearrange("p j s -> p (j s)"), psb(sub, 1024)[:, 0:512], reads=["ps%d" % sub], writes=[("HT", sub)])
        for sub in range(2):
            r0 = b * BLKS + sub * 128
            for oc in range(4):
                for j in range(4):
                    mm(p, ps[4 + oc][:, :], HT[sub][:, j, :], w2b[s2][:, j, oc * 512:(oc + 1) * 512], j == 0, j == 3, reads=[("HT", sub), ("w2b", s2)], writes=["ps%d" % (4 + oc)])
                cp(p, "act" if oc % 2 == 0 else "dve", ybs[sub][:, oc * 512:(oc + 1) * 512], ps[4 + oc][:, :], reads=["ps%d" % (4 + oc)], writes=[("ybs", sub)])
            dm(p, "sync", YB[r0:r0 + 128, :], ybs[sub], reads=[("ybs", sub)], writes=["YB"])
    p.barrier()
    ar.off = mark
    lng = ar.f32(D)
    lnb = ar.f32(D)
    y0 = [ar.f32(D) for _ in range(2)]
    y1 = [ar.f32(D) for _ in range(2)]
    x1 = [ar.f32(D) for _ in range(2)]
    hb2 = [ar.bf(D) for _ in range(2)]
    stats = ar.f32(24)
    mv = ar.f32(4)
    bcast_load(lng, "lng", wl["ln2_g"], D)
    bcast_load(lnb, "lnb", wl["ln2_b"], D)
    for tile in range(NT):
        b = tile % 2
        rs = slice(tile * 128, (tile + 1) * 128)
        for k, yk in ((0, y0), (1, y1)):
            j = k * NT + tile
            p.dma("pool", lambda e, j=j, yk=yk, b=b: e.indirect_dma_start(
                out=yk[b], out_offset=None, in_=YB[:, :], in_offset=IOA(ap=desti[:, j:j + 1], axis=0)),
                reads=["desti", "YB"], writes=[("y%d" % k, b)])
        dm(p, "sync", x1[b], XA[rs, :], writes=[("x1", b)])
        ts(p, "dve", y0[b], y0[b], wts[:, tile, 0:1], None, ALU.mult, reads=[("y0", b), "wts"], writes=[("y0", b)])
        stt(p, y0[b], y1[b], wts[:, tile, 1:2], y0[b], ALU.mult, ALU.add, reads=[("y0", b), ("y1", b), "wts"], writes=[("y0", b)])
        stt(p, x1[b], x1[b], ALPHA, y0[b], ALU.mult, ALU.add, reads=[("y0", b), ("x1", b)], writes=[("x1", b)])
        layer_norm_rows(x1[b], ("x1", b), x1[b], ("x1", b), D, lng, lnb, ("lng", "lnb"), stats, mv, "ln2")
        dm(p, "sync", dst[rs, :], x1[b], reads=[("x1", b)], writes=["dst"])
        if not last:
            tile_to_xT(x1[b], ("x1", b), tile, 0, hb2[b], ("hb2", b))


DEPTH = 4
_NC_CACHE = {}


def kernel(**inputs):
    x = np.asarray(inputs["x"], dtype=np.float32)
    weights = {k: inputs[k] for k in WEIGHT_SHAPES if k in inputs}
    consts = host_consts()
    if "nc" not in _NC_CACHE:
        _NC_CACHE["nc"] = build(DEPTH)
    nc = _NC_CACHE["nc"]
    base = make_in_map(x[0], weights, consts)
    in_maps = []
    for c in range(8):
        m = dict(base)
        m["x"] = np.ascontiguousarray(x[c])
        in_maps.append(m)
    res = run_bass_kernel_spmd(nc, in_maps, core_ids=list(range(8)))
    return np.stack([np.asarray(r["y"], dtype=np.float32) for r in res.results], axis=0)
```
